# Optimizing a Trainium2 kernel written in Bass

```python
import math
import jax, jax.numpy as jnp
from jax import lax
import numpy as np

D_MODEL = 1024
BATCH = 1
SEQ = 16384
DEPTH = 2

GRID_W = 64
CTX_LEN = 256
RMS_EPS = 1e-6
ROPE_BASE = 10000.0

FNET_GROUPS = 4
FNET_GROUP_DIM = 64
FNET_WIDTH = FNET_GROUPS * FNET_GROUP_DIM
NA_HEADS = 12
NA_HEAD_DIM = 64
NA_WIDTH = NA_HEADS * NA_HEAD_DIM
NA_KH = 8
NA_KW = 16
NA_KEYW = 2 * NA_KW
NA_NCB = GRID_W // NA_KW
AB_IN = FNET_WIDTH + 3 * NA_WIDTH
AB_OUT = FNET_WIDTH + NA_WIDTH

RET_HEADS = 4
RET_DK = D_MODEL // RET_HEADS
RET_DV = 2 * RET_DK
RET_QK = RET_HEADS * RET_DK
RET_V = RET_HEADS * RET_DV
RET_IN = 2 * RET_QK + 2 * RET_V
RET_CHUNK = 128

N_EXPERTS = 16
N_GROUPS = 4
EXPERTS_PER_GROUP = N_EXPERTS // N_GROUPS
TOP_K = 2
D_EXPERT = D_MODEL // 2

kernel_name = 'hybrid_fnet_natten_retnet_moe_dit'


def rmsnorm(x, g):
    xf = x.astype(jnp.float32)
    y = xf * lax.rsqrt(jnp.mean(xf * xf, axis=-1, keepdims=True) + RMS_EPS)
    return y.astype(x.dtype) * g


def modulate(x, shift, scale):
    return x * (1 + scale) + shift


def _rope_half(x, pos):
    n = x.shape[-1] // 2
    inv_freq = ROPE_BASE ** (-jnp.arange(n, dtype=jnp.float32) / n)
    ang = pos.astype(jnp.float32)[:, None] * inv_freq[None, :]
    cos = jnp.cos(ang)[None, :, None, :]
    sin = jnp.sin(ang)[None, :, None, :]
    xf = x.astype(jnp.float32)
    x1, x2 = xf[..., :n], xf[..., n:]
    return jnp.concatenate([x1 * cos - x2 * sin, x2 * cos + x1 * sin], axis=-1)


def axial_rope(x, row, col):
    half = x.shape[-1] // 2
    return jnp.concatenate([_rope_half(x[..., :half], row), _rope_half(x[..., half:], col)], axis=-1).astype(x.dtype)


def fourier_mix(a):
    B, N, _ = a.shape
    ag = a.reshape(B, N, FNET_GROUPS, FNET_GROUP_DIM).astype(jnp.float32)
    return jnp.fft.fft2(ag, axes=(1, 3), norm='ortho').real.astype(a.dtype).reshape(B, N, FNET_WIDTH)


def dense_attention(q, k, v):
    s = jnp.einsum('bqhd,bkhd->bhqk', q, k).astype(jnp.float32) * (q.shape[-1] ** -0.5)
    p = jax.nn.softmax(s, axis=-1).astype(v.dtype)
    return jnp.einsum('bhqk,bkhd->bqhd', p, v)


def _na_static():
    j = np.arange(NA_NCB)
    key_start = np.clip(j * NA_KW - NA_KW // 2, 0, GRID_W - NA_KEYW)
    key_cols = key_start[:, None] + np.arange(NA_KEYW)[None, :]
    q_cols = j[:, None] * NA_KW + np.arange(NA_KW)[None, :]
    win_start = np.clip(q_cols - NA_KW // 2, 0, GRID_W - NA_KW)[:, :, None]
    kc = key_cols[:, None, :]
    col_mask = (kc >= win_start) & (kc < win_start + NA_KW)
    dc_idx = np.clip(kc - q_cols[:, :, None] + NA_KW - 1, 0, 2 * NA_KW - 2)
    return key_cols, col_mask, dc_idx


def neighborhood_attention(q, k, v, k_ctx, v_ctx, rpb):
    B, L, H, Dh = q.shape
    rows = L // GRID_W
    kh = min(NA_KH, rows)
    scale = Dh ** -0.5
    key_cols, col_mask, dc_idx = _na_static()
    qg = q.reshape(B, rows, NA_NCB, NA_KW, H, Dh)
    kg = k.reshape(B, rows, GRID_W, H, Dh)
    vg = v.reshape(B, rows, GRID_W, H, Dh)
    rpb_cols = rpb[:, :, dc_idx]
    mask = jnp.asarray(col_mask)[None, None, :, :, None, :]

    def row_block(r):
        rs = jnp.clip(r - kh // 2, 0, rows - kh)
        k_blk = lax.dynamic_slice_in_dim(kg, rs, kh, axis=1)[:, :, key_cols]
        v_blk = lax.dynamic_slice_in_dim(vg, rs, kh, axis=1)[:, :, key_cols]
        q_r = lax.dynamic_index_in_dim(qg, r, axis=1, keepdims=False)
        s_loc = jnp.einsum('bjqhd,brjkhd->bhjqrk', q_r, k_blk).astype(jnp.float32) * scale
        dr = rs + jnp.arange(kh) - r + (NA_KH - 1)
        bias = jnp.transpose(rpb_cols[:, dr], (0, 2, 3, 1, 4)).astype(jnp.float32)
        s_loc = jnp.where(mask, s_loc + bias[None], -jnp.inf).reshape(B, H, NA_NCB, NA_KW, kh * NA_KEYW)
        s_ctx = jnp.einsum('bjqhd,bkhd->bhjqk', q_r, k_ctx).astype(jnp.float32) * scale
        p = jax.nn.softmax(jnp.concatenate([s_loc, s_ctx], axis=-1), axis=-1).astype(v.dtype)
        p_loc = p[..., :kh * NA_KEYW].reshape(B, H, NA_NCB, NA_KW, kh, NA_KEYW)
        p_ctx = p[..., kh * NA_KEYW:]
        return (jnp.einsum('bhjqrk,brjkhd->bjqhd', p_loc, v_blk)
                + jnp.einsum('bhjqk,bkhd->bjqhd', p_ctx, v_ctx))

    o = lax.map(row_block, jnp.arange(rows))
    return jnp.moveaxis(o, 0, 1).reshape(B, L, H * Dh)


def fnet_na_mixer(xm, cm, w_in, w_out, rpb, need_ctx):
    B, L, _ = xm.shape
    pl = xm @ w_in
    pc = cm @ w_in

    def heads(p):
        qkv = p[..., FNET_WIDTH:].reshape(p.shape[0], p.shape[1], 3, NA_HEADS, NA_HEAD_DIM)
        return qkv[:, :, 0], qkv[:, :, 1], qkv[:, :, 2]

    ql, kl, vl = heads(pl)
    qc, kc, vc = heads(pc)
    a_lat = fourier_mix(pl[..., :FNET_WIDTH])
    o_lat = neighborhood_attention(ql, kl, vl, kc, vc, rpb)
    y_lat = jnp.concatenate([a_lat, o_lat], axis=-1) @ w_out
    y_ctx = None
    if need_ctx:
        a_ctx = fourier_mix(pc[..., :FNET_WIDTH])
        o_ctx = dense_attention(qc, kc, vc).reshape(B, -1, NA_WIDTH)
        y_ctx = jnp.concatenate([a_ctx, o_ctx], axis=-1) @ w_out
    return y_lat, y_ctx


def retention_scan(q, k, v, log_gamma):
    B, H, N, dk = q.shape
    dv = v.shape[-1]
    nc = N // RET_CHUNK

    def chunks(t):
        return jnp.moveaxis(t.reshape(B, H, nc, RET_CHUNK, t.shape[-1]), 2, 0)

    pos = jnp.arange(RET_CHUNK, dtype=jnp.float32)
    diff = pos[:, None] - pos[None, :]
    lower = diff >= 0
    intra_decay = jnp.where(lower, jnp.exp(jnp.where(lower, diff, 0.0)[None] * log_gamma[:, None, None]), 0.0)
    q_decay = jnp.exp((pos + 1.0)[None, :] * log_gamma[:, None])[..., None]
    k_decay = jnp.exp((RET_CHUNK - 1.0 - pos)[None, :] * log_gamma[:, None])[..., None]
    chunk_decay = jnp.exp(RET_CHUNK * log_gamma)[:, None, None]

    def step(state, qkv):
        qc, kc, vc = qkv
        scores = jnp.einsum('bhnd,bhmd->bhnm', qc, kc) * intra_decay
        out = (jnp.einsum('bhnm,bhmv->bhnv', scores, vc)
               + jnp.einsum('bhnd,bhdv->bhnv', qc * q_decay, state))
        state = state * chunk_decay + jnp.einsum('bhmd,bhmv->bhdv', kc * k_decay, vc)
        return state, out

    state0 = jnp.zeros((B, H, dk, dv), jnp.float32)
    _, out = lax.scan(step, state0, (chunks(q), chunks(k), chunks(v)))
    return jnp.moveaxis(out, 0, 2).reshape(B, H, N, dv)


def retention_mixer(xm, cm, w_in, w_out, decay_param, row, col, need_ctx):
    B, L, _ = xm.shape
    n_ctx = cm.shape[1]

    def split(p):
        n = p.shape[1]
        q = p[..., :RET_QK].reshape(B, n, RET_HEADS, RET_DK)
        k = p[..., RET_QK:2 * RET_QK].reshape(B, n, RET_HEADS, RET_DK) * (RET_DK ** -0.5)
        v = p[..., 2 * RET_QK:2 * RET_QK + RET_V].reshape(B, n, RET_HEADS, RET_DV)
        g = p[..., 2 * RET_QK + RET_V:]
        return q, k, v, g

    ql, kl, vl, gl = split(xm @ w_in)
    qc, kc, vc, gc = split(cm @ w_in)
    ql = axial_rope(ql, row, col)
    kl = axial_rope(kl, row, col)

    def bhnd(t_ctx, t_lat):
        return jnp.concatenate([t_ctx, t_lat], axis=1).transpose(0, 2, 1, 3).astype(jnp.float32)

    q, k, v = bhnd(qc, ql), bhnd(kc, kl), bhnd(vc, vl)
    log_gamma = jnp.log1p(-jnp.exp(decay_param.astype(jnp.float32)))

    def flip(t):
        return jnp.concatenate([t[:, :, :n_ctx][:, :, ::-1], t[:, :, n_ctx:][:, :, ::-1]], axis=2)

    o = retention_scan(q, k, v, log_gamma[0]) + flip(retention_scan(flip(q), flip(k), flip(v), log_gamma[1]))

    def gated_out(o_part, g):
        on = o_part * lax.rsqrt(jnp.mean(o_part * o_part, axis=-1, keepdims=True) + RMS_EPS)
        on = on.transpose(0, 2, 1, 3).reshape(B, -1, RET_V).astype(g.dtype)
        return (jax.nn.silu(g) * on) @ w_out

    y_lat = gated_out(o[:, :, n_ctx:], gl)
    y_ctx = gated_out(o[:, :, :n_ctx], gc) if need_ctx else None
    return y_lat, y_ctx


def moe_ffn(tok, router_w, router_b, w1, w3, w2):
    T = tok.shape[0]
    scores = jax.nn.sigmoid((tok @ router_w).astype(jnp.float32))
    biased = (scores + router_b.astype(jnp.float32)).reshape(T, N_GROUPS, EXPERTS_PER_GROUP)
    group_score = lax.top_k(biased, TOP_K)[0].sum(axis=-1)
    g_sel = jnp.argmax(group_score, axis=-1)
    in_group = jnp.take_along_axis(biased, g_sel[:, None, None], axis=1)[:, 0]
    _, local = lax.top_k(in_group, TOP_K)
    expert_idx = g_sel[:, None] * EXPERTS_PER_GROUP + local
    w = jnp.take_along_axis(scores, expert_idx, axis=-1)
    w = w / jnp.sum(w, axis=-1, keepdims=True)
    gate = jnp.sum(jax.nn.one_hot(expert_idx, N_EXPERTS, dtype=jnp.float32) * w[..., None], axis=1).astype(tok.dtype)
    out = jnp.zeros_like(tok)
    for e in range(N_EXPERTS):
        hid = jax.nn.silu(tok @ w1[e]) * (tok @ w3[e])
        out = out + gate[:, e:e + 1] * (hid @ w2[e])
    return out


def setup_inputs(seed: int = 0) -> dict:
    key = jax.random.key(seed)
    ks = jax.random.split(key, 20)
    n_even = (DEPTH + 1) // 2
    n_odd = DEPTH // 2

    def nrm(k, shape, scale):
        return jax.random.normal(k, shape, jnp.float32) * scale

    base_decay = -(5.0 + jnp.arange(RET_HEADS, dtype=jnp.float32)) * math.log(2.0)
    return {
        'x': nrm(ks[0], (BATCH, SEQ, D_MODEL), 1.0),
        'c': nrm(ks[1], (BATCH, D_MODEL), 1.0),
        'ctx': nrm(ks[2], (BATCH, CTX_LEN, D_MODEL), 1.0),
        'c_ctx': nrm(ks[3], (D_MODEL,), 1.0),
        'ada_w': nrm(ks[4], (DEPTH, D_MODEL, 6 * D_MODEL), 0.5 * D_MODEL ** -0.5),
        'ada_b': nrm(ks[5], (DEPTH, 6 * D_MODEL), 0.02),
        'norm_g': 1.0 + nrm(ks[6], (DEPTH, 2, D_MODEL), 0.02),
        'final_norm_g': 1.0 + nrm(ks[7], (D_MODEL,), 0.02),
        'mixab_w_in': nrm(ks[8], (n_even, D_MODEL, AB_IN), D_MODEL ** -0.5),
        'mixab_w_out': nrm(ks[9], (n_even, AB_OUT, D_MODEL), AB_OUT ** -0.5),
        'na_rpb': nrm(ks[10], (n_even, NA_HEADS, 2 * NA_KH - 1, 2 * NA_KW - 1), 0.1),
        'ret_w_in': nrm(ks[11], (n_odd, D_MODEL, RET_IN), D_MODEL ** -0.5),
        'ret_w_out': nrm(ks[12], (n_odd, RET_V, D_MODEL), RET_V ** -0.5),
        'ret_decay': base_decay[None, None, :] + nrm(ks[13], (n_odd, 2, RET_HEADS), 0.05),
        'router_w': nrm(ks[14], (D_MODEL, N_EXPERTS), D_MODEL ** -0.5),
        'router_b': nrm(ks[15], (N_EXPERTS,), 0.01),
        'moe_w1': nrm(ks[16], (DEPTH, N_EXPERTS, D_MODEL, D_EXPERT), D_MODEL ** -0.5),
        'moe_w3': nrm(ks[17], (DEPTH, N_EXPERTS, D_MODEL, D_EXPERT), D_MODEL ** -0.5),
        'moe_w2': nrm(ks[18], (DEPTH, N_EXPERTS, D_EXPERT, D_MODEL), D_EXPERT ** -0.5),
    }


def reference(x, c, ctx, c_ctx, ada_w, ada_b, norm_g, final_norm_g, mixab_w_in, mixab_w_out, na_rpb,
              ret_w_in, ret_w_out, ret_decay, router_w, router_b, moe_w1, moe_w3, moe_w2):
    B, L, D = x.shape
    t = jnp.arange(L)
    row, col = t // GRID_W, t % GRID_W
    h, hc = x, ctx
    for layer in range(DEPTH):
        need_ctx = layer < DEPTH - 1
        mod = jax.nn.silu(c) @ ada_w[layer] + ada_b[layer]
        mod_c = jax.nn.silu(c_ctx) @ ada_w[layer] + ada_b[layer]
        sh1, sc1, g1, sh2, sc2, g2 = [m[:, None, :] for m in jnp.split(mod, 6, axis=-1)]
        sh1c, sc1c, g1c, sh2c, sc2c, g2c = jnp.split(mod_c, 6, axis=-1)
        xm = modulate(rmsnorm(h, norm_g[layer, 0]), sh1, sc1)
        cm = modulate(rmsnorm(hc, norm_g[layer, 0]), sh1c, sc1c)
        idx = layer // 2
        if layer % 2 == 0:
            y, yc = fnet_na_mixer(xm, cm, mixab_w_in[idx], mixab_w_out[idx], na_rpb[idx], need_ctx)
        else:
            y, yc = retention_mixer(xm, cm, ret_w_in[idx], ret_w_out[idx], ret_decay[idx], row, col, need_ctx)
        h = h + g1 * y
        xm2 = modulate(rmsnorm(h, norm_g[layer, 1]), sh2, sc2)
        if need_ctx:
            hc = hc + g1c * yc
            cm2 = modulate(rmsnorm(hc, norm_g[layer, 1]), sh2c, sc2c)
            n_c = cm2.shape[0] * cm2.shape[1]
            tok = jnp.concatenate([cm2.reshape(-1, D), xm2.reshape(-1, D)], axis=0)
            f = moe_ffn(tok, router_w, router_b, moe_w1[layer], moe_w3[layer], moe_w2[layer])
            hc = hc + g2c * f[:n_c].reshape(hc.shape)
            h = h + g2 * f[n_c:].reshape(h.shape)
        else:
            f = moe_ffn(xm2.reshape(-1, D), router_w, router_b, moe_w1[layer], moe_w3[layer], moe_w2[layer])
            h = h + g2 * f.reshape(h.shape)
    return rmsnorm(h, final_norm_g)
```

```python
import numpy as np
import concourse.bass as bass
import concourse.mybir as mybir
from concourse.bass_utils import run_bass_kernel_spmd

F32 = mybir.dt.float32
BF = mybir.dt.bfloat16
AF = mybir.ActivationFunctionType
ALU = mybir.AluOpType
AX = mybir.AxisListType


ENGS = ("pe", "act", "dve", "pool", "sp")


def _region(ap):
    t = ap.tensor
    shape = list(t.shape)
    space = str(ap.space) if hasattr(ap, "space") else ""
    off = int(ap.offset)
    dims = [(int(s), int(c)) for s, c in ap.ap]
    is_dram = "DRam" in type(t).__name__
    if is_dram:
        lo = off
        hi = off + sum((c - 1) * abs(s) for s, c in dims) + 1
        return (t.name, 0, 1, lo, hi)
    F = 1
    for s in shape[1:]:
        F *= int(s)
    p0 = off // F
    f0 = off % F
    p1 = p0
    f1 = f0
    for s, c in dims:
        if c <= 1:
            continue
        if s != 0 and s % F == 0:
            p1 += (c - 1) * (s // F)
        else:
            f1 += (c - 1) * abs(s)
    if "PSum" in type(t).__name__:
        f0 = (f0 // 512) * 512
        f1 = ((f1 // 512) + 1) * 512 - 1
        return (t.name, 0, 128, f0, f1 + 1)
    return (t.name, p0, p1 + 1, f0, f1 + 1)


def _overlap(a, b):
    return a[1] < b[2] and b[1] < a[2] and a[3] < b[4] and b[3] < a[4]


def _covers(a, b):
    return a[1] <= b[1] and a[2] >= b[2] and a[3] <= b[3] and a[4] >= b[4]


class Sched:
    def __init__(self, nc, n_dma_sems=40, same_engine_sync=True):
        self.nc = nc
        self.eng = {"pe": nc.tensor, "act": nc.scalar, "dve": nc.vector,
                    "pool": nc.gpsimd, "sp": nc.sync}
        self.ops = []
        self.same_engine_sync = same_engine_sync
        self.n_dma_sems = n_dma_sems
        self.sem = {e: nc.alloc_semaphore(name="sem_" + e) for e in ENGS}
        self.dsem = [nc.alloc_semaphore(name="dsem%d" % i) for i in range(n_dma_sems)]
        self._dma_rr = 0

    def op(self, eng, fn, reads=(), writes=(), unordered_same=False):
        self.ops.append(dict(eng=eng, fn=fn, r=[_region(a) for a in reads],
                             w=[_region(a) for a in writes], dma=False,
                             relax=unordered_same,
                             xr=[_region(a) for a in reads if "PSum" in type(a.tensor).__name__]))

    def dma(self, q, out, in_, **kw):
        slot = self._dma_rr
        self._dma_rr = (self._dma_rr + 1) % self.n_dma_sems
        e = self.eng[q]
        self.ops.append(dict(eng=q, fn=lambda: e.dma_start(out=out, in_=in_, **kw),
                             r=[_region(in_)], w=[_region(out)], dma=True, slot=slot,
                             relax=False))

    def finalize(self):
        ops = self.ops
        n = len(ops)
        pos_of = [0] * n
        eng_cnt = {e: 0 for e in ENGS}
        eng_ops = {e: [] for e in ENGS}
        for i, o in enumerate(ops):
            eng_cnt[o["eng"]] += 1
            pos_of[i] = eng_cnt[o["eng"]]
            eng_ops[o["eng"]].append(i)
        writes = {}
        reads = {}
        known = {e: {x: 0 for x in ENGS} for e in ENGS}
        known_d = {e: {} for e in ENGS}
        vc = [None] * n
        vcd = [None] * n
        waits = [None] * n
        signal = [False] * n
        slot_last = {}
        dma_target = {}
        slot_cnt = {}
        for i, o in enumerate(ops):
            E = o["eng"]
            deps = set()
            for R in o["r"]:
                for (W, j) in writes.get(R[0], ()):
                    if _overlap(W, R):
                        deps.add(j)
            for Wn in o["w"]:
                for (W, j) in writes.get(Wn[0], ()):
                    if _overlap(W, Wn):
                        deps.add(j)
                for (R, j) in reads.get(Wn[0], ()):
                    if _overlap(R, Wn):
                        deps.add(j)
            for R in o.get("xr", ()):
                for (R2, j) in reads.get(R[0], ()):
                    if ops[j]["eng"] != E and _overlap(R2, R):
                        deps.add(j)
            if o["dma"]:
                s = o["slot"]
                if s in slot_last:
                    deps.add(slot_last[s])
                slot_last[s] = i
                slot_cnt[s] = slot_cnt.get(s, 0) + 1
                dma_target[i] = (s, 16 * slot_cnt[s])
            deps.discard(i)
            kn = known[E]
            kd = known_d[E]
            need_e = {}
            need_d = []
            for j in deps:
                oj = ops[j]
                if oj["dma"]:
                    if kd.get(j, False):
                        continue
                    need_d.append(j)
                else:
                    Ej = oj["eng"]
                    if Ej == E and not o["dma"]:
                        if (not self.same_engine_sync) or E == "pe" or o["relax"]:
                            continue
                    if kn[Ej] >= pos_of[j]:
                        continue
                    if need_e.get(Ej, (0, -1))[0] < pos_of[j]:
                        need_e[Ej] = (pos_of[j], j)
            w_list = []
            for Ej, (p, j) in need_e.items():
                w_list.append(("e", Ej, j))
                signal[j] = True
            for j in need_d:
                w_list.append(("d", None, j))
            waits[i] = w_list
            for kind, Ej, j in w_list:
                for x in ENGS:
                    if vc[j][x] > kn[x]:
                        kn[x] = vc[j][x]
                for dj in vcd[j]:
                    kd[dj] = True
                if kind == "d":
                    kd[j] = True
            if o["dma"]:
                vc[i] = dict(kn)
                vcd[i] = list(kd.keys()) if len(kd) < 64 else list(kd.keys())[-64:]
            else:
                vc[i] = dict(kn)
                vc[i][E] = pos_of[i]
                vcd[i] = list(kd.keys()) if len(kd) < 64 else list(kd.keys())[-64:]
            if len(kd) > 256:
                for key in list(kd.keys())[:128]:
                    del kd[key]
            tag = i
            for Wn in o["w"]:
                lw = writes.setdefault(Wn[0], [])
                lw[:] = [(W, j) for (W, j) in lw if not _covers(Wn, W)]
                lw.append((Wn, tag))
                lr = reads.get(Wn[0])
                if lr:
                    lr[:] = [(R, j) for (R, j) in lr if not _covers(Wn, R)]
            for R in o["r"]:
                lr = reads.setdefault(R[0], [])
                if not o["dma"]:
                    lr[:] = [(R2, j) for (R2, j) in lr
                             if not (ops[j]["eng"] == E and not ops[j]["dma"] and _covers(R, R2))]
                lr.append((R, tag))
        count_of = {}
        for e in ENGS:
            c = 0
            for i in eng_ops[e]:
                if ops[i]["dma"]:
                    continue
                if signal[i]:
                    c += 1
                    count_of[i] = c
        self.stats = dict(n_ops=n, n_signal=sum(signal), n_waits=sum(len(w) for w in waits),
                          per_eng={e: len(eng_ops[e]) for e in ENGS})
        self.trace = {e: [] for e in ENGS}
        for i, o in enumerate(ops):
            E = o["eng"]
            eng = self.eng[E]
            wl = []
            for kind, Ej, j in waits[i]:
                if kind == "e":
                    eng.wait_ge(self.sem[Ej], count_of[j])
                    wl.append(("E" + Ej, count_of[j]))
                else:
                    s, tgt = dma_target[j]
                    eng.wait_ge(self.dsem[s], tgt)
                    wl.append(("D%d" % s, tgt))
            inc = None
            if o["fn"] is not None:
                if o["dma"]:
                    inc = ("D%d" % dma_target[i][0], 16)
                elif signal[i]:
                    inc = ("E" + E, 1)
            self.trace[E].append((wl, inc, i))
            if o["fn"] is None:
                continue
            ins = o["fn"]()
            if o["dma"]:
                s, tgt = dma_target[i]
                ins.then_inc(self.dsem[s], 16)
            elif signal[i]:
                ins.then_inc(self.sem[E], 1)
        return self.stats

    def final_wait(self, aps, eng="sp"):
        self.op(eng, None, reads=list(aps), writes=[])


def simulate(trace):
    sem = {}
    pc = {e: 0 for e in trace}
    progressed = True
    while progressed:
        progressed = False
        for e, tr in trace.items():
            while pc[e] < len(tr):
                wl, inc, i = tr[pc[e]]
                if all(sem.get(sn, 0) >= v for sn, v in wl):
                    if inc:
                        sem[inc[0]] = sem.get(inc[0], 0) + inc[1]
                    pc[e] += 1
                    progressed = True
                else:
                    break
    stuck = {e: (pc[e], len(tr), tr[pc[e]] if pc[e] < len(tr) else None) for e, tr in trace.items()}
    return all(pc[e] == len(tr) for e, tr in trace.items()), stuck, sem


class KB:
    def __init__(self, same_engine_sync=True):
        self.nc = bass.Bass("TRN2", target_bir_lowering=False)
        self.S = Sched(self.nc, same_engine_sync=same_engine_sync)
        self.outs = []
        self._q = 0

    def din(self, name, shape, dt=F32):
        return self.nc.dram_tensor(name, list(shape), dt, kind="ExternalInput").ap()

    def dout(self, name, shape, dt=F32):
        ap = self.nc.dram_tensor(name, list(shape), dt, kind="ExternalOutput").ap()
        self.outs.append(ap)
        return ap

    def sb(self, name, shape, dt=F32):
        return self.nc.alloc_sbuf_tensor(name, list(shape), dt)

    def ps(self, name, shape, dt=F32):
        return self.nc.alloc_psum_tensor(name, list(shape), dt)

    def dma(self, out, in_, q=None):
        if q is None:
            q = "sp"
        self.S.dma(q, out, in_)

    def mm(self, out, lhsT, rhs, start=True, stop=True):
        nc = self.nc
        self.S.op("pe", lambda: nc.tensor.matmul(out, lhsT, rhs, start=start, stop=stop),
                  [lhsT, rhs], [out])

    def tr(self, out, in_, ident):
        nc = self.nc
        self.S.op("pe", lambda: nc.tensor.transpose(out, in_, ident), [in_, ident], [out])

    def act(self, out, in_, func, bias=None, scale=None, accum_out=None):
        nc = self.nc
        kw = {}
        rd = [in_]
        wr = [out]
        if bias is not None:
            kw["bias"] = bias
            if not isinstance(bias, (int, float)):
                rd.append(bias)
        if scale is not None:
            kw["scale"] = scale
            if not isinstance(scale, (int, float)):
                rd.append(scale)
        if accum_out is not None:
            kw["accum_out"] = accum_out
            wr.append(accum_out)
        self.S.op("act", lambda: nc.scalar.activation(out, in_, func, **kw), rd, wr)

    def _veng(self, eng):
        return {"dve": self.nc.vector, "pool": self.nc.gpsimd}[eng]

    def tt(self, out, a, b, op, eng="dve"):
        e = self._veng(eng)
        self.S.op(eng, lambda: e.tensor_tensor(out, a, b, op), [a, b], [out])

    def ts(self, out, a, s1, op0, s2=None, op1=None, eng="dve", accum_out=None):
        e = self._veng(eng)
        rd = [a]
        wr = [out]
        for s in (s1, s2):
            if s is not None and not isinstance(s, (int, float)):
                rd.append(s)
        kw = {}
        if accum_out is not None:
            kw["accum_out"] = accum_out
            wr.append(accum_out)
        if op1 is None:
            self.S.op(eng, lambda: e.tensor_scalar(out, a, s1, None, op0, **kw), rd, wr)
        else:
            self.S.op(eng, lambda: e.tensor_scalar(out, a, s1, s2, op0, op1, **kw), rd, wr)

    def stt(self, out, a, s, b, op0, op1, eng="dve"):
        e = self._veng(eng)
        rd = [a, b]
        if not isinstance(s, (int, float)):
            rd.append(s)
        self.S.op(eng, lambda: e.scalar_tensor_tensor(out, a, s, b, op0, op1), rd, [out])

    def copy(self, out, in_, eng="dve"):
        if eng == "act":
            nc = self.nc
            self.S.op("act", lambda: nc.scalar.activation(out, in_, AF.Copy), [in_], [out])
        else:
            e = self._veng(eng)
            self.S.op(eng, lambda: e.tensor_copy(out, in_), [in_], [out])

    def memset(self, ap, val, eng="dve"):
        e = self._veng(eng)
        self.S.op(eng, lambda: e.memset(ap, val), [], [ap])

    def reduce(self, out, in_, op, eng="dve"):
        e = self._veng(eng)
        self.S.op(eng, lambda: e.tensor_reduce(out, in_, AX.X, op), [in_], [out])

    def recip(self, out, in_):
        nc = self.nc
        self.S.op("dve", lambda: nc.vector.reciprocal(out, in_), [in_], [out])

    def finish(self):
        self.S.final_wait(self.outs)
        st = self.S.finalize()
        return st


KEY_START = [0, 8, 24, 32]


def emit_mod(k, adaw, adab, ng_ap, cc, psmod_t):
    nc = k.nc
    cs = k.sb("cs", [128, 8, 2])
    k.dma(cs[:], cc)
    k.act(cs[:], cs[:], AF.Silu)
    adabs = k.sb("adabs", [128, 48])
    k.dma(adabs[:], adab)
    ngs = k.sb("ngs", [128, 2, 8])
    k.dma(ngs[:], ng_ap)
    psmod = psmod_t[:, 0:96].rearrange("p (a b) -> p a b", b=2)
    aw = [k.sb("aw%d" % i, [128, 8, 128]) for i in range(2)]
    adv = adaw.rearrange("(k p) n -> p k n", p=128)
    for j in range(48):
        t = aw[j % 2]
        k.dma(t[:], adv[:, :, j * 128:(j + 1) * 128], q=("sp" if j % 2 == 0 else "act"))
        for kk in range(8):
            k.mm(psmod[:, j, :], t[:, kk, :], cs[:, kk, :],
                 start=(kk == 0), stop=(kk == 7))
    modT = k.sb("modTs", [128, 48, 2])
    for col in range(2):
        k.tt(modT[:, :, col], psmod[:, :, col], adabs[:], ALU.add)
    AB = k.sb("AB", [128, 2, 8, 2])
    tmp = k.sb("modtmp", [128, 8])
    for which, (sc_i, g_i) in enumerate(((1, 0), (4, 1))):
        for col in range(2):
            k.ts(tmp[:], modT[:, sc_i * 8:(sc_i + 1) * 8, col], 1.0, ALU.add)
            k.tt(AB[:, which, :, col], tmp[:], ngs[:, g_i, :], ALU.mult)
    return modT, AB


class NormT:
    def __init__(self, k, idf, pst, tag="n"):
        self.k = k
        self.idf = idf
        self.xt = [k.sb("nx%s%d" % (tag, i), [128, 1024]) for i in range(2)]
        self.junk = k.sb("njunk" + tag, [128, 1024], BF)
        self.st = k.sb("nst" + tag, [128, 4])
        self.pst = pst
        self.i = 0

    def run(self, src_rows, ntok, dst_fn, A_fn, B_fn, keep_fn=None):
        k = self.k
        xt = self.xt[self.i % 2]
        self.i += 1
        n = ntok
        k.dma(xt[:n, :], src_rows, q="sp")
        st = self.st
        k.act(self.junk[:n, :], xt[:n, :], AF.Square, accum_out=st[:n, 0:1])
        k.ts(st[:n, 1:2], st[:n, 0:1], 1.0 / 1024.0, ALU.mult, 1e-6, ALU.add)
        k.act(st[:n, 2:3], st[:n, 1:2], AF.Sqrt)
        k.recip(st[:n, 3:4], st[:n, 2:3])
        k.ts(xt[:n, :], xt[:n, :], st[:n, 3:4], ALU.mult)
        for kk in range(8):
            k.tr(self.pst[:, kk * 128:kk * 128 + n], xt[:n, kk * 128:(kk + 1) * 128], self.idf[:n, :n])
        for kk in range(8):
            src = self.pst[:, kk * 128:kk * 128 + n]
            if kk < 4:
                k.ts(dst_fn(kk), src, A_fn(kk), ALU.mult, B_fn(kk), ALU.add)
            else:
                k.act(dst_fn(kk), src, AF.Identity, bias=B_fn(kk), scale=A_fn(kk))


def build_l0a(phase=9):
    k = KB()
    nc = k.nc
    xh = k.din("xh", [39 * 64, 1024])
    ctx = k.din("ctx", [256, 1024])
    cc = k.din("cc", [128, 8, 2])
    adaw = k.din("adaw", [1024, 6144])
    adab = k.din("adab", [128, 48])
    ng = k.din("ng", [128, 2, 8])
    win = k.din("win", [1024, 2560])
    rpbg = k.din("rpbg", [12, 4, 128, 480])
    mask = k.din("mask", [4, 4, 128, 480])
    identf = k.din("identf", [128, 128])
    c64b = k.din("c64b", [128, 128])
    s64b = k.din("s64b", [128, 128])
    c256 = k.din("c256", [256, 256])
    ns256 = k.din("ns256", [256, 256])
    aT = k.dout("aT", [256, 2048])
    oT = k.dout("oT", [768, 2048], BF)
    mcT = k.dout("mcT", [1024, 256])
    modT_o = k.dout("modT", [128, 48, 2])

    idf = k.sb("idf", [128, 128])
    idb = k.sb("idb", [128, 128], BF)
    k.dma(idf[:], identf)
    k.copy(idb[:], idf[:])

    T0 = k.ps("T0", [128, 1024])
    T1 = k.ps("T1", [128, 2, 512])
    T2 = k.ps("T2", [128, 1024])
    psA = [k.ps("psA%d" % i, [128, 512]) for i in range(2)]
    modT, AB = emit_mod(k, adaw, adab, ng, cc, psA[0])
    k.dma(modT_o, modT[:], q="sp")

    if phase < 1:
        return k, k.finish()
    winb = k.sb("winb", [128, 8, 2560], BF)
    wst = [k.sb("wst%d" % i, [128, 1280]) for i in range(2)]
    for kk in range(16):
        t = wst[kk % 2]
        hf = kk % 2
        k.dma(t[:], win[(kk // 2) * 128:(kk // 2 + 1) * 128, hf * 1280:(hf + 1) * 1280], q=("sp" if kk % 2 == 0 else "act"))
        if kk % 2 == 0:
            k.copy(winb[:, kk // 2, hf * 1280:(hf + 1) * 1280], t[:], eng="dve")
        else:
            k.copy(winb[:, kk // 2, hf * 1280:(hf + 1) * 1280], t[:], eng="act")

    if phase < 2:
        return k, k.finish()
    norm = NormT(k, idf, T0)
    psS = [T1]
    psPT = T2[:, 0:768].rearrange("p (a b) -> p a b", b=128)
    psO = T2[:, 768:896]
    cnt = {"a": 0}

    def nextA():
        cnt["a"] += 1
        return psA[cnt["a"] % 2]

    def evac(out, in_, i, scale=None):
        if scale is None:
            if i % 2 == 0:
                k.copy(out, in_, eng="dve")
            else:
                k.copy(out, in_, eng="act")
        else:
            if i % 2 == 0:
                k.ts(out, in_, scale, ALU.mult)
            else:
                k.act(out, in_, AF.Copy, scale=scale)

    Ssb = k.sb("Ssb", [128, 736])
    Pexp = k.sb("Pexp", [128, 736], BF)
    sst = k.sb("sst", [128, 4])
    Dg = k.sb("Dg", [128, 128], BF)
    PT = k.sb("PT", [128, 6, 128], BF)

    def attn_unit(q_ap, kloc_ap, nloc, maskb_ap, bias_ap, kctx_ap, vloc_fn, vctx_fn, h, out_ap):
        hp = 64 * (h % 2)
        S = psS[0]
        ntot = nloc + 256
        if nloc:
            k.mm(S[:, 0, 0:nloc].rearrange("p (a b) -> p a b", b=32), q_ap, kloc_ap, start=True, stop=False)
            k.mm(S[:, 0, 0:nloc], idb[:], maskb_ap, start=False, stop=True)
            k.tt(Ssb[:, 0:nloc], S[:, 0, 0:nloc], bias_ap, ALU.add)
        k.mm(S[:, 1, 0:256], q_ap, kctx_ap)
        k.copy(Ssb[:, nloc:ntot], S[:, 1, 0:256], eng="act")
        k.reduce(sst[:, 0:1], Ssb[:, 0:ntot], ALU.max)
        k.ts(sst[:, 1:2], sst[:, 0:1], -1.0, ALU.mult)
        k.act(Pexp[:, 0:ntot], Ssb[:, 0:ntot], AF.Exp, bias=sst[:, 1:2], accum_out=sst[:, 2:3])
        k.recip(sst[:, 3:4], sst[:, 2:3])
        k.ts(Dg[:], idf[:], sst[:, 3:4], ALU.mult)
        chunks = []
        off = 0
        while off < nloc:
            kn = min(128, nloc - off)
            chunks.append((off, kn, "l", len(chunks)))
            off += kn
        nl = len(chunks)
        chunks.append((nloc, 128, "c", 0))
        chunks.append((nloc + 128, 128, "c", 1))
        for ci, (o, kn, kind, idx) in enumerate(chunks):
            k.mm(psPT[:kn, ci, :], Pexp[:, o:o + kn], Dg[:])
        nch = len(chunks)
        if nch > 4:
            k.copy(PT[:, 0:3, :], psPT[:, 0:3, :], eng="dve")
            k.copy(PT[:96, 3, :], psPT[:96, 3, :], eng="dve")
            k.copy(PT[:, 4:nch, :], psPT[:, 4:nch, :], eng="act")
        else:
            k.copy(PT[:, 0:nch, :], psPT[:, 0:nch, :], eng="dve")
        for ci, (o, kn, kind, idx) in enumerate(chunks):
            v = vloc_fn(idx, kn) if kind == "l" else vctx_fn(idx)
            k.mm(psO[hp:hp + 64, :], v, PT[:kn, ci, :], start=(ci == 0), stop=(ci == nch - 1))
        if len(out_ap.shape) == 3:
            k.copy(out_ap, psO[hp:hp + 64, :].rearrange("p (a b) -> p a b", b=16), eng="act")
        else:
            k.copy(out_ap, psO[hp:hp + 64, :], eng="act")

    cmT = k.sb("cmT", [128, 8, 256], BF)
    for t in range(2):
        norm.run(ctx[t * 128:(t + 1) * 128, :], 128,
                 lambda kk, t=t: cmT[:, kk, t * 128:(t + 1) * 128],
                 lambda kk: AB[:, 0, kk, 1:2], lambda kk: modT[:, kk, 1:2])
    acT = k.sb("acT", [128, 2, 256])
    qcT = k.sb("qcT", [128, 6, 256], BF)
    kcT = k.sb("kcT", [128, 6, 256], BF)
    Vc = k.sb("Vc", [128, 2, 768], BF)
    for oc in range(14):
        p = nextA()
        for kk in range(8):
            k.mm(p[:, 0:256], winb[:, kk, oc * 128:(oc + 1) * 128], cmT[:, kk, :],
                 start=(kk == 0), stop=(kk == 7))
        if oc < 2:
            evac(acT[:, oc, :], p[:, 0:256], oc)
        elif oc < 8:
            evac(qcT[:, oc - 2, :], p[:, 0:256], oc, scale=0.125)
        else:
            evac(kcT[:, oc - 8, :], p[:, 0:256], oc)
    for t in range(2):
        for half in range(2):
            p = nextA()
            for kk in range(8):
                k.mm(p[:, 0:384], cmT[:, kk, t * 128:(t + 1) * 128],
                     winb[:, kk, 1792 + half * 384:1792 + (half + 1) * 384],
                     start=(kk == 0), stop=(kk == 7))
            evac(Vc[:, t, half * 384:(half + 1) * 384], p[:, 0:384], half)
    if phase < 3:
        return k, k.finish()
    cst = k.sb("cst", [128, 2, 128])
    k.dma(cst[:, 0, :], c64b)
    k.dma(cst[:, 1, :], s64b)
    c2s = k.sb("c2s", [128, 2, 2, 256])
    k.dma(c2s[:, 0, :, :], c256.rearrange("(t p) n -> p t n", p=128))
    k.dma(c2s[:, 1, :, :], ns256.rearrange("(t p) n -> p t n", p=128))
    aCS = k.sb("aCS", [128, 2, 2, 256])
    for which in range(2):
        for t in range(2):
            for cch in range(2):
                p = nextA()
                k.mm(p[:, 0:128], acT[:, cch, t * 128:(t + 1) * 128], cst[:, which, :])
                evac(aCS[:, which, t, cch * 128:(cch + 1) * 128], p[:, 0:128], cch)
    mcs = k.sb("mcs", [128, 8, 256])
    for cch in range(2):
        p = nextA()
        n = 0
        for which in range(2):
            for t in range(2):
                k.mm(p[:, 0:256], aCS[:, which, t, cch * 128:(cch + 1) * 128], c2s[:, which, t, :],
                     start=(n == 0), stop=(n == 3))
                n += 1
        evac(mcs[:, cch, :], p[:, 0:256], cch)
    if phase < 4:
        return k, k.finish()
    for t in range(2):
        for h in range(12):
            hp, hc = 64 * (h % 2), h // 2
            attn_unit(qcT[hp:hp + 64, hc, t * 128:(t + 1) * 128], None, 0, None, None,
                      kcT[hp:hp + 64, hc, :], None,
                      lambda idx, h=h: Vc[:, idx, h * 64:(h + 1) * 64], h,
                      mcs[hp:hp + 64, 2 + hc, t * 128:(t + 1) * 128])
    k.dma(mcT.rearrange("(k p) n -> p k n", p=128), mcs[:], q="sp")

    if phase < 5:
        return k, k.finish()
    xmT = k.sb("xmT", [128, 8, 15, 64], BF)
    xmK = k.sb("xmK", [128, 8, 512], BF)
    qT = k.sb("qT", [128, 6, 4, 8, 16], BF)
    kT = k.sb("kT", [128, 6, 15, 64], BF)
    Vt = k.sb("Vt", [128, 4, 768], BF)
    aTs = k.sb("aTs", [128, 2, 512])
    oTs = k.sb("oTs", [128, 6, 8, 64], BF)
    maskst = k.sb("maskst", [128, 480])
    maskb = k.sb("maskb", [128, 4, 480], BF)
    rb = [k.sb("rb%d" % i, [128, 480]) for i in range(3)]
    xmTf = xmT[:].rearrange("p k a b -> p k (a b)")
    kTf = kT[:].rearrange("p k a b -> p k (a b)")
    for rg in range(4 if phase > 5 else 1):
        R0 = rg * 8
        for ti in range(8):
            n = 128 if ti < 7 else 64
            norm.run(xh[R0 * 64 + ti * 128: R0 * 64 + ti * 128 + n, :], n,
                     lambda kk, ti=ti, n=n: xmTf[:, kk, ti * 128: ti * 128 + n],
                     lambda kk: AB[:, 0, kk, 0:1], lambda kk: modT[:, kk, 0:1])
        for j in range(4):
            k.dma(maskst[:], mask[rg, j], q="act")
            k.copy(maskb[:, j, :], maskst[:], eng="act")
        for oc in range(8):
            p = nextA()
            for kk in range(8):
                k.mm(p[:, :], winb[:, kk, oc * 128:(oc + 1) * 128], xmTf[:, kk, 256:768],
                     start=(kk == 0), stop=(kk == 7))
            if oc < 2:
                evac(aTs[:, oc, :], p[:, :], oc)
            else:
                pv = p[:, :].rearrange("p (r j c) -> p r j c", r=8, j=4)
                ov = qT[:, oc - 2, :, :, :].rearrange("p j r c -> p r j c")
                evac(ov, pv, oc, scale=0.125)
        k.dma(aT.rearrange("(k p) n -> p k n", p=128)[:, :, rg * 512:(rg + 1) * 512], aTs[:], q="sp")
        for oc in range(6):
            for half in range(2):
                p = nextA()
                for kk in range(8):
                    k.mm(p[:, 0:480], winb[:, kk, 1024 + oc * 128:1024 + (oc + 1) * 128],
                         xmTf[:, kk, half * 480:(half + 1) * 480], start=(kk == 0), stop=(kk == 7))
                evac(kTf[:, oc, half * 480:(half + 1) * 480], p[:, 0:480], half)
        u = 0
        for j in range(4):
            cs_ = KEY_START[j]
            k.copy(xmK[:, :, 0:480].rearrange("p k (a b) -> p k a b", b=32),
                   xmT[:, :, :, cs_:cs_ + 32], eng=("dve" if j % 2 == 0 else "act"))
            for rc in range(4):
                nk = 128 if rc < 3 else 96
                for half in range(2):
                    p = nextA()
                    for kk in range(8):
                        k.mm(p[:nk, 0:384], xmK[:, kk, rc * 128: rc * 128 + nk],
                             winb[:, kk, 1792 + half * 384:1792 + (half + 1) * 384],
                             start=(kk == 0), stop=(kk == 7))
                    evac(Vt[:nk, rc, half * 384:(half + 1) * 384], p[:nk, 0:384], half)
            for h in range(12):
                hp, hc = 64 * (h % 2), h // 2
                r = rb[u % 3]
                u += 1
                k.dma(r[:], rpbg[h, j], q="sp")
                attn_unit(qT[hp:hp + 64, hc, j, :, :].rearrange("p r c -> p (r c)"),
                          kT[hp:hp + 64, hc, :, cs_:cs_ + 32], 480, maskb[:, j, :], r[:],
                          kcT[hp:hp + 64, hc, :],
                          lambda idx, kn, h=h: Vt[:kn, idx, h * 64:(h + 1) * 64],
                          lambda idx, h=h: Vc[:, idx, h * 64:(h + 1) * 64], h,
                          oTs[hp:hp + 64, hc, :, j * 16:(j + 1) * 16])
        k.dma(oT.rearrange("(k p) (g n) -> p k g n", p=128, g=4)[:, :, rg, :],
              oTs[:].rearrange("p k a b -> p k (a b)"), q="sp")
    st = k.finish()
    return k, st


def build_l0b():
    k = KB()
    X = k.din("X", [128, 32, 128])
    cs1 = k.din("cs1", [128, 256])
    tw = k.din("tw", [128, 2, 128])
    cs2 = k.din("cs2", [128, 3, 128])
    U = k.dout("U", [128, 2, 32, 128])
    Xs = k.sb("Xs", [128, 32, 128])
    k.dma(Xs[:, 0:16, :], X[:, 0:16, :], q="sp")
    k.dma(Xs[:, 16:32, :], X[:, 16:32, :], q="act")
    c1 = k.sb("c1", [128, 256]); k.dma(c1[:], cs1)
    tws = k.sb("tws", [128, 2, 128]); k.dma(tws[:], tw)
    c2 = k.sb("c2", [128, 3, 128]); k.dma(c2[:], cs2)
    Bs = k.sb("Bs", [128, 2, 32, 128])
    t = [k.sb("dt%d" % i, [128, 128]) for i in range(4)]
    ps = [k.ps("dps%d" % i, [128, 512]) for i in range(4)]
    for ch in range(32):
        p = ps[ch % 2]
        k.mm(p[:, 0:256], Xs[:, ch, :], c1[:])
        Ar, Ai = p[:, 0:128], p[:, 128:256]
        k.tt(t[0][:], Ar, tws[:, 0, :], ALU.mult)
        k.tt(t[1][:], Ai, tws[:, 1, :], ALU.mult)
        k.tt(Bs[:, 0, ch, :], t[0][:], t[1][:], ALU.add)
        k.tt(t[2][:], Ai, tws[:, 0, :], ALU.mult)
        k.tt(t[3][:], Ar, tws[:, 1, :], ALU.mult)
        k.tt(Bs[:, 1, ch, :], t[2][:], t[3][:], ALU.subtract)
    Us = k.sb("Us", [128, 2, 32, 128])
    for blk in range(8):
        sl = slice(blk * 4, blk * 4 + 4)
        pr = ps[2]; pi = ps[3]
        br = Bs[:, 0, sl, :].rearrange("p a b -> p (a b)")
        bi = Bs[:, 1, sl, :].rearrange("p a b -> p (a b)")
        k.mm(pr[:], c2[:, 0, :], br, start=True, stop=False)
        k.mm(pr[:], c2[:, 1, :], bi, start=False, stop=True)
        k.mm(pi[:], c2[:, 0, :], bi, start=True, stop=False)
        k.mm(pi[:], c2[:, 2, :], br, start=False, stop=True)
        k.copy(Us[:, 0, sl, :].rearrange("p a b -> p (a b)"), pr[:], eng="dve")
        k.copy(Us[:, 1, sl, :].rearrange("p a b -> p (a b)"), pi[:], eng="act")
    k.dma(U[:, 0, :, :], Us[:, 0, :, :], q="sp")
    k.dma(U[:, 1, :, :], Us[:, 1, :, :], q="act")
    return k, k.finish()


def l0b_consts():
    n = np.arange(128)
    ang = 2 * np.pi * np.outer(n, n) / 128
    cs1 = np.concatenate([np.cos(ang), -np.sin(ang)], axis=1).astype(np.float32)
    ang2 = 2 * np.pi * np.outer(n, n) / 16384
    tw = np.stack([np.cos(ang2), np.sin(ang2)], axis=1).astype(np.float32)
    sc = 1.0 / 1024.0
    cs2 = np.stack([np.cos(ang) * sc, np.sin(ang) * sc, -np.sin(ang) * sc], axis=1).astype(np.float32)
    return dict(cs1=cs1, tw=tw, cs2=cs2)


def l0b_inputs(aT_full, core, consts):
    X = aT_full[32 * core:32 * core + 32].reshape(32, 128, 128).transpose(1, 0, 2)
    d = dict(consts)
    d["X"] = np.ascontiguousarray(X)
    return d


class Post:
    def __init__(self, k, NT, ncol_of, idf, modT, AB, T0, pbank, psR, hs, actT, wbuf, stg, Gb,
                 rw, rbb, w1, w3, w2, Gsrc):
        self.k = k
        self.NT = NT
        self.col_of = ncol_of
        self.idf, self.modT, self.AB = idf, modT, AB
        self.T0, self.pbank, self.psR = T0, pbank, psR
        self.hs, self.actT, self.wbuf, self.stg, self.Gb = hs, actT, wbuf, stg, Gb
        self.w1, self.w3, self.w2, self.Gsrc = w1, w3, w2, Gsrc
        self.xs = k.sb("p_xs", [128, 1024])
        self.junk = k.sb("p_junk", [128, 1024], BF)
        self.st = k.sb("p_st", [128, 4])
        self.xm2f = k.sb("p_xm2f", [128, 8, 128])
        self.gate = k.sb("p_gate", [128, NT, 16])
        self.rws = k.sb("p_rws", [128, 8, 16])
        k.dma(self.rws[:], rw.rearrange("(k p) n -> p k n", p=128))
        self.rbs = k.sb("p_rbs", [128, 16])
        k.dma(self.rbs[:], rbb)
        self.r = k.sb("p_r", [128, 8, 16])
        self.pr = k.sb("p_pr", [128, 4, 6])
        self.rs = k.sb("p_rs", [128, 8])
        self.hid = k.sb("p_hid", [128, 4, 512], BF)
        self.s1 = [k.sb("p_s1%d" % i, [128, 512]) for i in range(2)]

    def norm_router(self, ti):
        k = self.k
        col = self.col_of(ti)
        h = self.hs[:, ti, :]
        st = self.st
        k.act(self.junk[:], h, AF.Square, accum_out=st[:, 0:1])
        k.ts(st[:, 1:2], st[:, 0:1], 1.0 / 1024.0, ALU.mult, 1e-6, ALU.add)
        k.act(st[:, 2:3], st[:, 1:2], AF.Sqrt)
        k.recip(st[:, 3:4], st[:, 2:3])
        k.ts(self.xs[:], h, st[:, 3:4], ALU.mult)
        for kk in range(8):
            k.tr(self.T0[:, kk * 128:(kk + 1) * 128], self.xs[:, kk * 128:(kk + 1) * 128], self.idf[:])
        for kk in range(8):
            src = self.T0[:, kk * 128:(kk + 1) * 128]
            A = self.AB[:, 1, kk, col:col + 1]
            B = self.modT[:, 24 + kk, col:col + 1]
            if kk < 4:
                k.ts(self.xm2f[:, kk, :], src, A, ALU.mult, B, ALU.add)
            else:
                k.act(self.xm2f[:, kk, :], src, AF.Identity, bias=B, scale=A)
        k.copy(self.actT[:, :, ti * 128:(ti + 1) * 128], self.xm2f[:], eng=("dve" if ti % 2 == 0 else "act"))
        pR = self.psR[:, 0:16]
        for kk in range(8):
            k.mm(pR, self.xm2f[:, kk, :], self.rws[:, kk, :], start=(kk == 0), stop=(kk == 7))
        r = self.r
        sc, bi, mb, m1, tmp, sel = (r[:, i, :] for i in range(6))
        k.act(sc, pR, AF.Sigmoid)
        k.tt(bi, sc, self.rbs[:], ALU.add)
        b4 = r[:, 1, :].rearrange("p (g i) -> p g i", i=4)
        pr = self.pr
        k.tt(pr[:, :, 0:3], b4[:, :, 0:3], b4[:, :, 1:4], ALU.add)
        k.tt(pr[:, :, 3:5], b4[:, :, 0:2], b4[:, :, 2:4], ALU.add)
        k.tt(pr[:, :, 5:6], b4[:, :, 0:1], b4[:, :, 3:4], ALU.add)
        rs = self.rs
        k.reduce(rs[:, 0:4], pr[:], ALU.max)
        k.reduce(rs[:, 4:5], rs[:, 0:4], ALU.max)
        k.ts(rs[:, 0:4], rs[:, 0:4], rs[:, 4:5], ALU.is_ge)
        mb4 = r[:, 2, :].rearrange("p (g i) -> p g i", i=4)
        for i in range(4):
            k.ts(mb4[:, :, i], rs[:, 0:4], 1.0, ALU.subtract, 1.0e4, ALU.mult)
        k.tt(mb, mb, bi, ALU.add)
        k.reduce(rs[:, 5:6], mb, ALU.max)
        k.ts(m1, mb, rs[:, 5:6], ALU.is_ge)
        k.ts(tmp, m1, -1.0e4, ALU.mult)
        k.tt(tmp, tmp, mb, ALU.add)
        k.reduce(rs[:, 6:7], tmp, ALU.max)
        k.ts(sel, mb, rs[:, 6:7], ALU.is_ge)
        k.tt(sel, sel, sc, ALU.mult)
        k.reduce(rs[:, 7:8], sel, ALU.add)
        k.recip(rs[:, 7:8], rs[:, 7:8])
        k.ts(self.gate[:, ti, :], sel, rs[:, 7:8], ALU.mult)

    def moe(self, blocks):
        k = self.k
        wb = self.wbuf
        w1b = wb[:, 0:4096].rearrange("p (a b) -> p a b", b=512)
        w3b = wb[:, 4096:8192].rearrange("p (a b) -> p a b", b=512)
        w2b = [wb[:, 8192:12288].rearrange("p (a b) -> p a b", b=1024),
               wb[:, 12288:16384].rearrange("p (a b) -> p a b", b=1024)]
        ncols = sorted(set(b[2] for b in blocks))
        for c in ncols:
            k.dma(self.Gb[:, c, :], self.Gsrc[:, c, 1, :], q="act")
        pb = self.pbank
        si = 0
        for e in range(16):
            for wi, (wsrc, wdst) in enumerate(((self.w1, w1b), (self.w3, w3b))):
                for hf in range(2):
                    s_ = self.stg[si % 2]
                    si += 1
                    k.dma(s_[:], wsrc[e].rearrange("(k p) n -> p k n", p=128)[:, hf * 4:(hf + 1) * 4, :],
                          q=("sp" if si % 2 == 0 else "act"))
                    k.copy(wdst[:, hf * 4:(hf + 1) * 4, :], s_[:], eng=("dve" if si % 2 == 0 else "act"))
            for hf in range(2):
                s_ = self.stg[si % 2]
                si += 1
                k.dma(s_[:], self.w2[e].rearrange("(k p) n -> p k n", p=128)[:, :, hf * 512:(hf + 1) * 512],
                      q=("sp" if si % 2 == 0 else "act"))
                for c in ncols:
                    for fc in range(4):
                        k.tt(w2b[c][:, fc, hf * 512:(hf + 1) * 512], s_[:, fc, :],
                             self.Gb[:, c, hf * 512:(hf + 1) * 512], ALU.mult)
            for bi_, (tok0, ntok, col, tiles) in enumerate(blocks):
                for fc in range(4):
                    p1 = pb[(2 * fc) % 4]
                    p3 = pb[(2 * fc + 1) % 4]
                    for kk in range(8):
                        k.mm(p1[:, 0:ntok], w1b[:, kk, fc * 128:(fc + 1) * 128],
                             self.actT[:, kk, tok0:tok0 + ntok], start=(kk == 0), stop=(kk == 7))
                    for kk in range(8):
                        k.mm(p3[:, 0:ntok], w3b[:, kk, fc * 128:(fc + 1) * 128],
                             self.actT[:, kk, tok0:tok0 + ntok], start=(kk == 0), stop=(kk == 7))
                    s1 = self.s1[fc % 2]
                    k.act(s1[:, 0:ntok], p1[:, 0:ntok], AF.Silu)
                    k.tt(self.hid[:, fc, 0:ntok], s1[:, 0:ntok], p3[:, 0:ntok], ALU.mult)
                for tl, ti in enumerate(tiles):
                    for half in range(2):
                        pY = self.T0[:, half * 512:(half + 1) * 512]
                        for fc in range(4):
                            k.mm(pY, self.hid[:, fc, tl * 128:(tl + 1) * 128],
                                 w2b[col][:, fc, half * 512:(half + 1) * 512],
                                 start=(fc == 0), stop=(fc == 3))
                        hsl = self.hs[:, ti, half * 512:(half + 1) * 512]
                        k.stt(hsl, pY, self.gate[:, ti, e:e + 1], hsl, ALU.mult, ALU.add)


def build_l0c():
    k = KB()
    x = k.din("x", [2048, 1024])
    ctx = k.din("ctx", [256, 1024])
    UT = k.din("UT", [2, 256, 2048])
    oT = k.din("oT", [768, 2048], BF)
    mcT = k.din("mcT", [1024, 256])
    Gsrc = k.din("Gsrc", [128, 2, 2, 1024])
    modT_i = k.din("modT", [128, 48, 2])
    ng = k.din("ng", [128, 2, 8])
    wout = k.din("wout", [1024, 1024])
    c64b = k.din("c64b", [128, 128])
    s64b = k.din("s64b", [128, 128])
    identf = k.din("identf", [128, 128])
    rw = k.din("rw", [1024, 16])
    rbb = k.din("rbb", [128, 16])
    w1 = k.din("w1", [16, 1024, 512])
    w3 = k.din("w3", [16, 1024, 512])
    w2 = k.din("w2", [16, 512, 1024])
    h_o = k.dout("h", [2048, 1024])
    hc_o = k.dout("hc", [256, 1024])
    NT = 18

    idf = k.sb("idf", [128, 128]); k.dma(idf[:], identf)
    modT = k.sb("modTs", [128, 48, 2]); k.dma(modT[:], modT_i)
    ngs = k.sb("ngs", [128, 2, 8]); k.dma(ngs[:], ng)
    AB = k.sb("AB", [128, 2, 8, 2])
    tmp8 = k.sb("tmp8", [128, 8])
    for col in range(2):
        k.ts(tmp8[:], modT[:, 32:40, col], 1.0, ALU.add)
        k.tt(AB[:, 1, :, col], tmp8[:], ngs[:, 1, :], ALU.mult)
    T0 = k.ps("T0", [128, 1024])
    pbank = [k.ps("pb%d" % i, [128, 512]) for i in range(4)]
    psR = k.ps("psR", [128, 512])
    hs = k.sb("hs", [128, NT, 1024])
    actT = k.sb("actT", [128, 8, NT * 128], BF)
    wbuf = k.sb("wbuf", [128, 16384], BF)
    stg = [k.sb("stg%d" % i, [128, 4, 512]) for i in range(2)]
    Gb = k.sb("Gb", [128, 2, 1024])
    cst = k.sb("cst", [128, 2, 128])
    k.dma(cst[:, 0, :], c64b); k.dma(cst[:, 1, :], s64b)
    for c in range(2):
        k.dma(Gb[:, c, :], Gsrc[:, c, 0, :], q="act")
    k.dma(actT[:, 2:8, 0:2048], oT.rearrange("(k p) n -> p k n", p=128), q="sp")
    UTv = UT.rearrange("r (c p) n -> p r c n", p=128)
    for blk in range(4):
        s_ = stg[blk % 2]
        sv = s_[:].rearrange("p (r c) n -> p r c n", r=2)
        k.dma(sv, UTv[:, :, :, blk * 512:(blk + 1) * 512], q="sp")
        for cch in range(2):
            p = pbank[cch]
            k.mm(p[:], cst[:, 0, :], sv[:, 0, cch, :], start=True, stop=False)
            k.mm(p[:], cst[:, 1, :], sv[:, 1, cch, :], start=False, stop=True)
            k.copy(actT[:, cch, blk * 512:(blk + 1) * 512], p[:], eng=("dve" if cch == 0 else "act"))
    mcv = mcT.rearrange("(k p) n -> p k n", p=128)
    for hf in range(2):
        s_ = stg[hf % 2]
        k.dma(s_[:, :, 0:256], mcv[:, hf * 4:(hf + 1) * 4, :], q="act")
        k.copy(actT[:, hf * 4:(hf + 1) * 4, 2048:2304], s_[:, :, 0:256], eng="dve")
    woutb = wbuf[:, 0:8192].rearrange("p (a b) -> p a b", b=1024)
    wv = wout.rearrange("(k p) n -> p k n", p=128)
    for q4 in range(4):
        s_ = stg[q4 % 2]
        k.dma(s_[:].rearrange("p (a c) n -> p a (c n)", a=2), wv[:, q4 * 2:(q4 + 1) * 2, :], q="sp")
        k.copy(woutb[:, q4 * 2:(q4 + 1) * 2, :], s_[:].rearrange("p (a c) n -> p a (c n)", a=2),
               eng=("dve" if q4 % 2 == 0 else "act"))
    post = Post(k, NT, lambda ti: 0 if ti < 16 else 1, idf, modT, AB, T0, pbank, psR, hs, actT, wbuf, stg, Gb,
                rw, rbb, w1, w3, w2, Gsrc)
    xt = [k.sb("xt%d" % i, [128, 1024]) for i in range(2)]
    for ti in range(NT):
        col = 0 if ti < 16 else 1
        src = x[ti * 128:(ti + 1) * 128, :] if ti < 16 else ctx[(ti - 16) * 128:(ti - 15) * 128, :]
        t = xt[ti % 2]
        k.dma(t[:], src, q="sp")
        for half in range(2):
            pY = T0[:, half * 512:(half + 1) * 512]
            for kk in range(8):
                k.mm(pY, actT[:, kk, ti * 128:(ti + 1) * 128], woutb[:, kk, half * 512:(half + 1) * 512],
                     start=(kk == 0), stop=(kk == 7))
            hsl = hs[:, ti, half * 512:(half + 1) * 512]
            k.tt(hsl, pY, Gb[:, col, half * 512:(half + 1) * 512], ALU.mult)
            k.tt(hsl, hsl, t[:, half * 512:(half + 1) * 512], ALU.add)
        post.norm_router(ti)
    blocks = [(b * 512, 512, 0, [4 * b + i for i in range(4)]) for b in range(4)]
    blocks.append((2048, 256, 1, [16, 17]))
    post.moe(blocks)
    for ti in range(NT):
        dst = h_o[ti * 128:(ti + 1) * 128, :] if ti < 16 else hc_o[(ti - 16) * 128:(ti - 15) * 128, :]
        k.dma(dst, hs[:, ti, :], q=("sp" if ti % 2 == 0 else "act"))
    return k, k.finish()


def build_l1a(nchunks=130):
    k = KB()
    hseq = k.din("hseq", [16640, 1024])
    cc = k.din("cc", [128, 8, 2])
    adaw = k.din("adaw", [1024, 6144])
    adab = k.din("adab", [128, 48])
    ng = k.din("ng", [128, 2, 8])
    wq = k.din("wq", [1024, 256])
    wk = k.din("wk", [1024, 256])
    wv = k.din("wv", [1024, 512])
    dec = k.din("dec", [128, 1])
    iota1 = k.din("iota1", [128, 128])
    kpos = k.din("kpos", [128, 1])
    diffm = k.din("diffm", [128, 128])
    tri = k.din("tri", [128, 128])
    rowcs = k.din("rowcs", [128, 2, 256])
    colcs = k.din("colcs", [128, 2, 128])
    identf = k.din("identf", [128, 128])
    o = k.dout("o", [16384, 512])
    modT_o = k.dout("modT", [128, 48, 2])

    idf = k.sb("idf", [128, 128]); k.dma(idf[:], identf)
    idb = k.sb("idb", [128, 128], BF); k.copy(idb[:], idf[:])
    T0 = k.ps("T0", [128, 1024])
    pQ = k.ps("pQ", [128, 512])
    pK = k.ps("pK", [128, 512])
    pV = k.ps("pV", [128, 512])
    pO = k.ps("pO", [128, 512])
    pS = k.ps("pS", [128, 128])
    pT = k.ps("pT", [128, 256], BF)
    modT, AB = emit_mod(k, adaw, adab, ng, cc, pQ)
    k.dma(modT_o, modT[:], q="sp")

    stg = k.sb("stg", [128, 8, 512])
    wqb = k.sb("wqb", [128, 8, 256], BF)
    wqr = k.sb("wqr", [128, 8, 256], BF)
    wkb = k.sb("wkb", [128, 8, 256], BF)
    wkr = k.sb("wkr", [128, 8, 256], BF)
    wvb = k.sb("wvb", [128, 8, 512], BF)
    for src, dst, rot, sc in ((wq, wqb, wqr, 1.0), (wk, wkb, wkr, 0.0625)):
        k.dma(stg[:, :, 0:256], src.rearrange("(k p) n -> p k n", p=128), q="sp")
        k.ts(dst[:], stg[:, :, 0:256], sc, ALU.mult)
        for hf in range(2):
            b = hf * 128
            k.ts(rot[:, :, b:b + 64], stg[:, :, b + 64:b + 128], -sc, ALU.mult)
            k.ts(rot[:, :, b + 64:b + 128], stg[:, :, b:b + 64], sc, ALU.mult)
    k.dma(stg[:], wv.rearrange("(k p) n -> p k n", p=128), q="sp")
    k.copy(wvb[:], stg[:], eng="act")

    d = k.sb("dcy", [128, 8])
    k.dma(d[:, 0:1], dec)
    k.act(d[:, 1:2], d[:, 0:1], AF.Exp)
    k.ts(d[:, 2:3], d[:, 1:2], -1.0, ALU.mult, 1.0, ALU.add)
    k.act(d[:, 3:4], d[:, 2:3], AF.Ln)
    lg = d[:, 3:4]
    cst = k.sb("cst", [128, 4, 128])
    k.dma(cst[:, 0, :], iota1); k.dma(cst[:, 1, :], diffm); k.dma(cst[:, 2, :], tri)
    kps = k.sb("kps", [128, 1]); k.dma(kps[:], kpos)
    QD = k.sb("QD", [128, 128])
    k.act(QD[:], cst[:, 0, :], AF.Exp, scale=lg)
    DT = k.sb("DT", [128, 128])
    k.act(DT[:], cst[:, 1, :], AF.Exp, scale=lg)
    k.tt(DT[:], DT[:], cst[:, 2, :], ALU.mult)
    k.act(d[:, 4:5], kps[:], AF.Exp, scale=lg)
    KD = d[:, 4:5]
    k.ts(d[:, 5:6], lg, 128.0, ALU.mult)
    k.act(d[:, 6:7], d[:, 5:6], AF.Exp)
    CD = d[:, 6:7]
    rcs = k.sb("rcs", [128, 2, 256]); k.dma(rcs[:], rowcs)
    ccs = k.sb("ccs", [128, 2, 128]); k.dma(ccs[:], colcs)

    norm = NormT(k, idf, T0)
    xmT = [k.sb("xmT%d" % i, [128, 8, 128], BF) for i in range(2)]
    qTr = [k.sb("qTr%d" % i, [128, 2, 128], BF) for i in range(2)]
    kTr = [k.sb("kTr%d" % i, [128, 2, 128], BF) for i in range(2)]
    qd = [k.sb("qd%d" % i, [128, 2, 128], BF) for i in range(2)]
    tq = [k.sb("tq%d" % i, [128, 128]) for i in range(2)]
    vb = [k.sb("vb%d" % i, [128, 512], BF) for i in range(2)]
    kdec = [k.sb("kdec%d" % i, [128, 256], BF) for i in range(2)]
    sTd = k.sb("sTd", [128, 128], BF)
    ob = [k.sb("ob%d" % i, [128, 512]) for i in range(2)]
    Sf = k.sb("Sf", [128, 2, 512])
    Sb = k.sb("Sb", [128, 2, 512], BF)
    k.memset(Sf[:], 0.0)
    k.memset(Sb[:], 0.0)

    def rope(dst, P, gi0):
        for g in range(2):
            sl = slice(g * 64, (g + 1) * 64)
            gi = gi0 + g
            k.ts(tq[0][:, sl], P[:, 0:128][:, sl], rcs[:, 0, gi:gi + 1], ALU.mult)
            k.stt(dst[:, 0, sl], P[:, 256:384][:, sl], rcs[:, 1, gi:gi + 1], tq[0][:, sl], ALU.mult, ALU.add)
        k.tt(tq[0][:], P[:, 128:256], ccs[:, 0, :], ALU.mult)
        k.tt(tq[1][:], P[:, 384:512], ccs[:, 1, :], ALU.mult)
        k.tt(dst[:, 1, :], tq[0][:], tq[1][:], ALU.add)

    def proj(c):
        b = c % 2
        is_ctx = c < 2
        col = 1 if is_ctx else 0
        xm = xmT[b]
        norm.run(hseq[c * 128:(c + 1) * 128, :], 128, lambda kk, xm=xm: xm[:, kk, :],
                 lambda kk, col=col: AB[:, 0, kk, col:col + 1], lambda kk, col=col: modT[:, kk, col:col + 1])
        for P, wa, wr in ((pQ, wqb, wqr), (pK, wkb, wkr)):
            for part, w in enumerate((wa, wr)):
                if is_ctx and part == 1:
                    continue
                for oc in range(2):
                    out = P[:, part * 256 + oc * 128: part * 256 + (oc + 1) * 128]
                    for kk in range(8):
                        k.mm(out, w[:, kk, oc * 128:(oc + 1) * 128], xm[:, kk, :], start=(kk == 0), stop=(kk == 7))
        for kk in range(8):
            k.mm(pV[:], xm[:, kk, :], wvb[:, kk, :], start=(kk == 0), stop=(kk == 7))
        if is_ctx:
            k.copy(qTr[b][:].rearrange("p a b -> p (a b)"), pQ[:, 0:256], eng="dve")
            k.copy(kTr[b][:].rearrange("p a b -> p (a b)"), pK[:, 0:256], eng="dve")
        else:
            gi0 = 2 * (c - 2)
            rope(qTr[b], pQ, gi0)
            rope(kTr[b], pK, gi0)
        k.copy(vb[b][:], pV[:], eng="act")
        for dc in range(2):
            k.tr(pT[:, dc * 128:(dc + 1) * 128], kTr[b][:, dc, :], idb[:])
        k.ts(kdec[b][:], pT[:], KD, ALU.mult)
        if not is_ctx:
            for dc in range(2):
                k.tt(qd[b][:, dc, :], qTr[b][:, dc, :], QD[:], ALU.mult)

    def scan(c):
        b = c % 2
        is_ctx = c < 2
        if not is_ctx:
            for dc in range(2):
                k.mm(pS[:], kTr[b][:, dc, :], qTr[b][:, dc, :], start=(dc == 0), stop=(dc == 1))
            k.tt(sTd[:], pS[:], DT[:], ALU.mult)
            k.mm(pO[:], sTd[:], vb[b][:], start=True, stop=False)
            for dc in range(2):
                k.mm(pO[:], qd[b][:, dc, :], Sb[:, dc, :], start=False, stop=(dc == 1))
            obt = ob[c % 2]
            k.copy(obt[:], pO[:], eng="act")
            k.dma(o[(c - 2) * 128:(c - 1) * 128, :], obt[:], q="act")
        for dc in range(2):
            k.mm(pO[:], kdec[b][:, dc * 128:(dc + 1) * 128], vb[b][:], start=True, stop=True)
            k.stt(Sf[:, dc, :], Sf[:, dc, :], CD, pO[:], ALU.mult, ALU.add)
            k.copy(Sb[:, dc, :], Sf[:, dc, :], eng="act")

    proj(0)
    for c in range(nchunks):
        if c + 1 < nchunks:
            proj(c + 1)
        scan(c)
    return k, k.finish()


def rope_tables(rows, cols):
    p = np.arange(128)
    inv = 10000.0 ** (-(p % 64).astype(np.float64) / 64.0)
    ar = inv[:, None] * np.asarray(rows, np.float64)[None, :]
    ac = inv[:, None] * np.asarray(cols, np.float64)[None, :]
    rowcs = np.stack([np.cos(ar), np.sin(ar)], axis=1).astype(np.float32)
    colcs = np.stack([np.cos(ac), np.sin(ac)], axis=1).astype(np.float32)
    return rowcs, colcs


def l1a_inputs(inp, core, h_all, hc):
    hd, dr = core % 4, core // 4
    if dr == 0:
        seq = np.concatenate([hc, h_all], axis=0)
        rows = np.arange(256)
        cols = np.concatenate([np.arange(64), np.arange(64)])
    else:
        seq = np.concatenate([hc[::-1], h_all[::-1]], axis=0)
        rows = np.arange(256)[::-1]
        cols = np.concatenate([np.arange(64)[::-1], np.arange(64)[::-1]])
    rowcs, colcs = rope_tables(rows, cols)
    w = inp['ret_w_in'][0]
    n = np.arange(128)
    cc = np.stack([lay_vec(inp['c'][0]), lay_vec(inp['c_ctx'])], axis=-1)
    ng = np.stack([lay_vec(inp['norm_g'][1, 0]), lay_vec(inp['norm_g'][1, 1])], axis=1)
    diff = (n[None, :] - n[:, None]).astype(np.float32)
    return dict(hseq=np.ascontiguousarray(seq), cc=np.ascontiguousarray(cc), adaw=np.ascontiguousarray(inp['ada_w'][1]),
                adab=lay_vec(inp['ada_b'][1]), ng=np.ascontiguousarray(ng),
                wq=np.ascontiguousarray(w[:, hd * 256:(hd + 1) * 256]),
                wk=np.ascontiguousarray(w[:, 1024 + hd * 256:1024 + (hd + 1) * 256]),
                wv=np.ascontiguousarray(w[:, 2048 + hd * 512:2048 + (hd + 1) * 512]),
                dec=np.full((128, 1), inp['ret_decay'][0, dr, hd], np.float32),
                iota1=np.ascontiguousarray(np.broadcast_to((n + 1).astype(np.float32)[None, :], (128, 128))),
                kpos=(127 - n).astype(np.float32).reshape(128, 1),
                diffm=np.maximum(diff, 0.0), tri=(diff >= 0).astype(np.float32),
                rowcs=rowcs, colcs=colcs, identf=np.eye(128, dtype=np.float32))


def build_l1b():
    k = KB()
    h0 = k.din("h0", [2048, 1024])
    of = k.din("of", [2048, 2048])
    obk = k.din("ob", [2048, 2048])
    modT_i = k.din("modT", [128, 48, 2])
    ng = k.din("ng", [128, 2, 8])
    wg = k.din("wg", [1024, 2048])
    wo = k.din("wo", [2048, 1024])
    Gsrc = k.din("Gsrc", [128, 2, 2, 1024])
    identf = k.din("identf", [128, 128])
    hmid = k.dout("hmid", [2048, 1024])
    idf = k.sb("idf", [128, 128]); k.dma(idf[:], identf)
    idb = k.sb("idb", [128, 128], BF); k.copy(idb[:], idf[:])
    modT = k.sb("modTs", [128, 48, 2]); k.dma(modT[:], modT_i)
    ngs = k.sb("ngs", [128, 2, 8]); k.dma(ngs[:], ng)
    AB = k.sb("AB", [128, 2, 8, 2])
    tmp8 = k.sb("tmp8", [128, 8])
    k.ts(tmp8[:], modT[:, 8:16, 0], 1.0, ALU.add)
    k.tt(AB[:, 0, :, 0], tmp8[:], ngs[:, 0, :], ALU.mult)
    Gb = k.sb("Gb", [128, 1024]); k.dma(Gb[:], Gsrc[:, 0, 0, :], q="act")
    T0 = k.ps("T0", [128, 1024])
    pG = [k.ps("pG%d" % i, [128, 512]) for i in range(4)]
    pTr = k.ps("pTr", [128, 2048], BF)
    stg = k.sb("stg", [128, 8, 512])
    wgb = k.sb("wgb", [128, 8, 2048], BF)
    wob = k.sb("wob", [128, 16, 1024], BF)
    wgv = wg.rearrange("(k p) n -> p k n", p=128)
    for b in range(4):
        k.dma(stg[:], wgv[:, :, b * 512:(b + 1) * 512], q="sp")
        k.copy(wgb[:, :, b * 512:(b + 1) * 512], stg[:], eng=("dve" if b % 2 == 0 else "act"))
    wov = wo.rearrange("(c p) n -> p c n", p=128)
    sv = stg[:].rearrange("p (a c) n -> p a (c n)", a=4)
    for b in range(4):
        k.dma(sv, wov[:, b * 4:(b + 1) * 4, :], q="sp")
        k.copy(wob[:, b * 4:(b + 1) * 4, :], sv, eng=("dve" if b % 2 == 0 else "act"))
    norm = NormT(k, idf, T0)
    xmT = k.sb("xmT", [128, 8, 128], BF)
    oft = [k.sb("oft%d" % i, [128, 2048]) for i in range(2)]
    obt = [k.sb("obt%d" % i, [128, 2048]) for i in range(2)]
    sg = k.sb("sg", [128, 2048])
    gated = k.sb("gated", [128, 2048], BF)
    gT = k.sb("gT", [128, 16, 128], BF)
    junk = k.sb("junk2", [128, 512], BF)
    st = k.sb("st2", [128, 16])
    xt = [k.sb("xres%d" % i, [128, 1024]) for i in range(2)]
    hm = [k.sb("hm%d" % i, [128, 1024]) for i in range(2)]
    for ti in range(16):
        rows = slice(ti * 128, (ti + 1) * 128)
        norm.run(h0[rows, :], 128, lambda kk: xmT[:, kk, :],
                 lambda kk: AB[:, 0, kk, 0:1], lambda kk: modT[:, kk, 0:1])
        a, b = oft[ti % 2], obt[ti % 2]
        k.dma(a[:], of[rows, :], q="sp")
        k.dma(b[:], obk[rows, :], q="act")
        x = xt[ti % 2]
        k.dma(x[:], h0[rows, :], q="sp")
        for blk in range(4):
            for kk in range(8):
                k.mm(pG[blk][:], xmT[:, kk, :], wgb[:, kk, blk * 512:(blk + 1) * 512],
                     start=(kk == 0), stop=(kk == 7))
            k.act(sg[:, blk * 512:(blk + 1) * 512], pG[blk][:], AF.Silu)
        k.tt(a[:], a[:], b[:], ALU.add)
        for hd in range(4):
            hs_ = slice(hd * 512, (hd + 1) * 512)
            k.act(junk[:], a[:, hs_], AF.Square, accum_out=st[:, hd:hd + 1])
        k.ts(st[:, 4:8], st[:, 0:4], 1.0 / 512.0, ALU.mult, 1e-6, ALU.add)
        k.act(st[:, 8:12], st[:, 4:8], AF.Sqrt)
        k.recip(st[:, 12:16], st[:, 8:12])
        for hd in range(4):
            hs_ = slice(hd * 512, (hd + 1) * 512)
            k.stt(gated[:, hs_], a[:, hs_], st[:, 12 + hd:13 + hd], sg[:, hs_], ALU.mult, ALU.mult)
        for c in range(16):
            k.tr(pTr[:, c * 128:(c + 1) * 128], gated[:, c * 128:(c + 1) * 128], idb[:])
        gTf = gT[:].rearrange("p a b -> p (a b)")
        k.copy(gTf[:, 0:1024], pTr[:, 0:1024], eng="dve")
        k.copy(gTf[:, 1024:2048], pTr[:, 1024:2048], eng="act")
        h = hm[ti % 2]
        for half in range(2):
            pY = T0[:, half * 512:(half + 1) * 512]
            for c in range(16):
                k.mm(pY, gT[:, c, :], wob[:, c, half * 512:(half + 1) * 512], start=(c == 0), stop=(c == 15))
            hsl = h[:, half * 512:(half + 1) * 512]
            k.tt(hsl, pY, Gb[:, half * 512:(half + 1) * 512], ALU.mult)
            k.tt(hsl, hsl, x[:, half * 512:(half + 1) * 512], ALU.add)
        k.dma(hmid[rows, :], h[:], q="act")
    return k, k.finish()


def build_l1c():
    k = KB()
    hmid = k.din("hmid", [2048, 1024])
    modT_i = k.din("modT", [128, 48, 2])
    ng = k.din("ng", [128, 2, 8])
    Gsrc = k.din("Gsrc", [128, 2, 2, 1024])
    identf = k.din("identf", [128, 128])
    rw = k.din("rw", [1024, 16])
    rbb = k.din("rbb", [128, 16])
    w1 = k.din("w1", [16, 1024, 512])
    w3 = k.din("w3", [16, 1024, 512])
    w2 = k.din("w2", [16, 512, 1024])
    fgb = k.din("fgb", [128, 1024])
    out = k.dout("out", [2048, 1024])
    NT = 16
    idf = k.sb("idf", [128, 128]); k.dma(idf[:], identf)
    modT = k.sb("modTs", [128, 48, 2]); k.dma(modT[:], modT_i)
    ngs = k.sb("ngs", [128, 2, 8]); k.dma(ngs[:], ng)
    AB = k.sb("AB", [128, 2, 8, 2])
    tmp8 = k.sb("tmp8", [128, 8])
    k.ts(tmp8[:], modT[:, 32:40, 0], 1.0, ALU.add)
    k.tt(AB[:, 1, :, 0], tmp8[:], ngs[:, 1, :], ALU.mult)
    T0 = k.ps("T0", [128, 1024])
    pbank = [k.ps("pb%d" % i, [128, 512]) for i in range(4)]
    psR = k.ps("psR", [128, 512])
    hs = k.sb("hs", [128, NT, 1024])
    actT = k.sb("actT", [128, 8, NT * 128], BF)
    wbuf = k.sb("wbuf", [128, 16384], BF)
    stg = [k.sb("stg%d" % i, [128, 4, 512]) for i in range(2)]
    Gb = k.sb("Gb", [128, 2, 1024])
    fgs = k.sb("fgs", [128, 1024]); k.dma(fgs[:], fgb, q="act")
    post = Post(k, NT, lambda ti: 0, idf, modT, AB, T0, pbank, psR, hs, actT, wbuf, stg, Gb,
                rw, rbb, w1, w3, w2, Gsrc)
    for ti in range(NT):
        k.dma(hs[:, ti, :], hmid[ti * 128:(ti + 1) * 128, :], q=("sp" if ti % 2 == 0 else "act"))
        post.norm_router(ti)
    post.moe([(b * 512, 512, 0, [4 * b + i for i in range(4)]) for b in range(4)])
    st = k.sb("fst", [128, 4])
    ot = [k.sb("fot%d" % i, [128, 1024]) for i in range(2)]
    for ti in range(NT):
        h = hs[:, ti, :]
        k.act(post.junk[:], h, AF.Square, accum_out=st[:, 0:1])
        k.ts(st[:, 1:2], st[:, 0:1], 1.0 / 1024.0, ALU.mult, 1e-6, ALU.add)
        k.act(st[:, 2:3], st[:, 1:2], AF.Sqrt)
        k.recip(st[:, 3:4], st[:, 2:3])
        o_ = ot[ti % 2]
        k.stt(o_[:], h, st[:, 3:4], fgs[:], ALU.mult, ALU.mult)
        k.dma(out[ti * 128:(ti + 1) * 128, :], o_[:], q=("sp" if ti % 2 == 0 else "act"))
    return k, k.finish()


GRID_W = 64; NA_KW = 16; NA_KEYW = 32; NA_NCB = 4; NA_KH = 8

def na_static():
    j = np.arange(NA_NCB)
    key_start = np.clip(j * NA_KW - NA_KW // 2, 0, GRID_W - NA_KEYW)
    key_cols = key_start[:, None] + np.arange(NA_KEYW)[None, :]
    q_cols = j[:, None] * NA_KW + np.arange(NA_KW)[None, :]
    win_start = np.clip(q_cols - NA_KW // 2, 0, GRID_W - NA_KW)[:, :, None]
    kc = key_cols[:, None, :]
    col_mask = (kc >= win_start) & (kc < win_start + NA_KW)
    dc_idx = np.clip(kc - q_cols[:, :, None] + NA_KW - 1, 0, 2 * NA_KW - 2)
    return key_cols, col_mask, dc_idx

def lay_vec(v):
    v = np.asarray(v, np.float32).reshape(-1, 128)
    return np.ascontiguousarray(v.T)

def l0a_inputs(inp, core):
    x = inp['x'][0]
    rows = x.reshape(256, 64, 1024)
    xh = np.zeros((39, 64, 1024), np.float32)
    r0 = core * 32 - 4
    lo, hi = max(r0, 0), min(r0 + 39, 256)
    xh[lo - r0: hi - r0] = rows[lo:hi]
    cc = np.stack([lay_vec(inp['c'][0]), lay_vec(inp['c_ctx'])], axis=-1)
    ng = np.stack([lay_vec(inp['norm_g'][0, 0]), lay_vec(inp['norm_g'][0, 1])], axis=1)
    _, col_mask, dc_idx = na_static()
    rpb = inp['na_rpb'][0]
    qr = np.arange(8)[:, None, None, None]; kr = np.arange(15)[None, None, :, None]
    dr = np.clip(kr - qr + 3, 0, 14)
    rpbg = np.empty((12, 4, 128, 480), np.float32)
    mask = np.empty((4, 4, 128, 480), np.float32)
    for j in range(4):
        dc = dc_idx[j][None, :, None, :]
        drb = np.broadcast_to(dr, (8, 16, 15, 32)); dcb = np.broadcast_to(dc, (8, 16, 15, 32))
        rpbg[:, j] = rpb[:, drb, dcb].reshape(12, 128, 480)
        cm = np.broadcast_to(col_mask[j][None, :, None, :], (8, 16, 15, 32))
        for rg in range(4):
            r = core * 32 + rg * 8 + np.arange(8)
            rs = np.clip(r - 4, 0, 248)
            keyrow = core * 32 + rg * 8 - 4 + np.arange(15)
            rm = (keyrow[None, :] >= rs[:, None]) & (keyrow[None, :] < rs[:, None] + 8)
            valid = cm & rm[:, None, :, None]
            mask[rg, j] = np.where(valid, 0.0, -30000.0).reshape(128, 480).astype(np.float32)
    a = np.arange(64)
    C64 = np.cos(2 * np.pi * np.outer(a, a) / 64); S64 = np.sin(2 * np.pi * np.outer(a, a) / 64)
    c64b = np.kron(np.eye(2), C64).astype(np.float32); s64b = np.kron(np.eye(2), S64).astype(np.float32)
    n = np.arange(256)
    c256 = (np.cos(2 * np.pi * np.outer(n, n) / 256) / 128).astype(np.float32)
    ns256 = (-np.sin(2 * np.pi * np.outer(n, n) / 256) / 128).astype(np.float32)
    return dict(xh=xh.reshape(39 * 64, 1024), ctx=np.ascontiguousarray(inp['ctx'][0]), cc=np.ascontiguousarray(cc),
                adaw=np.ascontiguousarray(inp['ada_w'][0]), adab=lay_vec(inp['ada_b'][0]), ng=np.ascontiguousarray(ng),
                win=np.ascontiguousarray(inp['mixab_w_in'][0]), rpbg=rpbg, mask=mask,
                identf=np.eye(128, dtype=np.float32), c64b=c64b, s64b=s64b, c256=c256, ns256=ns256)

def gsrc_from_modT(modT):
    rows = np.empty((2, 2, 1024), np.float32)
    for col in range(2):
        for gi, which in enumerate((2, 5)):
            rows[col, gi] = modT[:, which * 8:(which + 1) * 8, col].T.reshape(-1)
    return np.ascontiguousarray(np.broadcast_to(rows[None], (128, 2, 2, 1024)))

def l0c_inputs(inp, core, UT, oT, mcT, modT, layer=0):
    a = np.arange(64)
    C64 = np.cos(2 * np.pi * np.outer(a, a) / 64); S64 = np.sin(2 * np.pi * np.outer(a, a) / 64)
    ng = np.stack([lay_vec(inp['norm_g'][layer, 0]), lay_vec(inp['norm_g'][layer, 1])], axis=1)
    return dict(x=np.ascontiguousarray(inp['x'][0, core * 2048:(core + 1) * 2048]), ctx=np.ascontiguousarray(inp['ctx'][0]),
                UT=np.ascontiguousarray(UT), oT=oT, mcT=mcT, Gsrc=gsrc_from_modT(modT), modT=modT, ng=np.ascontiguousarray(ng),
                wout=np.ascontiguousarray(inp['mixab_w_out'][0]),
                c64b=np.kron(np.eye(2), C64).astype(np.float32), s64b=np.kron(np.eye(2), S64).astype(np.float32),
                identf=np.eye(128, dtype=np.float32), rw=np.ascontiguousarray(inp['router_w']),
                rbb=np.ascontiguousarray(np.broadcast_to(inp['router_b'][None, :], (128, 16))),
                w1=np.ascontiguousarray(inp['moe_w1'][layer]), w3=np.ascontiguousarray(inp['moe_w3'][layer]),
                w2=np.ascontiguousarray(inp['moe_w2'][layer]))


def _run(kb, ins):
    return run_bass_kernel_spmd(kb.nc, ins, core_ids=list(range(8))).results


def kernel(**inputs):
    inp = {k_: np.asarray(v) for k_, v in inputs.items()}
    NC = 8
    kb, _ = build_l0a()
    r = _run(kb, [l0a_inputs(inp, c) for c in range(NC)])
    aT = np.concatenate([np.asarray(r[c]["aT"]) for c in range(NC)], axis=1)
    oT = [np.asarray(r[c]["oT"]) for c in range(NC)]
    mcT = np.asarray(r[0]["mcT"])
    modT0 = np.asarray(r[0]["modT"])
    del r
    kb, _ = build_l0b()
    cst = l0b_consts()
    r = _run(kb, [l0b_inputs(aT, c, cst) for c in range(NC)])
    U = np.concatenate([np.asarray(r[c]["U"]).transpose(1, 2, 0, 3).reshape(2, 32, 16384) for c in range(NC)], axis=1)
    del r
    kb, _ = build_l0c()
    r = _run(kb, [l0c_inputs(inp, c, U[:, :, c * 2048:(c + 1) * 2048], oT[c], mcT, modT0) for c in range(NC)])
    h_all = np.concatenate([np.asarray(r[c]["h"]) for c in range(NC)], axis=0)
    hc = np.asarray(r[0]["hc"])
    del r, U, oT
    kb, _ = build_l1a()
    r = _run(kb, [l1a_inputs(inp, c, h_all, hc) for c in range(NC)])
    modT1 = np.asarray(r[0]["modT"])
    of = np.concatenate([np.asarray(r[c]["o"]) for c in range(4)], axis=1)
    ob = np.concatenate([np.asarray(r[c]["o"])[::-1] for c in range(4, 8)], axis=1)
    del r
    ng1 = np.ascontiguousarray(np.stack([lay_vec(inp['norm_g'][1, 0]), lay_vec(inp['norm_g'][1, 1])], axis=1))
    G1 = gsrc_from_modT(modT1)
    idn = np.eye(128, dtype=np.float32)
    kb, _ = build_l1b()
    wg = np.ascontiguousarray(inp['ret_w_in'][0][:, 4096:6144])
    wo = np.ascontiguousarray(inp['ret_w_out'][0])
    r = _run(kb, [dict(h0=np.ascontiguousarray(h_all[c * 2048:(c + 1) * 2048]),
                       of=np.ascontiguousarray(of[c * 2048:(c + 1) * 2048]),
                       ob=np.ascontiguousarray(ob[c * 2048:(c + 1) * 2048]),
                       modT=modT1, ng=ng1, wg=wg, wo=wo, Gsrc=G1, identf=idn) for c in range(NC)])
    hmid = [np.asarray(r[c]["hmid"]) for c in range(NC)]
    del r, of, ob
    kb, _ = build_l1c()
    common = dict(modT=modT1, ng=ng1, Gsrc=G1, identf=idn, rw=np.ascontiguousarray(inp['router_w']),
                  rbb=np.ascontiguousarray(np.broadcast_to(inp['router_b'][None, :], (128, 16))),
                  w1=np.ascontiguousarray(inp['moe_w1'][1]), w3=np.ascontiguousarray(inp['moe_w3'][1]),
                  w2=np.ascontiguousarray(inp['moe_w2'][1]),
                  fgb=np.ascontiguousarray(np.broadcast_to(inp['final_norm_g'][None, :], (128, 1024))))
    r = _run(kb, [dict(common, hmid=hmid[c]) for c in range(NC)])
    out = np.concatenate([np.asarray(r[c]["out"]) for c in range(NC)], axis=0)
    return out.reshape(1, 16384, 1024).astype(np.float32, copy=False)
```

```python
import numpy as np
import concourse.bass as bass
import concourse.mybir as mybir
from concourse.bass_utils import run_bass_kernel_spmd

F32 = mybir.dt.float32
BF = mybir.dt.bfloat16
AF = mybir.ActivationFunctionType
ALU = mybir.AluOpType
AX = mybir.AxisListType


ENGS = ("pe", "act", "dve", "pool", "sp")


def _region(ap):
    t = ap.tensor
    shape = list(t.shape)
    space = str(ap.space) if hasattr(ap, "space") else ""
    off = int(ap.offset)
    dims = [(int(s), int(c)) for s, c in ap.ap]
    is_dram = "DRam" in type(t).__name__
    if is_dram:
        lo = off
        hi = off + sum((c - 1) * abs(s) for s, c in dims) + 1
        return (t.name, 0, 1, lo, hi)
    F = 1
    for s in shape[1:]:
        F *= int(s)
    p0 = off // F
    f0 = off % F
    p1 = p0
    f1 = f0
    for s, c in dims:
        if c <= 1:
            continue
        if s != 0 and s % F == 0:
            p1 += (c - 1) * (s // F)
        else:
            f1 += (c - 1) * abs(s)
    if "PSum" in type(t).__name__:
        f0 = (f0 // 512) * 512
        f1 = ((f1 // 512) + 1) * 512 - 1
        return (t.name, 0, 128, f0, f1 + 1)
    return (t.name, p0, p1 + 1, f0, f1 + 1)


def _overlap(a, b):
    return a[1] < b[2] and b[1] < a[2] and a[3] < b[4] and b[3] < a[4]


def _covers(a, b):
    return a[1] <= b[1] and a[2] >= b[2] and a[3] <= b[3] and a[4] >= b[4]


class Sched:
    def __init__(self, nc, n_dma_sems=40, same_engine_sync=True):
        self.nc = nc
        self.eng = {"pe": nc.tensor, "act": nc.scalar, "dve": nc.vector,
                    "pool": nc.gpsimd, "sp": nc.sync}
        self.ops = []
        self.same_engine_sync = same_engine_sync
        self.n_dma_sems = n_dma_sems
        self.sem = {e: nc.alloc_semaphore(name="sem_" + e) for e in ENGS}
        self.dsem = [nc.alloc_semaphore(name="dsem%d" % i) for i in range(n_dma_sems)]
        self._dma_rr = 0

    def op(self, eng, fn, reads=(), writes=(), unordered_same=False):
        self.ops.append(dict(eng=eng, fn=fn, r=[_region(a) for a in reads],
                             w=[_region(a) for a in writes], dma=False,
                             relax=unordered_same,
                             xr=[_region(a) for a in reads if "PSum" in type(a.tensor).__name__]))

    def dma(self, q, out, in_, **kw):
        slot = self._dma_rr
        self._dma_rr = (self._dma_rr + 1) % self.n_dma_sems
        e = self.eng[q]
        self.ops.append(dict(eng=q, fn=lambda: e.dma_start(out=out, in_=in_, **kw),
                             r=[_region(in_)], w=[_region(out)], dma=True, slot=slot,
                             relax=False))

    def finalize(self):
        ops = self.ops
        n = len(ops)
        pos_of = [0] * n
        eng_cnt = {e: 0 for e in ENGS}
        eng_ops = {e: [] for e in ENGS}
        for i, o in enumerate(ops):
            eng_cnt[o["eng"]] += 1
            pos_of[i] = eng_cnt[o["eng"]]
            eng_ops[o["eng"]].append(i)
        writes = {}
        reads = {}
        known = {e: {x: 0 for x in ENGS} for e in ENGS}
        known_d = {e: {} for e in ENGS}
        vc = [None] * n
        vcd = [None] * n
        waits = [None] * n
        signal = [False] * n
        slot_last = {}
        dma_target = {}
        slot_cnt = {}
        for i, o in enumerate(ops):
            E = o["eng"]
            deps = set()
            for R in o["r"]:
                for (W, j) in writes.get(R[0], ()):
                    if _overlap(W, R):
                        deps.add(j)
            for Wn in o["w"]:
                for (W, j) in writes.get(Wn[0], ()):
                    if _overlap(W, Wn):
                        deps.add(j)
                for (R, j) in reads.get(Wn[0], ()):
                    if _overlap(R, Wn):
                        deps.add(j)
            for R in o.get("xr", ()):
                for (R2, j) in reads.get(R[0], ()):
                    if ops[j]["eng"] != E and _overlap(R2, R):
                        deps.add(j)
            if o["dma"]:
                s = o["slot"]
                if s in slot_last:
                    deps.add(slot_last[s])
                slot_last[s] = i
                slot_cnt[s] = slot_cnt.get(s, 0) + 1
                dma_target[i] = (s, 16 * slot_cnt[s])
            deps.discard(i)
            kn = known[E]
            kd = known_d[E]
            need_e = {}
            need_d = []
            for j in deps:
                oj = ops[j]
                if oj["dma"]:
                    if kd.get(j, False):
                        continue
                    need_d.append(j)
                else:
                    Ej = oj["eng"]
                    if Ej == E and not o["dma"]:
                        if (not self.same_engine_sync) or E == "pe" or o["relax"]:
                            continue
                    if kn[Ej] >= pos_of[j]:
                        continue
                    if need_e.get(Ej, (0, -1))[0] < pos_of[j]:
                        need_e[Ej] = (pos_of[j], j)
            w_list = []
            for Ej, (p, j) in need_e.items():
                w_list.append(("e", Ej, j))
                signal[j] = True
            for j in need_d:
                w_list.append(("d", None, j))
            waits[i] = w_list
            for kind, Ej, j in w_list:
                for x in ENGS:
                    if vc[j][x] > kn[x]:
                        kn[x] = vc[j][x]
                for dj in vcd[j]:
                    kd[dj] = True
                if kind == "d":
                    kd[j] = True
            if o["dma"]:
                vc[i] = dict(kn)
                vcd[i] = list(kd.keys()) if len(kd) < 64 else list(kd.keys())[-64:]
            else:
                vc[i] = dict(kn)
                vc[i][E] = pos_of[i]
                vcd[i] = list(kd.keys()) if len(kd) < 64 else list(kd.keys())[-64:]
            if len(kd) > 256:
                for key in list(kd.keys())[:128]:
                    del kd[key]
            tag = i
            for Wn in o["w"]:
                lw = writes.setdefault(Wn[0], [])
                lw[:] = [(W, j) for (W, j) in lw if not _covers(Wn, W)]
                lw.append((Wn, tag))
                lr = reads.get(Wn[0])
                if lr:
                    lr[:] = [(R, j) for (R, j) in lr if not _covers(Wn, R)]
            for R in o["r"]:
                lr = reads.setdefault(R[0], [])
                if not o["dma"]:
                    lr[:] = [(R2, j) for (R2, j) in lr
                             if not (ops[j]["eng"] == E and not ops[j]["dma"] and _covers(R, R2))]
                lr.append((R, tag))
        count_of = {}
        for e in ENGS:
            c = 0
            for i in eng_ops[e]:
                if ops[i]["dma"]:
                    continue
                if signal[i]:
                    c += 1
                    count_of[i] = c
        self.stats = dict(n_ops=n, n_signal=sum(signal), n_waits=sum(len(w) for w in waits),
                          per_eng={e: len(eng_ops[e]) for e in ENGS})
        self.trace = {e: [] for e in ENGS}
        for i, o in enumerate(ops):
            E = o["eng"]
            eng = self.eng[E]
            wl = []
            for kind, Ej, j in waits[i]:
                if kind == "e":
                    eng.wait_ge(self.sem[Ej], count_of[j])
                    wl.append(("E" + Ej, count_of[j]))
                else:
                    s, tgt = dma_target[j]
                    eng.wait_ge(self.dsem[s], tgt)
                    wl.append(("D%d" % s, tgt))
            inc = None
            if o["fn"] is not None:
                if o["dma"]:
                    inc = ("D%d" % dma_target[i][0], 16)
                elif signal[i]:
                    inc = ("E" + E, 1)
            self.trace[E].append((wl, inc, i))
            if o["fn"] is None:
                continue
            ins = o["fn"]()
            if o["dma"]:
                s, tgt = dma_target[i]
                ins.then_inc(self.dsem[s], 16)
            elif signal[i]:
                ins.then_inc(self.sem[E], 1)
        return self.stats

    def final_wait(self, aps, eng="sp"):
        self.op(eng, None, reads=list(aps), writes=[])


def simulate(trace):
    sem = {}
    pc = {e: 0 for e in trace}
    progressed = True
    while progressed:
        progressed = False
        for e, tr in trace.items():
            while pc[e] < len(tr):
                wl, inc, i = tr[pc[e]]
                if all(sem.get(sn, 0) >= v for sn, v in wl):
                    if inc:
                        sem[inc[0]] = sem.get(inc[0], 0) + inc[1]
                    pc[e] += 1
                    progressed = True
                else:
                    break
    stuck = {e: (pc[e], len(tr), tr[pc[e]] if pc[e] < len(tr) else None) for e, tr in trace.items()}
    return all(pc[e] == len(tr) for e, tr in trace.items()), stuck, sem


class KB:
    def __init__(self, same_engine_sync=True):
        self.nc = bass.Bass("TRN2", target_bir_lowering=False)
        self.S = Sched(self.nc, same_engine_sync=same_engine_sync)
        self.outs = []
        self._q = 0

    def din(self, name, shape, dt=F32):
        return self.nc.dram_tensor(name, list(shape), dt, kind="ExternalInput").ap()

    def dout(self, name, shape, dt=F32):
        ap = self.nc.dram_tensor(name, list(shape), dt, kind="ExternalOutput").ap()
        self.outs.append(ap)
        return ap

    def sb(self, name, shape, dt=F32):
        return self.nc.alloc_sbuf_tensor(name, list(shape), dt)

    def ps(self, name, shape, dt=F32):
        return self.nc.alloc_psum_tensor(name, list(shape), dt)

    def dma(self, out, in_, q=None):
        if q is None:
            q = "sp"
        self.S.dma(q, out, in_)

    def mm(self, out, lhsT, rhs, start=True, stop=True):
        nc = self.nc
        self.S.op("pe", lambda: nc.tensor.matmul(out, lhsT, rhs, start=start, stop=stop),
                  [lhsT, rhs], [out])

    def tr(self, out, in_, ident):
        nc = self.nc
        self.S.op("pe", lambda: nc.tensor.transpose(out, in_, ident), [in_, ident], [out])

    def act(self, out, in_, func, bias=None, scale=None, accum_out=None):
        nc = self.nc
        kw = {}
        rd = [in_]
        wr = [out]
        if bias is not None:
            kw["bias"] = bias
            if not isinstance(bias, (int, float)):
                rd.append(bias)
        if scale is not None:
            kw["scale"] = scale
            if not isinstance(scale, (int, float)):
                rd.append(scale)
        if accum_out is not None:
            kw["accum_out"] = accum_out
            wr.append(accum_out)
        self.S.op("act", lambda: nc.scalar.activation(out, in_, func, **kw), rd, wr)

    def _veng(self, eng):
        return {"dve": self.nc.vector, "pool": self.nc.gpsimd}[eng]

    def tt(self, out, a, b, op, eng="dve"):
        e = self._veng(eng)
        self.S.op(eng, lambda: e.tensor_tensor(out, a, b, op), [a, b], [out])

    def ts(self, out, a, s1, op0, s2=None, op1=None, eng="dve", accum_out=None):
        e = self._veng(eng)
        rd = [a]
        wr = [out]
        for s in (s1, s2):
            if s is not None and not isinstance(s, (int, float)):
                rd.append(s)
        kw = {}
        if accum_out is not None:
            kw["accum_out"] = accum_out
            wr.append(accum_out)
        if op1 is None:
            self.S.op(eng, lambda: e.tensor_scalar(out, a, s1, None, op0, **kw), rd, wr)
        else:
            self.S.op(eng, lambda: e.tensor_scalar(out, a, s1, s2, op0, op1, **kw), rd, wr)

    def stt(self, out, a, s, b, op0, op1, eng="dve"):
        e = self._veng(eng)
        rd = [a, b]
        if not isinstance(s, (int, float)):
            rd.append(s)
        self.S.op(eng, lambda: e.scalar_tensor_tensor(out, a, s, b, op0, op1), rd, [out])

    def copy(self, out, in_, eng="dve"):
        if eng == "act":
            nc = self.nc
            self.S.op("act", lambda: nc.scalar.activation(out, in_, AF.Copy), [in_], [out])
        else:
            e = self._veng(eng)
            self.S.op(eng, lambda: e.tensor_copy(out, in_), [in_], [out])

    def memset(self, ap, val, eng="dve"):
        e = self._veng(eng)
        self.S.op(eng, lambda: e.memset(ap, val), [], [ap])

    def reduce(self, out, in_, op, eng="dve"):
        e = self._veng(eng)
        self.S.op(eng, lambda: e.tensor_reduce(out, in_, AX.X, op), [in_], [out])

    def recip(self, out, in_):
        nc = self.nc
        self.S.op("dve", lambda: nc.vector.reciprocal(out, in_), [in_], [out])

    def finish(self):
        self.S.final_wait(self.outs)
        st = self.S.finalize()
        return st


KEY_START = [0, 8, 24, 32]


def emit_mod(k, adaw, adab, ng_ap, cc, psmod_t):
    nc = k.nc
    cs = k.sb("cs", [128, 8, 2])
    k.dma(cs[:], cc)
    k.act(cs[:], cs[:], AF.Silu)
    adabs = k.sb("adabs", [128, 48])
    k.dma(adabs[:], adab)
    ngs = k.sb("ngs", [128, 2, 8])
    k.dma(ngs[:], ng_ap)
    psmod = psmod_t[:, 0:96].rearrange("p (a b) -> p a b", b=2)
    aw = [k.sb("aw%d" % i, [128, 8, 128]) for i in range(2)]
    adv = adaw.rearrange("(k p) n -> p k n", p=128)
    for j in range(48):
        t = aw[j % 2]
        k.dma(t[:], adv[:, :, j * 128:(j + 1) * 128], q=("sp" if j % 2 == 0 else "act"))
        for kk in range(8):
            k.mm(psmod[:, j, :], t[:, kk, :], cs[:, kk, :],
                 start=(kk == 0), stop=(kk == 7))
    modT = k.sb("modTs", [128, 48, 2])
    for col in range(2):
        k.tt(modT[:, :, col], psmod[:, :, col], adabs[:], ALU.add)
    AB = k.sb("AB", [128, 2, 8, 2])
    tmp = k.sb("modtmp", [128, 8])
    for which, (sc_i, g_i) in enumerate(((1, 0), (4, 1))):
        for col in range(2):
            k.ts(tmp[:], modT[:, sc_i * 8:(sc_i + 1) * 8, col], 1.0, ALU.add)
            k.tt(AB[:, which, :, col], tmp[:], ngs[:, g_i, :], ALU.mult)
    return modT, AB


class NormT:
    def __init__(self, k, idf, pst, tag="n"):
        self.k = k
        self.idf = idf
        self.xt = [k.sb("nx%s%d" % (tag, i), [128, 1024]) for i in range(2)]
        self.junk = k.sb("njunk" + tag, [128, 1024], BF)
        self.st = k.sb("nst" + tag, [128, 4])
        self.pst = pst
        self.i = 0

    def run(self, src_rows, ntok, dst_fn, A_fn, B_fn, keep_fn=None):
        k = self.k
        xt = self.xt[self.i % 2]
        self.i += 1
        n = ntok
        k.dma(xt[:n, :], src_rows, q="sp")
        st = self.st
        k.act(self.junk[:n, :], xt[:n, :], AF.Square, accum_out=st[:n, 0:1])
        k.ts(st[:n, 1:2], st[:n, 0:1], 1.0 / 1024.0, ALU.mult, 1e-6, ALU.add)
        k.act(st[:n, 2:3], st[:n, 1:2], AF.Sqrt)
        k.recip(st[:n, 3:4], st[:n, 2:3])
        k.ts(xt[:n, :], xt[:n, :], st[:n, 3:4], ALU.mult)
        for kk in range(8):
            k.tr(self.pst[:, kk * 128:kk * 128 + n], xt[:n, kk * 128:(kk + 1) * 128], self.idf[:n, :n])
        for kk in range(8):
            src = self.pst[:, kk * 128:kk * 128 + n]
            if kk < 4:
                k.ts(dst_fn(kk), src, A_fn(kk), ALU.mult, B_fn(kk), ALU.add)
            else:
                k.act(dst_fn(kk), src, AF.Identity, bias=B_fn(kk), scale=A_fn(kk))


def build_l0a(phase=9):
    k = KB()
    nc = k.nc
    xh = k.din("xh", [39 * 64, 1024])
    ctx = k.din("ctx", [256, 1024])
    cc = k.din("cc", [128, 8, 2])
    adaw = k.din("adaw", [1024, 6144])
    adab = k.din("adab", [128, 48])
    ng = k.din("ng", [128, 2, 8])
    win = k.din("win", [1024, 2560])
    rpbg = k.din("rpbg", [12, 4, 128, 480])
    mask = k.din("mask", [4, 4, 128, 480])
    identf = k.din("identf", [128, 128])
    c64b = k.din("c64b", [128, 128])
    s64b = k.din("s64b", [128, 128])
    c256 = k.din("c256", [256, 256])
    ns256 = k.din("ns256", [256, 256])
    aT = k.dout("aT", [256, 2048])
    oT = k.dout("oT", [768, 2048], BF)
    mcT = k.dout("mcT", [1024, 256])
    modT_o = k.dout("modT", [128, 48, 2])

    idf = k.sb("idf", [128, 128])
    idb = k.sb("idb", [128, 128], BF)
    k.dma(idf[:], identf)
    k.copy(idb[:], idf[:])

    T0 = k.ps("T0", [128, 1024])
    T1 = k.ps("T1", [128, 2, 512])
    T2 = k.ps("T2", [128, 1024])
    psA = [k.ps("psA%d" % i, [128, 512]) for i in range(2)]
    modT, AB = emit_mod(k, adaw, adab, ng, cc, psA[0])
    k.dma(modT_o, modT[:], q="sp")

    if phase < 1:
        return k, k.finish()
    winb = k.sb("winb", [128, 8, 2560], BF)
    wst = [k.sb("wst%d" % i, [128, 1280]) for i in range(2)]
    for kk in range(16):
        t = wst[kk % 2]
        hf = kk % 2
        k.dma(t[:], win[(kk // 2) * 128:(kk // 2 + 1) * 128, hf * 1280:(hf + 1) * 1280], q=("sp" if kk % 2 == 0 else "act"))
        if kk % 2 == 0:
            k.copy(winb[:, kk // 2, hf * 1280:(hf + 1) * 1280], t[:], eng="dve")
        else:
            k.copy(winb[:, kk // 2, hf * 1280:(hf + 1) * 1280], t[:], eng="act")

    if phase < 2:
        return k, k.finish()
    norm = NormT(k, idf, T0)
    psS = [T1]
    psPT = T2[:, 0:768].rearrange("p (a b) -> p a b", b=128)
    psO = T2[:, 768:896]
    cnt = {"a": 0}

    def nextA():
        cnt["a"] += 1
        return psA[cnt["a"] % 2]

    def evac(out, in_, i, scale=None):
        if scale is None:
            if i % 2 == 0:
                k.copy(out, in_, eng="dve")
            else:
                k.copy(out, in_, eng="act")
        else:
            if i % 2 == 0:
                k.ts(out, in_, scale, ALU.mult)
            else:
                k.act(out, in_, AF.Copy, scale=scale)

    Ssb = k.sb("Ssb", [128, 736])
    Pexp = k.sb("Pexp", [128, 736], BF)
    sst = k.sb("sst", [128, 4])
    Dg = k.sb("Dg", [128, 128], BF)
    PT = k.sb("PT", [128, 6, 128], BF)

    def attn_unit(q_ap, kloc_ap, nloc, maskb_ap, bias_ap, kctx_ap, vloc_fn, vctx_fn, h, out_ap):
        hp = 64 * (h % 2)
        S = psS[0]
        ntot = nloc + 256
        if nloc:
            k.mm(S[:, 0, 0:nloc].rearrange("p (a b) -> p a b", b=32), q_ap, kloc_ap, start=True, stop=False)
            k.mm(S[:, 0, 0:nloc], idb[:], maskb_ap, start=False, stop=True)
            k.tt(Ssb[:, 0:nloc], S[:, 0, 0:nloc], bias_ap, ALU.add)
        k.mm(S[:, 1, 0:256], q_ap, kctx_ap)
        k.copy(Ssb[:, nloc:ntot], S[:, 1, 0:256], eng="act")
        k.reduce(sst[:, 0:1], Ssb[:, 0:ntot], ALU.max)
        k.ts(sst[:, 1:2], sst[:, 0:1], -1.0, ALU.mult)
        k.act(Pexp[:, 0:ntot], Ssb[:, 0:ntot], AF.Exp, bias=sst[:, 1:2], accum_out=sst[:, 2:3])
        k.recip(sst[:, 3:4], sst[:, 2:3])
        k.ts(Dg[:], idf[:], sst[:, 3:4], ALU.mult)
        chunks = []
        off = 0
        while off < nloc:
            kn = min(128, nloc - off)
            chunks.append((off, kn, "l", len(chunks)))
            off += kn
        nl = len(chunks)
        chunks.append((nloc, 128, "c", 0))
        chunks.append((nloc + 128, 128, "c", 1))
        for ci, (o, kn, kind, idx) in enumerate(chunks):
            k.mm(psPT[:kn, ci, :], Pexp[:, o:o + kn], Dg[:])
        nch = len(chunks)
        if nch > 4:
            k.copy(PT[:, 0:3, :], psPT[:, 0:3, :], eng="dve")
            k.copy(PT[:96, 3, :], psPT[:96, 3, :], eng="dve")
            k.copy(PT[:, 4:nch, :], psPT[:, 4:nch, :], eng="act")
        else:
            k.copy(PT[:, 0:nch, :], psPT[:, 0:nch, :], eng="dve")
        for ci, (o, kn, kind, idx) in enumerate(chunks):
            v = vloc_fn(idx, kn) if kind == "l" else vctx_fn(idx)
            k.mm(psO[hp:hp + 64, :], v, PT[:kn, ci, :], start=(ci == 0), stop=(ci == nch - 1))
        if len(out_ap.shape) == 3:
            k.copy(out_ap, psO[hp:hp + 64, :].rearrange("p (a b) -> p a b", b=16), eng="act")
        else:
            k.copy(out_ap, psO[hp:hp + 64, :], eng="act")

    cmT = k.sb("cmT", [128, 8, 256], BF)
    for t in range(2):
        norm.run(ctx[t * 128:(t + 1) * 128, :], 128,
                 lambda kk, t=t: cmT[:, kk, t * 128:(t + 1) * 128],
                 lambda kk: AB[:, 0, kk, 1:2], lambda kk: modT[:, kk, 1:2])
    acT = k.sb("acT", [128, 2, 256])
    qcT = k.sb("qcT", [128, 6, 256], BF)
    kcT = k.sb("kcT", [128, 6, 256], BF)
    Vc = k.sb("Vc", [128, 2, 768], BF)
    for oc in range(14):
        p = nextA()
        for kk in range(8):
            k.mm(p[:, 0:256], winb[:, kk, oc * 128:(oc + 1) * 128], cmT[:, kk, :],
                 start=(kk == 0), stop=(kk == 7))
        if oc < 2:
            evac(acT[:, oc, :], p[:, 0:256], oc)
        elif oc < 8:
            evac(qcT[:, oc - 2, :], p[:, 0:256], oc, scale=0.125)
        else:
            evac(kcT[:, oc - 8, :], p[:, 0:256], oc)
    for t in range(2):
        for half in range(2):
            p = nextA()
            for kk in range(8):
                k.mm(p[:, 0:384], cmT[:, kk, t * 128:(t + 1) * 128],
                     winb[:, kk, 1792 + half * 384:1792 + (half + 1) * 384],
                     start=(kk == 0), stop=(kk == 7))
            evac(Vc[:, t, half * 384:(half + 1) * 384], p[:, 0:384], half)
    if phase < 3:
        return k, k.finish()
    cst = k.sb("cst", [128, 2, 128])
    k.dma(cst[:, 0, :], c64b)
    k.dma(cst[:, 1, :], s64b)
    c2s = k.sb("c2s", [128, 2, 2, 256])
    k.dma(c2s[:, 0, :, :], c256.rearrange("(t p) n -> p t n", p=128))
    k.dma(c2s[:, 1, :, :], ns256.rearrange("(t p) n -> p t n", p=128))
    aCS = k.sb("aCS", [128, 2, 2, 256])
    for which in range(2):
        for t in range(2):
            for cch in range(2):
                p = nextA()
                k.mm(p[:, 0:128], acT[:, cch, t * 128:(t + 1) * 128], cst[:, which, :])
                evac(aCS[:, which, t, cch * 128:(cch + 1) * 128], p[:, 0:128], cch)
    mcs = k.sb("mcs", [128, 8, 256])
    for cch in range(2):
        p = nextA()
        n = 0
        for which in range(2):
            for t in range(2):
                k.mm(p[:, 0:256], aCS[:, which, t, cch * 128:(cch + 1) * 128], c2s[:, which, t, :],
                     start=(n == 0), stop=(n == 3))
                n += 1
        evac(mcs[:, cch, :], p[:, 0:256], cch)
    if phase < 4:
        return k, k.finish()
    for t in range(2):
        for h in range(12):
            hp, hc = 64 * (h % 2), h // 2
            attn_unit(qcT[hp:hp + 64, hc, t * 128:(t + 1) * 128], None, 0, None, None,
                      kcT[hp:hp + 64, hc, :], None,
                      lambda idx, h=h: Vc[:, idx, h * 64:(h + 1) * 64], h,
                      mcs[hp:hp + 64, 2 + hc, t * 128:(t + 1) * 128])
    k.dma(mcT.rearrange("(k p) n -> p k n", p=128), mcs[:], q="sp")

    if phase < 5:
        return k, k.finish()
    xmT = k.sb("xmT", [128, 8, 15, 64], BF)
    xmK = k.sb("xmK", [128, 8, 512], BF)
    qT = k.sb("qT", [128, 6, 4, 8, 16], BF)
    kT = k.sb("kT", [128, 6, 15, 64], BF)
    Vt = k.sb("Vt", [128, 4, 768], BF)
    aTs = k.sb("aTs", [128, 2, 512])
    oTs = k.sb("oTs", [128, 6, 8, 64], BF)
    maskst = k.sb("maskst", [128, 480])
    maskb = k.sb("maskb", [128, 4, 480], BF)
    rb = [k.sb("rb%d" % i, [128, 480]) for i in range(3)]
    xmTf = xmT[:].rearrange("p k a b -> p k (a b)")
    kTf = kT[:].rearrange("p k a b -> p k (a b)")
    for rg in range(4 if phase > 5 else 1):
        R0 = rg * 8
        for ti in range(8):
            n = 128 if ti < 7 else 64
            norm.run(xh[R0 * 64 + ti * 128: R0 * 64 + ti * 128 + n, :], n,
                     lambda kk, ti=ti, n=n: xmTf[:, kk, ti * 128: ti * 128 + n],
                     lambda kk: AB[:, 0, kk, 0:1], lambda kk: modT[:, kk, 0:1])
        for j in range(4):
            k.dma(maskst[:], mask[rg, j], q="act")
            k.copy(maskb[:, j, :], maskst[:], eng="act")
        for oc in range(8):
            p = nextA()
            for kk in range(8):
                k.mm(p[:, :], winb[:, kk, oc * 128:(oc + 1) * 128], xmTf[:, kk, 256:768],
                     start=(kk == 0), stop=(kk == 7))
            if oc < 2:
                evac(aTs[:, oc, :], p[:, :], oc)
            else:
                pv = p[:, :].rearrange("p (r j c) -> p r j c", r=8, j=4)
                ov = qT[:, oc - 2, :, :, :].rearrange("p j r c -> p r j c")
                evac(ov, pv, oc, scale=0.125)
        k.dma(aT.rearrange("(k p) n -> p k n", p=128)[:, :, rg * 512:(rg + 1) * 512], aTs[:], q="sp")
        for oc in range(6):
            for half in range(2):
                p = nextA()
                for kk in range(8):
                    k.mm(p[:, 0:480], winb[:, kk, 1024 + oc * 128:1024 + (oc + 1) * 128],
                         xmTf[:, kk, half * 480:(half + 1) * 480], start=(kk == 0), stop=(kk == 7))
                evac(kTf[:, oc, half * 480:(half + 1) * 480], p[:, 0:480], half)
        u = 0
        for j in range(4):
            cs_ = KEY_START[j]
            k.copy(xmK[:, :, 0:480].rearrange("p k (a b) -> p k a b", b=32),
                   xmT[:, :, :, cs_:cs_ + 32], eng=("dve" if j % 2 == 0 else "act"))
            for rc in range(4):
                nk = 128 if rc < 3 else 96
                for half in range(2):
                    p = nextA()
                    for kk in range(8):
                        k.mm(p[:nk, 0:384], xmK[:, kk, rc * 128: rc * 128 + nk],
                             winb[:, kk, 1792 + half * 384:1792 + (half + 1) * 384],
                             start=(kk == 0), stop=(kk == 7))
                    evac(Vt[:nk, rc, half * 384:(half + 1) * 384], p[:nk, 0:384], half)
            for h in range(12):
                hp, hc = 64 * (h % 2), h // 2
                r = rb[u % 3]
                u += 1
                k.dma(r[:], rpbg[h, j], q="sp")
                attn_unit(qT[hp:hp + 64, hc, j, :, :].rearrange("p r c -> p (r c)"),
                          kT[hp:hp + 64, hc, :, cs_:cs_ + 32], 480, maskb[:, j, :], r[:],
                          kcT[hp:hp + 64, hc, :],
                          lambda idx, kn, h=h: Vt[:kn, idx, h * 64:(h + 1) * 64],
                          lambda idx, h=h: Vc[:, idx, h * 64:(h + 1) * 64], h,
                          oTs[hp:hp + 64, hc, :, j * 16:(j + 1) * 16])
        k.dma(oT.rearrange("(k p) (g n) -> p k g n", p=128, g=4)[:, :, rg, :],
              oTs[:].rearrange("p k a b -> p k (a b)"), q="sp")
    st = k.finish()
    return k, st


def build_l0b():
    k = KB()
    X = k.din("X", [128, 32, 128])
    cs1 = k.din("cs1", [128, 256])
    tw = k.din("tw", [128, 2, 128])
    cs2 = k.din("cs2", [128, 3, 128])
    U = k.dout("U", [128, 2, 32, 128])
    Xs = k.sb("Xs", [128, 32, 128])
    k.dma(Xs[:, 0:16, :], X[:, 0:16, :], q="sp")
    k.dma(Xs[:, 16:32, :], X[:, 16:32, :], q="act")
    c1 = k.sb("c1", [128, 256]); k.dma(c1[:], cs1)
    tws = k.sb("tws", [128, 2, 128]); k.dma(tws[:], tw)
    c2 = k.sb("c2", [128, 3, 128]); k.dma(c2[:], cs2)
    Bs = k.sb("Bs", [128, 2, 32, 128])
    t = [k.sb("dt%d" % i, [128, 128]) for i in range(4)]
    ps = [k.ps("dps%d" % i, [128, 512]) for i in range(4)]
    for ch in range(32):
        p = ps[ch % 2]
        k.mm(p[:, 0:256], Xs[:, ch, :], c1[:])
        Ar, Ai = p[:, 0:128], p[:, 128:256]
        k.tt(t[0][:], Ar, tws[:, 0, :], ALU.mult)
        k.tt(t[1][:], Ai, tws[:, 1, :], ALU.mult)
        k.tt(Bs[:, 0, ch, :], t[0][:], t[1][:], ALU.add)
        k.tt(t[2][:], Ai, tws[:, 0, :], ALU.mult)
        k.tt(t[3][:], Ar, tws[:, 1, :], ALU.mult)
        k.tt(Bs[:, 1, ch, :], t[2][:], t[3][:], ALU.subtract)
    Us = k.sb("Us", [128, 2, 32, 128])
    for blk in range(8):
        sl = slice(blk * 4, blk * 4 + 4)
        pr = ps[2]; pi = ps[3]
        br = Bs[:, 0, sl, :].rearrange("p a b -> p (a b)")
        bi = Bs[:, 1, sl, :].rearrange("p a b -> p (a b)")
        k.mm(pr[:], c2[:, 0, :], br, start=True, stop=False)
        k.mm(pr[:], c2[:, 1, :], bi, start=False, stop=True)
        k.mm(pi[:], c2[:, 0, :], bi, start=True, stop=False)
        k.mm(pi[:], c2[:, 2, :], br, start=False, stop=True)
        k.copy(Us[:, 0, sl, :].rearrange("p a b -> p (a b)"), pr[:], eng="dve")
        k.copy(Us[:, 1, sl, :].rearrange("p a b -> p (a b)"), pi[:], eng="act")
    k.dma(U[:, 0, :, :], Us[:, 0, :, :], q="sp")
    k.dma(U[:, 1, :, :], Us[:, 1, :, :], q="act")
    return k, k.finish()


def l0b_consts():
    n = np.arange(128)
    ang = 2 * np.pi * np.outer(n, n) / 128
    cs1 = np.concatenate([np.cos(ang), -np.sin(ang)], axis=1).astype(np.float32)
    ang2 = 2 * np.pi * np.outer(n, n) / 16384
    tw = np.stack([np.cos(ang2), np.sin(ang2)], axis=1).astype(np.float32)
    sc = 1.0 / 1024.0
    cs2 = np.stack([np.cos(ang) * sc, np.sin(ang) * sc, -np.sin(ang) * sc], axis=1).astype(np.float32)
    return dict(cs1=cs1, tw=tw, cs2=cs2)


def l0b_inputs(aT_full, core, consts):
    X = aT_full[32 * core:32 * core + 32].reshape(32, 128, 128).transpose(1, 0, 2)
    d = dict(consts)
    d["X"] = np.ascontiguousarray(X)
    return d


class Post:
    def __init__(self, k, NT, ncol_of, idf, modT, AB, T0, pbank, psR, hs, actT, wbuf, stg, Gb,
                 rw, rbb, w1, w3, w2, Gsrc):
        self.k = k
        self.NT = NT
        self.col_of = ncol_of
        self.idf, self.modT, self.AB = idf, modT, AB
        self.T0, self.pbank, self.psR = T0, pbank, psR
        self.hs, self.actT, self.wbuf, self.stg, self.Gb = hs, actT, wbuf, stg, Gb
        self.w1, self.w3, self.w2, self.Gsrc = w1, w3, w2, Gsrc
        self.xs = k.sb("p_xs", [128, 1024])
        self.junk = k.sb("p_junk", [128, 1024], BF)
        self.st = k.sb("p_st", [128, 4])
        self.xm2f = k.sb("p_xm2f", [128, 8, 128])
        self.gate = k.sb("p_gate", [128, NT, 16])
        self.rws = k.sb("p_rws", [128, 8, 16])
        k.dma(self.rws[:], rw.rearrange("(k p) n -> p k n", p=128))
        self.rbs = k.sb("p_rbs", [128, 16])
        k.dma(self.rbs[:], rbb)
        self.r = k.sb("p_r", [128, 8, 16])
        self.pr = k.sb("p_pr", [128, 4, 6])
        self.rs = k.sb("p_rs", [128, 8])
        self.hid = k.sb("p_hid", [128, 4, 512], BF)
        self.s1 = [k.sb("p_s1%d" % i, [128, 512]) for i in range(2)]

    def norm_router(self, ti):
        k = self.k
        col = self.col_of(ti)
        h = self.hs[:, ti, :]
        st = self.st
        k.act(self.junk[:], h, AF.Square, accum_out=st[:, 0:1])
        k.ts(st[:, 1:2], st[:, 0:1], 1.0 / 1024.0, ALU.mult, 1e-6, ALU.add)
        k.act(st[:, 2:3], st[:, 1:2], AF.Sqrt)
        k.recip(st[:, 3:4], st[:, 2:3])
        k.ts(self.xs[:], h, st[:, 3:4], ALU.mult)
        for kk in range(8):
            k.tr(self.T0[:, kk * 128:(kk + 1) * 128], self.xs[:, kk * 128:(kk + 1) * 128], self.idf[:])
        for kk in range(8):
            src = self.T0[:, kk * 128:(kk + 1) * 128]
            A = self.AB[:, 1, kk, col:col + 1]
            B = self.modT[:, 24 + kk, col:col + 1]
            if kk < 4:
                k.ts(self.xm2f[:, kk, :], src, A, ALU.mult, B, ALU.add)
            else:
                k.act(self.xm2f[:, kk, :], src, AF.Identity, bias=B, scale=A)
        k.copy(self.actT[:, :, ti * 128:(ti + 1) * 128], self.xm2f[:], eng=("dve" if ti % 2 == 0 else "act"))
        pR = self.psR[:, 0:16]
        for kk in range(8):
            k.mm(pR, self.xm2f[:, kk, :], self.rws[:, kk, :], start=(kk == 0), stop=(kk == 7))
        r = self.r
        sc, bi, mb, m1, tmp, sel = (r[:, i, :] for i in range(6))
        k.act(sc, pR, AF.Sigmoid)
        k.tt(bi, sc, self.rbs[:], ALU.add)
        b4 = r[:, 1, :].rearrange("p (g i) -> p g i", i=4)
        pr = self.pr
        k.tt(pr[:, :, 0:3], b4[:, :, 0:3], b4[:, :, 1:4], ALU.add)
        k.tt(pr[:, :, 3:5], b4[:, :, 0:2], b4[:, :, 2:4], ALU.add)
        k.tt(pr[:, :, 5:6], b4[:, :, 0:1], b4[:, :, 3:4], ALU.add)
        rs = self.rs
        k.reduce(rs[:, 0:4], pr[:], ALU.max)
        k.reduce(rs[:, 4:5], rs[:, 0:4], ALU.max)
        k.ts(rs[:, 0:4], rs[:, 0:4], rs[:, 4:5], ALU.is_ge)
        mb4 = r[:, 2, :].rearrange("p (g i) -> p g i", i=4)
        for i in range(4):
            k.ts(mb4[:, :, i], rs[:, 0:4], 1.0, ALU.subtract, 1.0e4, ALU.mult)
        k.tt(mb, mb, bi, ALU.add)
        k.reduce(rs[:, 5:6], mb, ALU.max)
        k.ts(m1, mb, rs[:, 5:6], ALU.is_ge)
        k.ts(tmp, m1, -1.0e4, ALU.mult)
        k.tt(tmp, tmp, mb, ALU.add)
        k.reduce(rs[:, 6:7], tmp, ALU.max)
        k.ts(sel, mb, rs[:, 6:7], ALU.is_ge)
        k.tt(sel, sel, sc, ALU.mult)
        k.reduce(rs[:, 7:8], sel, ALU.add)
        k.recip(rs[:, 7:8], rs[:, 7:8])
        k.ts(self.gate[:, ti, :], sel, rs[:, 7:8], ALU.mult)

    def moe(self, blocks):
        k = self.k
        wb = self.wbuf
        w1b = wb[:, 0:4096].rearrange("p (a b) -> p a b", b=512)
        w3b = wb[:, 4096:8192].rearrange("p (a b) -> p a b", b=512)
        w2b = [wb[:, 8192:12288].rearrange("p (a b) -> p a b", b=1024),
               wb[:, 12288:16384].rearrange("p (a b) -> p a b", b=1024)]
        ncols = sorted(set(b[2] for b in blocks))
        for c in ncols:
            k.dma(self.Gb[:, c, :], self.Gsrc[:, c, 1, :], q="act")
        pb = self.pbank
        si = 0
        for e in range(16):
            for wi, (wsrc, wdst) in enumerate(((self.w1, w1b), (self.w3, w3b))):
                for hf in range(2):
                    s_ = self.stg[si % 2]
                    si += 1
                    k.dma(s_[:], wsrc[e].rearrange("(k p) n -> p k n", p=128)[:, hf * 4:(hf + 1) * 4, :],
                          q=("sp" if si % 2 == 0 else "act"))
                    k.copy(wdst[:, hf * 4:(hf + 1) * 4, :], s_[:], eng=("dve" if si % 2 == 0 else "act"))
            for hf in range(2):
                s_ = self.stg[si % 2]
                si += 1
                k.dma(s_[:], self.w2[e].rearrange("(k p) n -> p k n", p=128)[:, :, hf * 512:(hf + 1) * 512],
                      q=("sp" if si % 2 == 0 else "act"))
                for c in ncols:
                    for fc in range(4):
                        k.tt(w2b[c][:, fc, hf * 512:(hf + 1) * 512], s_[:, fc, :],
                             self.Gb[:, c, hf * 512:(hf + 1) * 512], ALU.mult)
            for bi_, (tok0, ntok, col, tiles) in enumerate(blocks):
                for fc in range(4):
                    p1 = pb[(2 * fc) % 4]
                    p3 = pb[(2 * fc + 1) % 4]
                    for kk in range(8):
                        k.mm(p1[:, 0:ntok], w1b[:, kk, fc * 128:(fc + 1) * 128],
                             self.actT[:, kk, tok0:tok0 + ntok], start=(kk == 0), stop=(kk == 7))
                    for kk in range(8):
                        k.mm(p3[:, 0:ntok], w3b[:, kk, fc * 128:(fc + 1) * 128],
                             self.actT[:, kk, tok0:tok0 + ntok], start=(kk == 0), stop=(kk == 7))
                    s1 = self.s1[fc % 2]
                    k.act(s1[:, 0:ntok], p1[:, 0:ntok], AF.Silu)
                    k.tt(self.hid[:, fc, 0:ntok], s1[:, 0:ntok], p3[:, 0:ntok], ALU.mult)
                for tl, ti in enumerate(tiles):
                    for half in range(2):
                        pY = self.T0[:, half * 512:(half + 1) * 512]
                        for fc in range(4):
                            k.mm(pY, self.hid[:, fc, tl * 128:(tl + 1) * 128],
                                 w2b[col][:, fc, half * 512:(half + 1) * 512],
                                 start=(fc == 0), stop=(fc == 3))
                        hsl = self.hs[:, ti, half * 512:(half + 1) * 512]
                        k.stt(hsl, pY, self.gate[:, ti, e:e + 1], hsl, ALU.mult, ALU.add)


def build_l0c():
    k = KB()
    x = k.din("x", [2048, 1024])
    ctx = k.din("ctx", [256, 1024])
    UT = k.din("UT", [2, 256, 2048])
    oT = k.din("oT", [768, 2048], BF)
    mcT = k.din("mcT", [1024, 256])
    Gsrc = k.din("Gsrc", [128, 2, 2, 1024])
    modT_i = k.din("modT", [128, 48, 2])
    ng = k.din("ng", [128, 2, 8])
    wout = k.din("wout", [1024, 1024])
    c64b = k.din("c64b", [128, 128])
    s64b = k.din("s64b", [128, 128])
    identf = k.din("identf", [128, 128])
    rw = k.din("rw", [1024, 16])
    rbb = k.din("rbb", [128, 16])
    w1 = k.din("w1", [16, 1024, 512])
    w3 = k.din("w3", [16, 1024, 512])
    w2 = k.din("w2", [16, 512, 1024])
    h_o = k.dout("h", [2048, 1024])
    hc_o = k.dout("hc", [256, 1024])
    NT = 18

    idf = k.sb("idf", [128, 128]); k.dma(idf[:], identf)
    modT = k.sb("modTs", [128, 48, 2]); k.dma(modT[:], modT_i)
    ngs = k.sb("ngs", [128, 2, 8]); k.dma(ngs[:], ng)
    AB = k.sb("AB", [128, 2, 8, 2])
    tmp8 = k.sb("tmp8", [128, 8])
    for col in range(2):
        k.ts(tmp8[:], modT[:, 32:40, col], 1.0, ALU.add)
        k.tt(AB[:, 1, :, col], tmp8[:], ngs[:, 1, :], ALU.mult)
    T0 = k.ps("T0", [128, 1024])
    pbank = [k.ps("pb%d" % i, [128, 512]) for i in range(4)]
    psR = k.ps("psR", [128, 512])
    hs = k.sb("hs", [128, NT, 1024])
    actT = k.sb("actT", [128, 8, NT * 128], BF)
    wbuf = k.sb("wbuf", [128, 16384], BF)
    stg = [k.sb("stg%d" % i, [128, 4, 512]) for i in range(2)]
    Gb = k.sb("Gb", [128, 2, 1024])
    cst = k.sb("cst", [128, 2, 128])
    k.dma(cst[:, 0, :], c64b); k.dma(cst[:, 1, :], s64b)
    for c in range(2):
        k.dma(Gb[:, c, :], Gsrc[:, c, 0, :], q="act")
    k.dma(actT[:, 2:8, 0:2048], oT.rearrange("(k p) n -> p k n", p=128), q="sp")
    UTv = UT.rearrange("r (c p) n -> p r c n", p=128)
    for blk in range(4):
        s_ = stg[blk % 2]
        sv = s_[:].rearrange("p (r c) n -> p r c n", r=2)
        k.dma(sv, UTv[:, :, :, blk * 512:(blk + 1) * 512], q="sp")
        for cch in range(2):
            p = pbank[cch]
            k.mm(p[:], cst[:, 0, :], sv[:, 0, cch, :], start=True, stop=False)
            k.mm(p[:], cst[:, 1, :], sv[:, 1, cch, :], start=False, stop=True)
            k.copy(actT[:, cch, blk * 512:(blk + 1) * 512], p[:], eng=("dve" if cch == 0 else "act"))
    mcv = mcT.rearrange("(k p) n -> p k n", p=128)
    for hf in range(2):
        s_ = stg[hf % 2]
        k.dma(s_[:, :, 0:256], mcv[:, hf * 4:(hf + 1) * 4, :], q="act")
        k.copy(actT[:, hf * 4:(hf + 1) * 4, 2048:2304], s_[:, :, 0:256], eng="dve")
    woutb = wbuf[:, 0:8192].rearrange("p (a b) -> p a b", b=1024)
    wv = wout.rearrange("(k p) n -> p k n", p=128)
    for q4 in range(4):
        s_ = stg[q4 % 2]
        k.dma(s_[:].rearrange("p (a c) n -> p a (c n)", a=2), wv[:, q4 * 2:(q4 + 1) * 2, :], q="sp")
        k.copy(woutb[:, q4 * 2:(q4 + 1) * 2, :], s_[:].rearrange("p (a c) n -> p a (c n)", a=2),
               eng=("dve" if q4 % 2 == 0 else "act"))
    post = Post(k, NT, lambda ti: 0 if ti < 16 else 1, idf, modT, AB, T0, pbank, psR, hs, actT, wbuf, stg, Gb,
                rw, rbb, w1, w3, w2, Gsrc)
    xt = [k.sb("xt%d" % i, [128, 1024]) for i in range(2)]
    for ti in range(NT):
        col = 0 if ti < 16 else 1
        src = x[ti * 128:(ti + 1) * 128, :] if ti < 16 else ctx[(ti - 16) * 128:(ti - 15) * 128, :]
        t = xt[ti % 2]
        k.dma(t[:], src, q="sp")
        for half in range(2):
            pY = T0[:, half * 512:(half + 1) * 512]
            for kk in range(8):
                k.mm(pY, actT[:, kk, ti * 128:(ti + 1) * 128], woutb[:, kk, half * 512:(half + 1) * 512],
                     start=(kk == 0), stop=(kk == 7))
            hsl = hs[:, ti, half * 512:(half + 1) * 512]
            k.tt(hsl, pY, Gb[:, col, half * 512:(half + 1) * 512], ALU.mult)
            k.tt(hsl, hsl, t[:, half * 512:(half + 1) * 512], ALU.add)
        post.norm_router(ti)
    blocks = [(b * 512, 512, 0, [4 * b + i for i in range(4)]) for b in range(4)]
    blocks.append((2048, 256, 1, [16, 17]))
    post.moe(blocks)
    for ti in range(NT):
        dst = h_o[ti * 128:(ti + 1) * 128, :] if ti < 16 else hc_o[(ti - 16) * 128:(ti - 15) * 128, :]
        k.dma(dst, hs[:, ti, :], q=("sp" if ti % 2 == 0 else "act"))
    return k, k.finish()


def build_l1a(nchunks=130):
    k = KB()
    hseq = k.din("hseq", [16640, 1024])
    cc = k.din("cc", [128, 8, 2])
    adaw = k.din("adaw", [1024, 6144])
    adab = k.din("adab", [128, 48])
    ng = k.din("ng", [128, 2, 8])
    wq = k.din("wq", [1024, 256])
    wk = k.din("wk", [1024, 256])
    wv = k.din("wv", [1024, 512])
    dec = k.din("dec", [128, 1])
    iota1 = k.din("iota1", [128, 128])
    kpos = k.din("kpos", [128, 1])
    diffm = k.din("diffm", [128, 128])
    tri = k.din("tri", [128, 128])
    rowcs = k.din("rowcs", [128, 2, 256])
    colcs = k.din("colcs", [128, 2, 128])
    identf = k.din("identf", [128, 128])
    o = k.dout("o", [16384, 512])
    modT_o = k.dout("modT", [128, 48, 2])

    idf = k.sb("idf", [128, 128]); k.dma(idf[:], identf)
    idb = k.sb("idb", [128, 128], BF); k.copy(idb[:], idf[:])
    T0 = k.ps("T0", [128, 1024])
    pQ = k.ps("pQ", [128, 512])
    pK = k.ps("pK", [128, 512])
    pV = k.ps("pV", [128, 512])
    pO = k.ps("pO", [128, 512])
    pS = k.ps("pS", [128, 128])
    pT = k.ps("pT", [128, 256], BF)
    modT, AB = emit_mod(k, adaw, adab, ng, cc, pQ)
    k.dma(modT_o, modT[:], q="sp")

    stg = k.sb("stg", [128, 8, 512])
    wqb = k.sb("wqb", [128, 8, 256], BF)
    wqr = k.sb("wqr", [128, 8, 256], BF)
    wkb = k.sb("wkb", [128, 8, 256], BF)
    wkr = k.sb("wkr", [128, 8, 256], BF)
    wvb = k.sb("wvb", [128, 8, 512], BF)
    for src, dst, rot, sc in ((wq, wqb, wqr, 1.0), (wk, wkb, wkr, 0.0625)):
        k.dma(stg[:, :, 0:256], src.rearrange("(k p) n -> p k n", p=128), q="sp")
        k.ts(dst[:], stg[:, :, 0:256], sc, ALU.mult)
        for hf in range(2):
            b = hf * 128
            k.ts(rot[:, :, b:b + 64], stg[:, :, b + 64:b + 128], -sc, ALU.mult)
            k.ts(rot[:, :, b + 64:b + 128], stg[:, :, b:b + 64], sc, ALU.mult)
    k.dma(stg[:], wv.rearrange("(k p) n -> p k n", p=128), q="sp")
    k.copy(wvb[:], stg[:], eng="act")

    d = k.sb("dcy", [128, 8])
    k.dma(d[:, 0:1], dec)
    k.act(d[:, 1:2], d[:, 0:1], AF.Exp)
    k.ts(d[:, 2:3], d[:, 1:2], -1.0, ALU.mult, 1.0, ALU.add)
    k.act(d[:, 3:4], d[:, 2:3], AF.Ln)
    lg = d[:, 3:4]
    cst = k.sb("cst", [128, 4, 128])
    k.dma(cst[:, 0, :], iota1); k.dma(cst[:, 1, :], diffm); k.dma(cst[:, 2, :], tri)
    kps = k.sb("kps", [128, 1]); k.dma(kps[:], kpos)
    QD = k.sb("QD", [128, 128])
    k.act(QD[:], cst[:, 0, :], AF.Exp, scale=lg)
    DT = k.sb("DT", [128, 128])
    k.act(DT[:], cst[:, 1, :], AF.Exp, scale=lg)
    k.tt(DT[:], DT[:], cst[:, 2, :], ALU.mult)
    k.act(d[:, 4:5], kps[:], AF.Exp, scale=lg)
    KD = d[:, 4:5]
    k.ts(d[:, 5:6], lg, 128.0, ALU.mult)
    k.act(d[:, 6:7], d[:, 5:6], AF.Exp)
    CD = d[:, 6:7]
    rcs = k.sb("rcs", [128, 2, 256]); k.dma(rcs[:], rowcs)
    ccs = k.sb("ccs", [128, 2, 128]); k.dma(ccs[:], colcs)

    norm = NormT(k, idf, T0)
    xmT = [k.sb("xmT%d" % i, [128, 8, 128], BF) for i in range(2)]
    qTr = [k.sb("qTr%d" % i, [128, 2, 128], BF) for i in range(2)]
    kTr = [k.sb("kTr%d" % i, [128, 2, 128], BF) for i in range(2)]
    qd = [k.sb("qd%d" % i, [128, 2, 128], BF) for i in range(2)]
    tq = [k.sb("tq%d" % i, [128, 128]) for i in range(2)]
    vb = [k.sb("vb%d" % i, [128, 512], BF) for i in range(2)]
    kdec = [k.sb("kdec%d" % i, [128, 256], BF) for i in range(2)]
    sTd = k.sb("sTd", [128, 128], BF)
    ob = [k.sb("ob%d" % i, [128, 512]) for i in range(2)]
    Sf = k.sb("Sf", [128, 2, 512])
    Sb = k.sb("Sb", [128, 2, 512], BF)
    k.memset(Sf[:], 0.0)
    k.memset(Sb[:], 0.0)

    def rope(dst, P, gi0):
        for g in range(2):
            sl = slice(g * 64, (g + 1) * 64)
            gi = gi0 + g
            k.ts(tq[0][:, sl], P[:, 0:128][:, sl], rcs[:, 0, gi:gi + 1], ALU.mult)
            k.stt(dst[:, 0, sl], P[:, 256:384][:, sl], rcs[:, 1, gi:gi + 1], tq[0][:, sl], ALU.mult, ALU.add)
        k.tt(tq[0][:], P[:, 128:256], ccs[:, 0, :], ALU.mult)
        k.tt(tq[1][:], P[:, 384:512], ccs[:, 1, :], ALU.mult)
        k.tt(dst[:, 1, :], tq[0][:], tq[1][:], ALU.add)

    def proj(c):
        b = c % 2
        is_ctx = c < 2
        col = 1 if is_ctx else 0
        xm = xmT[b]
        norm.run(hseq[c * 128:(c + 1) * 128, :], 128, lambda kk, xm=xm: xm[:, kk, :],
                 lambda kk, col=col: AB[:, 0, kk, col:col + 1], lambda kk, col=col: modT[:, kk, col:col + 1])
        for P, wa, wr in ((pQ, wqb, wqr), (pK, wkb, wkr)):
            for part, w in enumerate((wa, wr)):
                if is_ctx and part == 1:
                    continue
                for oc in range(2):
                    out = P[:, part * 256 + oc * 128: part * 256 + (oc + 1) * 128]
                    for kk in range(8):
                        k.mm(out, w[:, kk, oc * 128:(oc + 1) * 128], xm[:, kk, :], start=(kk == 0), stop=(kk == 7))
        for kk in range(8):
            k.mm(pV[:], xm[:, kk, :], wvb[:, kk, :], start=(kk == 0), stop=(kk == 7))
        if is_ctx:
            k.copy(qTr[b][:].rearrange("p a b -> p (a b)"), pQ[:, 0:256], eng="dve")
            k.copy(kTr[b][:].rearrange("p a b -> p (a b)"), pK[:, 0:256], eng="dve")
        else:
            gi0 = 2 * (c - 2)
            rope(qTr[b], pQ, gi0)
            rope(kTr[b], pK, gi0)
        k.copy(vb[b][:], pV[:], eng="act")
        for dc in range(2):
            k.tr(pT[:, dc * 128:(dc + 1) * 128], kTr[b][:, dc, :], idb[:])
        k.ts(kdec[b][:], pT[:], KD, ALU.mult)
        if not is_ctx:
            for dc in range(2):
                k.tt(qd[b][:, dc, :], qTr[b][:, dc, :], QD[:], ALU.mult)

    def scan(c):
        b = c % 2
        is_ctx = c < 2
        if not is_ctx:
            for dc in range(2):
                k.mm(pS[:], kTr[b][:, dc, :], qTr[b][:, dc, :], start=(dc == 0), stop=(dc == 1))
            k.tt(sTd[:], pS[:], DT[:], ALU.mult)
            k.mm(pO[:], sTd[:], vb[b][:], start=True, stop=False)
            for dc in range(2):
                k.mm(pO[:], qd[b][:, dc, :], Sb[:, dc, :], start=False, stop=(dc == 1))
            obt = ob[c % 2]
            k.copy(obt[:], pO[:], eng="act")
            k.dma(o[(c - 2) * 128:(c - 1) * 128, :], obt[:], q="act")
        for dc in range(2):
            k.mm(pO[:], kdec[b][:, dc * 128:(dc + 1) * 128], vb[b][:], start=True, stop=True)
            k.stt(Sf[:, dc, :], Sf[:, dc, :], CD, pO[:], ALU.mult, ALU.add)
            k.copy(Sb[:, dc, :], Sf[:, dc, :], eng="act")

    def capture(fn, *a):
        n0 = len(k.S.ops)
        fn(*a)
        lst = k.S.ops[n0:]
        del k.S.ops[n0:]
        return lst

    def merge(A, B):
        out = []
        ia = ib = 0
        na, nb = len(A), len(B)
        while ia < na or ib < nb:
            if ib >= nb or (ia < na and ia * nb <= ib * na):
                out.append(A[ia]); ia += 1
            else:
                out.append(B[ib]); ib += 1
        return out

    proj(0)
    for c in range(nchunks):
        A = capture(proj, c + 1) if c + 1 < nchunks else []
        B = capture(scan, c)
        k.S.ops.extend(merge(A, B))
    return k, k.finish()


def rope_tables(rows, cols):
    p = np.arange(128)
    inv = 10000.0 ** (-(p % 64).astype(np.float64) / 64.0)
    ar = inv[:, None] * np.asarray(rows, np.float64)[None, :]
    ac = inv[:, None] * np.asarray(cols, np.float64)[None, :]
    rowcs = np.stack([np.cos(ar), np.sin(ar)], axis=1).astype(np.float32)
    colcs = np.stack([np.cos(ac), np.sin(ac)], axis=1).astype(np.float32)
    return rowcs, colcs


def l1a_inputs(inp, core, h_all, hc):
    hd, dr = core % 4, core // 4
    if dr == 0:
        seq = np.concatenate([hc, h_all], axis=0)
        rows = np.arange(256)
        cols = np.concatenate([np.arange(64), np.arange(64)])
    else:
        seq = np.concatenate([hc[::-1], h_all[::-1]], axis=0)
        rows = np.arange(256)[::-1]
        cols = np.concatenate([np.arange(64)[::-1], np.arange(64)[::-1]])
    rowcs, colcs = rope_tables(rows, cols)
    w = inp['ret_w_in'][0]
    n = np.arange(128)
    cc = np.stack([lay_vec(inp['c'][0]), lay_vec(inp['c_ctx'])], axis=-1)
    ng = np.stack([lay_vec(inp['norm_g'][1, 0]), lay_vec(inp['norm_g'][1, 1])], axis=1)
    diff = (n[None, :] - n[:, None]).astype(np.float32)
    return dict(hseq=np.ascontiguousarray(seq), cc=np.ascontiguousarray(cc), adaw=np.ascontiguousarray(inp['ada_w'][1]),
                adab=lay_vec(inp['ada_b'][1]), ng=np.ascontiguousarray(ng),
                wq=np.ascontiguousarray(w[:, hd * 256:(hd + 1) * 256]),
                wk=np.ascontiguousarray(w[:, 1024 + hd * 256:1024 + (hd + 1) * 256]),
                wv=np.ascontiguousarray(w[:, 2048 + hd * 512:2048 + (hd + 1) * 512]),
                dec=np.full((128, 1), inp['ret_decay'][0, dr, hd], np.float32),
                iota1=np.ascontiguousarray(np.broadcast_to((n + 1).astype(np.float32)[None, :], (128, 128))),
                kpos=(127 - n).astype(np.float32).reshape(128, 1),
                diffm=np.maximum(diff, 0.0), tri=(diff >= 0).astype(np.float32),
                rowcs=rowcs, colcs=colcs, identf=np.eye(128, dtype=np.float32))


def build_l1b():
    k = KB()
    h0 = k.din("h0", [2048, 1024])
    of = k.din("of", [2048, 2048])
    obk = k.din("ob", [2048, 2048])
    modT_i = k.din("modT", [128, 48, 2])
    ng = k.din("ng", [128, 2, 8])
    wg = k.din("wg", [1024, 2048])
    wo = k.din("wo", [2048, 1024])
    Gsrc = k.din("Gsrc", [128, 2, 2, 1024])
    identf = k.din("identf", [128, 128])
    hmid = k.dout("hmid", [2048, 1024])
    idf = k.sb("idf", [128, 128]); k.dma(idf[:], identf)
    idb = k.sb("idb", [128, 128], BF); k.copy(idb[:], idf[:])
    modT = k.sb("modTs", [128, 48, 2]); k.dma(modT[:], modT_i)
    ngs = k.sb("ngs", [128, 2, 8]); k.dma(ngs[:], ng)
    AB = k.sb("AB", [128, 2, 8, 2])
    tmp8 = k.sb("tmp8", [128, 8])
    k.ts(tmp8[:], modT[:, 8:16, 0], 1.0, ALU.add)
    k.tt(AB[:, 0, :, 0], tmp8[:], ngs[:, 0, :], ALU.mult)
    Gb = k.sb("Gb", [128, 1024]); k.dma(Gb[:], Gsrc[:, 0, 0, :], q="act")
    T0 = k.ps("T0", [128, 1024])
    pG = [k.ps("pG%d" % i, [128, 512]) for i in range(4)]
    pTr = k.ps("pTr", [128, 2048], BF)
    stg = k.sb("stg", [128, 8, 512])
    wgb = k.sb("wgb", [128, 8, 2048], BF)
    wob = k.sb("wob", [128, 16, 1024], BF)
    wgv = wg.rearrange("(k p) n -> p k n", p=128)
    for b in range(4):
        k.dma(stg[:], wgv[:, :, b * 512:(b + 1) * 512], q="sp")
        k.copy(wgb[:, :, b * 512:(b + 1) * 512], stg[:], eng=("dve" if b % 2 == 0 else "act"))
    wov = wo.rearrange("(c p) n -> p c n", p=128)
    sv = stg[:].rearrange("p (a c) n -> p a (c n)", a=4)
    for b in range(4):
        k.dma(sv, wov[:, b * 4:(b + 1) * 4, :], q="sp")
        k.copy(wob[:, b * 4:(b + 1) * 4, :], sv, eng=("dve" if b % 2 == 0 else "act"))
    norm = NormT(k, idf, T0)
    xmT = k.sb("xmT", [128, 8, 128], BF)
    oft = [k.sb("oft%d" % i, [128, 2048]) for i in range(2)]
    obt = [k.sb("obt%d" % i, [128, 2048]) for i in range(2)]
    sg = k.sb("sg", [128, 2048])
    gated = k.sb("gated", [128, 2048], BF)
    gT = k.sb("gT", [128, 16, 128], BF)
    junk = k.sb("junk2", [128, 512], BF)
    st = k.sb("st2", [128, 16])
    xt = [k.sb("xres%d" % i, [128, 1024]) for i in range(2)]
    hm = [k.sb("hm%d" % i, [128, 1024]) for i in range(2)]
    for ti in range(16):
        rows = slice(ti * 128, (ti + 1) * 128)
        norm.run(h0[rows, :], 128, lambda kk: xmT[:, kk, :],
                 lambda kk: AB[:, 0, kk, 0:1], lambda kk: modT[:, kk, 0:1])
        a, b = oft[ti % 2], obt[ti % 2]
        k.dma(a[:], of[rows, :], q="sp")
        k.dma(b[:], obk[rows, :], q="act")
        x = xt[ti % 2]
        k.dma(x[:], h0[rows, :], q="sp")
        for blk in range(4):
            for kk in range(8):
                k.mm(pG[blk][:], xmT[:, kk, :], wgb[:, kk, blk * 512:(blk + 1) * 512],
                     start=(kk == 0), stop=(kk == 7))
            k.act(sg[:, blk * 512:(blk + 1) * 512], pG[blk][:], AF.Silu)
        k.tt(a[:], a[:], b[:], ALU.add)
        for hd in range(4):
            hs_ = slice(hd * 512, (hd + 1) * 512)
            k.act(junk[:], a[:, hs_], AF.Square, accum_out=st[:, hd:hd + 1])
        k.ts(st[:, 4:8], st[:, 0:4], 1.0 / 512.0, ALU.mult, 1e-6, ALU.add)
        k.act(st[:, 8:12], st[:, 4:8], AF.Sqrt)
        k.recip(st[:, 12:16], st[:, 8:12])
        for hd in range(4):
            hs_ = slice(hd * 512, (hd + 1) * 512)
            k.stt(gated[:, hs_], a[:, hs_], st[:, 12 + hd:13 + hd], sg[:, hs_], ALU.mult, ALU.mult)
        for c in range(16):
            k.tr(pTr[:, c * 128:(c + 1) * 128], gated[:, c * 128:(c + 1) * 128], idb[:])
        gTf = gT[:].rearrange("p a b -> p (a b)")
        k.copy(gTf[:, 0:1024], pTr[:, 0:1024], eng="dve")
        k.copy(gTf[:, 1024:2048], pTr[:, 1024:2048], eng="act")
        h = hm[ti % 2]
        for half in range(2):
            pY = T0[:, half * 512:(half + 1) * 512]
            for c in range(16):
                k.mm(pY, gT[:, c, :], wob[:, c, half * 512:(half + 1) * 512], start=(c == 0), stop=(c == 15))
            hsl = h[:, half * 512:(half + 1) * 512]
            k.tt(hsl, pY, Gb[:, half * 512:(half + 1) * 512], ALU.mult)
            k.tt(hsl, hsl, x[:, half * 512:(half + 1) * 512], ALU.add)
        k.dma(hmid[rows, :], h[:], q="act")
    return k, k.finish()


def build_l1c():
    k = KB()
    hmid = k.din("hmid", [2048, 1024])
    modT_i = k.din("modT", [128, 48, 2])
    ng = k.din("ng", [128, 2, 8])
    Gsrc = k.din("Gsrc", [128, 2, 2, 1024])
    identf = k.din("identf", [128, 128])
    rw = k.din("rw", [1024, 16])
    rbb = k.din("rbb", [128, 16])
    w1 = k.din("w1", [16, 1024, 512])
    w3 = k.din("w3", [16, 1024, 512])
    w2 = k.din("w2", [16, 512, 1024])
    fgb = k.din("fgb", [128, 1024])
    out = k.dout("out", [2048, 1024])
    NT = 16
    idf = k.sb("idf", [128, 128]); k.dma(idf[:], identf)
    modT = k.sb("modTs", [128, 48, 2]); k.dma(modT[:], modT_i)
    ngs = k.sb("ngs", [128, 2, 8]); k.dma(ngs[:], ng)
    AB = k.sb("AB", [128, 2, 8, 2])
    tmp8 = k.sb("tmp8", [128, 8])
    k.ts(tmp8[:], modT[:, 32:40, 0], 1.0, ALU.add)
    k.tt(AB[:, 1, :, 0], tmp8[:], ngs[:, 1, :], ALU.mult)
    T0 = k.ps("T0", [128, 1024])
    pbank = [k.ps("pb%d" % i, [128, 512]) for i in range(4)]
    psR = k.ps("psR", [128, 512])
    hs = k.sb("hs", [128, NT, 1024])
    actT = k.sb("actT", [128, 8, NT * 128], BF)
    wbuf = k.sb("wbuf", [128, 16384], BF)
    stg = [k.sb("stg%d" % i, [128, 4, 512]) for i in range(2)]
    Gb = k.sb("Gb", [128, 2, 1024])
    fgs = k.sb("fgs", [128, 1024]); k.dma(fgs[:], fgb, q="act")
    post = Post(k, NT, lambda ti: 0, idf, modT, AB, T0, pbank, psR, hs, actT, wbuf, stg, Gb,
                rw, rbb, w1, w3, w2, Gsrc)
    for ti in range(NT):
        k.dma(hs[:, ti, :], hmid[ti * 128:(ti + 1) * 128, :], q=("sp" if ti % 2 == 0 else "act"))
        post.norm_router(ti)
    post.moe([(b * 512, 512, 0, [4 * b + i for i in range(4)]) for b in range(4)])
    st = k.sb("fst", [128, 4])
    ot = [k.sb("fot%d" % i, [128, 1024]) for i in range(2)]
    for ti in range(NT):
        h = hs[:, ti, :]
        k.act(post.junk[:], h, AF.Square, accum_out=st[:, 0:1])
        k.ts(st[:, 1:2], st[:, 0:1], 1.0 / 1024.0, ALU.mult, 1e-6, ALU.add)
        k.act(st[:, 2:3], st[:, 1:2], AF.Sqrt)
        k.recip(st[:, 3:4], st[:, 2:3])
        o_ = ot[ti % 2]
        k.stt(o_[:], h, st[:, 3:4], fgs[:], ALU.mult, ALU.mult)
        k.dma(out[ti * 128:(ti + 1) * 128, :], o_[:], q=("sp" if ti % 2 == 0 else "act"))
    return k, k.finish()


GRID_W = 64; NA_KW = 16; NA_KEYW = 32; NA_NCB = 4; NA_KH = 8

def na_static():
    j = np.arange(NA_NCB)
    key_start = np.clip(j * NA_KW - NA_KW // 2, 0, GRID_W - NA_KEYW)
    key_cols = key_start[:, None] + np.arange(NA_KEYW)[None, :]
    q_cols = j[:, None] * NA_KW + np.arange(NA_KW)[None, :]
    win_start = np.clip(q_cols - NA_KW // 2, 0, GRID_W - NA_KW)[:, :, None]
    kc = key_cols[:, None, :]
    col_mask = (kc >= win_start) & (kc < win_start + NA_KW)
    dc_idx = np.clip(kc - q_cols[:, :, None] + NA_KW - 1, 0, 2 * NA_KW - 2)
    return key_cols, col_mask, dc_idx

def lay_vec(v):
    v = np.asarray(v, np.float32).reshape(-1, 128)
    return np.ascontiguousarray(v.T)

def l0a_inputs(inp, core):
    x = inp['x'][0]
    rows = x.reshape(256, 64, 1024)
    xh = np.zeros((39, 64, 1024), np.float32)
    r0 = core * 32 - 4
    lo, hi = max(r0, 0), min(r0 + 39, 256)
    xh[lo - r0: hi - r0] = rows[lo:hi]
    cc = np.stack([lay_vec(inp['c'][0]), lay_vec(inp['c_ctx'])], axis=-1)
    ng = np.stack([lay_vec(inp['norm_g'][0, 0]), lay_vec(inp['norm_g'][0, 1])], axis=1)
    _, col_mask, dc_idx = na_static()
    rpb = inp['na_rpb'][0]
    qr = np.arange(8)[:, None, None, None]; kr = np.arange(15)[None, None, :, None]
    dr = np.clip(kr - qr + 3, 0, 14)
    rpbg = np.empty((12, 4, 128, 480), np.float32)
    mask = np.empty((4, 4, 128, 480), np.float32)
    for j in range(4):
        dc = dc_idx[j][None, :, None, :]
        drb = np.broadcast_to(dr, (8, 16, 15, 32)); dcb = np.broadcast_to(dc, (8, 16, 15, 32))
        rpbg[:, j] = rpb[:, drb, dcb].reshape(12, 128, 480)
        cm = np.broadcast_to(col_mask[j][None, :, None, :], (8, 16, 15, 32))
        for rg in range(4):
            r = core * 32 + rg * 8 + np.arange(8)
            rs = np.clip(r - 4, 0, 248)
            keyrow = core * 32 + rg * 8 - 4 + np.arange(15)
            rm = (keyrow[None, :] >= rs[:, None]) & (keyrow[None, :] < rs[:, None] + 8)
            valid = cm & rm[:, None, :, None]
            mask[rg, j] = np.where(valid, 0.0, -30000.0).reshape(128, 480).astype(np.float32)
    a = np.arange(64)
    C64 = np.cos(2 * np.pi * np.outer(a, a) / 64); S64 = np.sin(2 * np.pi * np.outer(a, a) / 64)
    c64b = np.kron(np.eye(2), C64).astype(np.float32); s64b = np.kron(np.eye(2), S64).astype(np.float32)
    n = np.arange(256)
    c256 = (np.cos(2 * np.pi * np.outer(n, n) / 256) / 128).astype(np.float32)
    ns256 = (-np.sin(2 * np.pi * np.outer(n, n) / 256) / 128).astype(np.float32)
    return dict(xh=xh.reshape(39 * 64, 1024), ctx=np.ascontiguousarray(inp['ctx'][0]), cc=np.ascontiguousarray(cc),
                adaw=np.ascontiguousarray(inp['ada_w'][0]), adab=lay_vec(inp['ada_b'][0]), ng=np.ascontiguousarray(ng),
                win=np.ascontiguousarray(inp['mixab_w_in'][0]), rpbg=rpbg, mask=mask,
                identf=np.eye(128, dtype=np.float32), c64b=c64b, s64b=s64b, c256=c256, ns256=ns256)

def gsrc_from_modT(modT):
    rows = np.empty((2, 2, 1024), np.float32)
    for col in range(2):
        for gi, which in enumerate((2, 5)):
            rows[col, gi] = modT[:, which * 8:(which + 1) * 8, col].T.reshape(-1)
    return np.ascontiguousarray(np.broadcast_to(rows[None], (128, 2, 2, 1024)))

def l0c_inputs(inp, core, UT, oT, mcT, modT, layer=0):
    a = np.arange(64)
    C64 = np.cos(2 * np.pi * np.outer(a, a) / 64); S64 = np.sin(2 * np.pi * np.outer(a, a) / 64)
    ng = np.stack([lay_vec(inp['norm_g'][layer, 0]), lay_vec(inp['norm_g'][layer, 1])], axis=1)
    return dict(x=np.ascontiguousarray(inp['x'][0, core * 2048:(core + 1) * 2048]), ctx=np.ascontiguousarray(inp['ctx'][0]),
                UT=np.ascontiguousarray(UT), oT=oT, mcT=mcT, Gsrc=gsrc_from_modT(modT), modT=modT, ng=np.ascontiguousarray(ng),
                wout=np.ascontiguousarray(inp['mixab_w_out'][0]),
                c64b=np.kron(np.eye(2), C64).astype(np.float32), s64b=np.kron(np.eye(2), S64).astype(np.float32),
                identf=np.eye(128, dtype=np.float32), rw=np.ascontiguousarray(inp['router_w']),
                rbb=np.ascontiguousarray(np.broadcast_to(inp['router_b'][None, :], (128, 16))),
                w1=np.ascontiguousarray(inp['moe_w1'][layer]), w3=np.ascontiguousarray(inp['moe_w3'][layer]),
                w2=np.ascontiguousarray(inp['moe_w2'][layer]))


def _run(kb, ins):
    return run_bass_kernel_spmd(kb.nc, ins, core_ids=list(range(8))).results


def kernel(**inputs):
    inp = {k_: np.asarray(v) for k_, v in inputs.items()}
    NC = 8
    kb, _ = build_l0a()
    r = _run(kb, [l0a_inputs(inp, c) for c in range(NC)])
    aT = np.concatenate([np.asarray(r[c]["aT"]) for c in range(NC)], axis=1)
    oT = [np.asarray(r[c]["oT"]) for c in range(NC)]
    mcT = np.asarray(r[0]["mcT"])
    modT0 = np.asarray(r[0]["modT"])
    del r
    kb, _ = build_l0b()
    cst = l0b_consts()
    r = _run(kb, [l0b_inputs(aT, c, cst) for c in range(NC)])
    U = np.concatenate([np.asarray(r[c]["U"]).transpose(1, 2, 0, 3).reshape(2, 32, 16384) for c in range(NC)], axis=1)
    del r
    kb, _ = build_l0c()
    r = _run(kb, [l0c_inputs(inp, c, U[:, :, c * 2048:(c + 1) * 2048], oT[c], mcT, modT0) for c in range(NC)])
    h_all = np.concatenate([np.asarray(r[c]["h"]) for c in range(NC)], axis=0)
    hc = np.asarray(r[0]["hc"])
    del r, U, oT
    kb, _ = build_l1a()
    r = _run(kb, [l1a_inputs(inp, c, h_all, hc) for c in range(NC)])
    modT1 = np.asarray(r[0]["modT"])
    of = np.concatenate([np.asarray(r[c]["o"]) for c in range(4)], axis=1)
    ob = np.concatenate([np.asarray(r[c]["o"])[::-1] for c in range(4, 8)], axis=1)
    del r
    ng1 = np.ascontiguousarray(np.stack([lay_vec(inp['norm_g'][1, 0]), lay_vec(inp['norm_g'][1, 1])], axis=1))
    G1 = gsrc_from_modT(modT1)
    idn = np.eye(128, dtype=np.float32)
    kb, _ = build_l1b()
    wg = np.ascontiguousarray(inp['ret_w_in'][0][:, 4096:6144])
    wo = np.ascontiguousarray(inp['ret_w_out'][0])
    r = _run(kb, [dict(h0=np.ascontiguousarray(h_all[c * 2048:(c + 1) * 2048]),
                       of=np.ascontiguousarray(of[c * 2048:(c + 1) * 2048]),
                       ob=np.ascontiguousarray(ob[c * 2048:(c + 1) * 2048]),
                       modT=modT1, ng=ng1, wg=wg, wo=wo, Gsrc=G1, identf=idn) for c in range(NC)])
    hmid = [np.asarray(r[c]["hmid"]) for c in range(NC)]
    del r, of, ob
    kb, _ = build_l1c()
    common = dict(modT=modT1, ng=ng1, Gsrc=G1, identf=idn, rw=np.ascontiguousarray(inp['router_w']),
                  rbb=np.ascontiguousarray(np.broadcast_to(inp['router_b'][None, :], (128, 16))),
                  w1=np.ascontiguousarray(inp['moe_w1'][1]), w3=np.ascontiguousarray(inp['moe_w3'][1]),
                  w2=np.ascontiguousarray(inp['moe_w2'][1]),
                  fgb=np.ascontiguousarray(np.broadcast_to(inp['final_norm_g'][None, :], (128, 1024))))
    r = _run(kb, [dict(common, hmid=hmid[c]) for c in range(NC)])
    out = np.concatenate([np.asarray(r[c]["out"]) for c in range(NC)], axis=0)
    return out.reshape(1, 16384, 1024).astype(np.float32, copy=False)
```

```python
import numpy as np
import concourse.bass as bass
import concourse.mybir as mybir
from concourse.bass_utils import run_bass_kernel_spmd

F32 = mybir.dt.float32
BF = mybir.dt.bfloat16
AF = mybir.ActivationFunctionType
ALU = mybir.AluOpType
AX = mybir.AxisListType


ENGS = ("pe", "act", "dve", "pool", "sp")


def _region(ap):
    t = ap.tensor
    shape = list(t.shape)
    space = str(ap.space) if hasattr(ap, "space") else ""
    off = int(ap.offset)
    dims = [(int(s), int(c)) for s, c in ap.ap]
    is_dram = "DRam" in type(t).__name__
    if is_dram:
        lo = off
        hi = off + sum((c - 1) * abs(s) for s, c in dims) + 1
        return (t.name, 0, 1, lo, hi)
    F = 1
    for s in shape[1:]:
        F *= int(s)
    p0 = off // F
    f0 = off % F
    p1 = p0
    f1 = f0
    for s, c in dims:
        if c <= 1:
            continue
        if s != 0 and s % F == 0:
            p1 += (c - 1) * (s // F)
        else:
            f1 += (c - 1) * abs(s)
    if "PSum" in type(t).__name__:
        f0 = (f0 // 512) * 512
        f1 = ((f1 // 512) + 1) * 512 - 1
        return (t.name, 0, 128, f0, f1 + 1)
    return (t.name, p0, p1 + 1, f0, f1 + 1)


def _overlap(a, b):
    return a[1] < b[2] and b[1] < a[2] and a[3] < b[4] and b[3] < a[4]


def _covers(a, b):
    return a[1] <= b[1] and a[2] >= b[2] and a[3] <= b[3] and a[4] >= b[4]


class Sched:
    def __init__(self, nc, n_dma_sems=40, same_engine_sync=True):
        self.nc = nc
        self.eng = {"pe": nc.tensor, "act": nc.scalar, "dve": nc.vector,
                    "pool": nc.gpsimd, "sp": nc.sync}
        self.ops = []
        self.same_engine_sync = same_engine_sync
        self.n_dma_sems = n_dma_sems
        self.sem = {e: nc.alloc_semaphore(name="sem_" + e) for e in ENGS}
        self.dsem = [nc.alloc_semaphore(name="dsem%d" % i) for i in range(n_dma_sems)]
        self._dma_rr = 0

    def op(self, eng, fn, reads=(), writes=(), unordered_same=False):
        self.ops.append(dict(eng=eng, fn=fn, r=[_region(a) for a in reads],
                             w=[_region(a) for a in writes], dma=False,
                             relax=unordered_same,
                             xr=[_region(a) for a in reads if "PSum" in type(a.tensor).__name__]))

    def dma(self, q, out, in_, **kw):
        slot = self._dma_rr
        self._dma_rr = (self._dma_rr + 1) % self.n_dma_sems
        e = self.eng[q]
        self.ops.append(dict(eng=q, fn=lambda: e.dma_start(out=out, in_=in_, **kw),
                             r=[_region(in_)], w=[_region(out)], dma=True, slot=slot,
                             relax=False))

    def finalize(self):
        ops = self.ops
        n = len(ops)
        pos_of = [0] * n
        eng_cnt = {e: 0 for e in ENGS}
        eng_ops = {e: [] for e in ENGS}
        for i, o in enumerate(ops):
            eng_cnt[o["eng"]] += 1
            pos_of[i] = eng_cnt[o["eng"]]
            eng_ops[o["eng"]].append(i)
        writes = {}
        reads = {}
        known = {e: {x: 0 for x in ENGS} for e in ENGS}
        known_d = {e: {} for e in ENGS}
        vc = [None] * n
        vcd = [None] * n
        waits = [None] * n
        signal = [False] * n
        slot_last = {}
        dma_target = {}
        slot_cnt = {}
        for i, o in enumerate(ops):
            E = o["eng"]
            deps = set()
            for R in o["r"]:
                for (W, j) in writes.get(R[0], ()):
                    if _overlap(W, R):
                        deps.add(j)
            for Wn in o["w"]:
                for (W, j) in writes.get(Wn[0], ()):
                    if _overlap(W, Wn):
                        deps.add(j)
                for (R, j) in reads.get(Wn[0], ()):
                    if _overlap(R, Wn):
                        deps.add(j)
            for R in o.get("xr", ()):
                for (R2, j) in reads.get(R[0], ()):
                    if ops[j]["eng"] != E and _overlap(R2, R):
                        deps.add(j)
            if o["dma"]:
                s = o["slot"]
                if s in slot_last:
                    deps.add(slot_last[s])
                slot_last[s] = i
                slot_cnt[s] = slot_cnt.get(s, 0) + 1
                dma_target[i] = (s, 16 * slot_cnt[s])
            deps.discard(i)
            kn = known[E]
            kd = known_d[E]
            need_e = {}
            need_d = []
            for j in deps:
                oj = ops[j]
                if oj["dma"]:
                    if kd.get(j, False):
                        continue
                    need_d.append(j)
                else:
                    Ej = oj["eng"]
                    if Ej == E and not o["dma"]:
                        if (not self.same_engine_sync) or E == "pe" or o["relax"]:
                            continue
                    if kn[Ej] >= pos_of[j]:
                        continue
                    if need_e.get(Ej, (0, -1))[0] < pos_of[j]:
                        need_e[Ej] = (pos_of[j], j)
            w_list = []
            for Ej, (p, j) in need_e.items():
                w_list.append(("e", Ej, j))
                signal[j] = True
            for j in need_d:
                w_list.append(("d", None, j))
            waits[i] = w_list
            for kind, Ej, j in w_list:
                for x in ENGS:
                    if vc[j][x] > kn[x]:
                        kn[x] = vc[j][x]
                for dj in vcd[j]:
                    kd[dj] = True
                if kind == "d":
                    kd[j] = True
            if o["dma"]:
                vc[i] = dict(kn)
                vcd[i] = list(kd.keys()) if len(kd) < 64 else list(kd.keys())[-64:]
            else:
                vc[i] = dict(kn)
                vc[i][E] = pos_of[i]
                vcd[i] = list(kd.keys()) if len(kd) < 64 else list(kd.keys())[-64:]
            if len(kd) > 256:
                for key in list(kd.keys())[:128]:
                    del kd[key]
            tag = i
            for Wn in o["w"]:
                lw = writes.setdefault(Wn[0], [])
                lw[:] = [(W, j) for (W, j) in lw if not _covers(Wn, W)]
                lw.append((Wn, tag))
                lr = reads.get(Wn[0])
                if lr:
                    lr[:] = [(R, j) for (R, j) in lr if not _covers(Wn, R)]
            for R in o["r"]:
                lr = reads.setdefault(R[0], [])
                if not o["dma"]:
                    lr[:] = [(R2, j) for (R2, j) in lr
                             if not (ops[j]["eng"] == E and not ops[j]["dma"] and _covers(R, R2))]
                lr.append((R, tag))
        count_of = {}
        for e in ENGS:
            c = 0
            for i in eng_ops[e]:
                if ops[i]["dma"]:
                    continue
                if signal[i]:
                    c += 1
                    count_of[i] = c
        self.stats = dict(n_ops=n, n_signal=sum(signal), n_waits=sum(len(w) for w in waits),
                          per_eng={e: len(eng_ops[e]) for e in ENGS})
        self.trace = {e: [] for e in ENGS}
        for i, o in enumerate(ops):
            E = o["eng"]
            eng = self.eng[E]
            wl = []
            for kind, Ej, j in waits[i]:
                if kind == "e":
                    eng.wait_ge(self.sem[Ej], count_of[j])
                    wl.append(("E" + Ej, count_of[j]))
                else:
                    s, tgt = dma_target[j]
                    eng.wait_ge(self.dsem[s], tgt)
                    wl.append(("D%d" % s, tgt))
            inc = None
            if o["fn"] is not None:
                if o["dma"]:
                    inc = ("D%d" % dma_target[i][0], 16)
                elif signal[i]:
                    inc = ("E" + E, 1)
            self.trace[E].append((wl, inc, i))
            if o["fn"] is None:
                continue
            ins = o["fn"]()
            if o["dma"]:
                s, tgt = dma_target[i]
                ins.then_inc(self.dsem[s], 16)
            elif signal[i]:
                ins.then_inc(self.sem[E], 1)
        return self.stats

    def final_wait(self, aps, eng="sp"):
        self.op(eng, None, reads=list(aps), writes=[])


def simulate(trace):
    sem = {}
    pc = {e: 0 for e in trace}
    progressed = True
    while progressed:
        progressed = False
        for e, tr in trace.items():
            while pc[e] < len(tr):
                wl, inc, i = tr[pc[e]]
                if all(sem.get(sn, 0) >= v for sn, v in wl):
                    if inc:
                        sem[inc[0]] = sem.get(inc[0], 0) + inc[1]
                    pc[e] += 1
                    progressed = True
                else:
                    break
    stuck = {e: (pc[e], len(tr), tr[pc[e]] if pc[e] < len(tr) else None) for e, tr in trace.items()}
    return all(pc[e] == len(tr) for e, tr in trace.items()), stuck, sem


class KB:
    def __init__(self, same_engine_sync=True):
        self.nc = bass.Bass("TRN2", target_bir_lowering=False)
        self.S = Sched(self.nc, same_engine_sync=same_engine_sync)
        self.outs = []
        self._q = 0

    def din(self, name, shape, dt=F32):
        return self.nc.dram_tensor(name, list(shape), dt, kind="ExternalInput").ap()

    def dout(self, name, shape, dt=F32):
        ap = self.nc.dram_tensor(name, list(shape), dt, kind="ExternalOutput").ap()
        self.outs.append(ap)
        return ap

    def sb(self, name, shape, dt=F32):
        return self.nc.alloc_sbuf_tensor(name, list(shape), dt)

    def ps(self, name, shape, dt=F32):
        return self.nc.alloc_psum_tensor(name, list(shape), dt)

    def dma(self, out, in_, q=None):
        if q is None:
            q = "sp"
        self.S.dma(q, out, in_)

    def mm(self, out, lhsT, rhs, start=True, stop=True):
        nc = self.nc
        self.S.op("pe", lambda: nc.tensor.matmul(out, lhsT, rhs, start=start, stop=stop),
                  [lhsT, rhs], [out])

    def tr(self, out, in_, ident):
        nc = self.nc
        self.S.op("pe", lambda: nc.tensor.transpose(out, in_, ident), [in_, ident], [out])

    def act(self, out, in_, func, bias=None, scale=None, accum_out=None):
        nc = self.nc
        kw = {}
        rd = [in_]
        wr = [out]
        if bias is not None:
            kw["bias"] = bias
            if not isinstance(bias, (int, float)):
                rd.append(bias)
        if scale is not None:
            kw["scale"] = scale
            if not isinstance(scale, (int, float)):
                rd.append(scale)
        if accum_out is not None:
            kw["accum_out"] = accum_out
            wr.append(accum_out)
        self.S.op("act", lambda: nc.scalar.activation(out, in_, func, **kw), rd, wr)

    def _veng(self, eng):
        return {"dve": self.nc.vector, "pool": self.nc.gpsimd}[eng]

    def tt(self, out, a, b, op, eng="dve"):
        e = self._veng(eng)
        self.S.op(eng, lambda: e.tensor_tensor(out, a, b, op), [a, b], [out])

    def ts(self, out, a, s1, op0, s2=None, op1=None, eng="dve", accum_out=None):
        e = self._veng(eng)
        rd = [a]
        wr = [out]
        for s in (s1, s2):
            if s is not None and not isinstance(s, (int, float)):
                rd.append(s)
        kw = {}
        if accum_out is not None:
            kw["accum_out"] = accum_out
            wr.append(accum_out)
        if op1 is None:
            self.S.op(eng, lambda: e.tensor_scalar(out, a, s1, None, op0, **kw), rd, wr)
        else:
            self.S.op(eng, lambda: e.tensor_scalar(out, a, s1, s2, op0, op1, **kw), rd, wr)

    def stt(self, out, a, s, b, op0, op1, eng="dve"):
        e = self._veng(eng)
        rd = [a, b]
        if not isinstance(s, (int, float)):
            rd.append(s)
        self.S.op(eng, lambda: e.scalar_tensor_tensor(out, a, s, b, op0, op1), rd, [out])

    def copy(self, out, in_, eng="dve"):
        if eng == "act":
            nc = self.nc
            self.S.op("act", lambda: nc.scalar.activation(out, in_, AF.Copy), [in_], [out])
        else:
            e = self._veng(eng)
            self.S.op(eng, lambda: e.tensor_copy(out, in_), [in_], [out])

    def memset(self, ap, val, eng="dve"):
        e = self._veng(eng)
        self.S.op(eng, lambda: e.memset(ap, val), [], [ap])

    def reduce(self, out, in_, op, eng="dve"):
        e = self._veng(eng)
        self.S.op(eng, lambda: e.tensor_reduce(out, in_, AX.X, op), [in_], [out])

    def recip(self, out, in_):
        nc = self.nc
        self.S.op("dve", lambda: nc.vector.reciprocal(out, in_), [in_], [out])

    def finish(self):
        self.S.final_wait(self.outs)
        st = self.S.finalize()
        return st


KEY_START = [0, 8, 24, 32]


def emit_mod(k, adaw, adab, ng_ap, cc, psmod_t):
    nc = k.nc
    cs = k.sb("cs", [128, 8, 2])
    k.dma(cs[:], cc)
    k.act(cs[:], cs[:], AF.Silu)
    adabs = k.sb("adabs", [128, 48])
    k.dma(adabs[:], adab)
    ngs = k.sb("ngs", [128, 2, 8])
    k.dma(ngs[:], ng_ap)
    psmod = psmod_t[:, 0:96].rearrange("p (a b) -> p a b", b=2)
    aw = [k.sb("aw%d" % i, [128, 8, 128]) for i in range(2)]
    adv = adaw.rearrange("(k p) n -> p k n", p=128)
    for j in range(48):
        t = aw[j % 2]
        k.dma(t[:], adv[:, :, j * 128:(j + 1) * 128], q=("sp" if j % 2 == 0 else "act"))
        for kk in range(8):
            k.mm(psmod[:, j, :], t[:, kk, :], cs[:, kk, :],
                 start=(kk == 0), stop=(kk == 7))
    modT = k.sb("modTs", [128, 48, 2])
    for col in range(2):
        k.tt(modT[:, :, col], psmod[:, :, col], adabs[:], ALU.add)
    AB = k.sb("AB", [128, 2, 8, 2])
    tmp = k.sb("modtmp", [128, 8])
    for which, (sc_i, g_i) in enumerate(((1, 0), (4, 1))):
        for col in range(2):
            k.ts(tmp[:], modT[:, sc_i * 8:(sc_i + 1) * 8, col], 1.0, ALU.add)
            k.tt(AB[:, which, :, col], tmp[:], ngs[:, g_i, :], ALU.mult)
    return modT, AB


class NormT:
    def __init__(self, k, idf, pst, tag="n"):
        self.k = k
        self.idf = idf
        self.xt = [k.sb("nx%s%d" % (tag, i), [128, 1024]) for i in range(2)]
        self.junk = k.sb("njunk" + tag, [128, 1024], BF)
        self.st = k.sb("nst" + tag, [128, 4])
        self.pst = pst
        self.i = 0

    def run(self, src_rows, ntok, dst_fn, A_fn, B_fn, keep_fn=None):
        k = self.k
        xt = self.xt[self.i % 2]
        self.i += 1
        n = ntok
        k.dma(xt[:n, :], src_rows, q="sp")
        st = self.st
        k.act(self.junk[:n, :], xt[:n, :], AF.Square, accum_out=st[:n, 0:1])
        k.ts(st[:n, 1:2], st[:n, 0:1], 1.0 / 1024.0, ALU.mult, 1e-6, ALU.add)
        k.act(st[:n, 2:3], st[:n, 1:2], AF.Sqrt)
        k.recip(st[:n, 3:4], st[:n, 2:3])
        k.ts(xt[:n, :], xt[:n, :], st[:n, 3:4], ALU.mult)
        for kk in range(8):
            k.tr(self.pst[:, kk * 128:kk * 128 + n], xt[:n, kk * 128:(kk + 1) * 128], self.idf[:n, :n])
        for kk in range(8):
            src = self.pst[:, kk * 128:kk * 128 + n]
            if kk < 4:
                k.ts(dst_fn(kk), src, A_fn(kk), ALU.mult, B_fn(kk), ALU.add)
            else:
                k.act(dst_fn(kk), src, AF.Identity, bias=B_fn(kk), scale=A_fn(kk))


def build_l0a(phase=9):
    k = KB()
    nc = k.nc
    xh = k.din("xh", [39 * 64, 1024])
    ctx = k.din("ctx", [256, 1024])
    cc = k.din("cc", [128, 8, 2])
    adaw = k.din("adaw", [1024, 6144])
    adab = k.din("adab", [128, 48])
    ng = k.din("ng", [128, 2, 8])
    win = k.din("win", [1024, 2560])
    rpbg = k.din("rpbg", [12, 4, 128, 480])
    mask = k.din("mask", [4, 4, 128, 480])
    identf = k.din("identf", [128, 128])
    c64b = k.din("c64b", [128, 128])
    s64b = k.din("s64b", [128, 128])
    c256 = k.din("c256", [256, 256])
    ns256 = k.din("ns256", [256, 256])
    aT = k.dout("aT", [256, 2048])
    oT = k.dout("oT", [768, 2048], BF)
    mcT = k.dout("mcT", [1024, 256])
    modT_o = k.dout("modT", [128, 48, 2])

    idf = k.sb("idf", [128, 128])
    idb = k.sb("idb", [128, 128], BF)
    k.dma(idf[:], identf)
    k.copy(idb[:], idf[:])

    T0 = k.ps("T0", [128, 1024])
    T1 = k.ps("T1", [128, 2, 512])
    T2 = k.ps("T2", [128, 1024])
    psA = [k.ps("psA%d" % i, [128, 512]) for i in range(2)]
    modT, AB = emit_mod(k, adaw, adab, ng, cc, psA[0])
    k.dma(modT_o, modT[:], q="sp")

    if phase < 1:
        return k, k.finish()
    winb = k.sb("winb", [128, 8, 2560], BF)
    wst = [k.sb("wst%d" % i, [128, 1280]) for i in range(2)]
    for kk in range(16):
        t = wst[kk % 2]
        hf = kk % 2
        k.dma(t[:], win[(kk // 2) * 128:(kk // 2 + 1) * 128, hf * 1280:(hf + 1) * 1280], q=("sp" if kk % 2 == 0 else "act"))
        if kk % 2 == 0:
            k.copy(winb[:, kk // 2, hf * 1280:(hf + 1) * 1280], t[:], eng="dve")
        else:
            k.copy(winb[:, kk // 2, hf * 1280:(hf + 1) * 1280], t[:], eng="act")

    if phase < 2:
        return k, k.finish()
    norm = NormT(k, idf, T0)
    psS = [T1]
    psPT = T2[:, 0:768].rearrange("p (a b) -> p a b", b=128)
    psO = T2[:, 768:896]
    cnt = {"a": 0}

    def nextA():
        cnt["a"] += 1
        return psA[cnt["a"] % 2]

    def evac(out, in_, i, scale=None):
        if scale is None:
            if i % 2 == 0:
                k.copy(out, in_, eng="dve")
            else:
                k.copy(out, in_, eng="act")
        else:
            if i % 2 == 0:
                k.ts(out, in_, scale, ALU.mult)
            else:
                k.act(out, in_, AF.Copy, scale=scale)

    Ssb = k.sb("Ssb", [128, 736])
    Pexp2 = [k.sb("Pexp%d" % i, [128, 736], BF) for i in range(2)]
    sst = k.sb("sst", [128, 4])
    Dg2 = [k.sb("Dg%d" % i, [128, 128], BF) for i in range(2)]
    PT = k.sb("PT", [128, 6, 128], BF)

    def unit_front(u, bpar):
        (q_ap, kloc_ap, nloc, maskb_ap, bias_fn, kctx_ap, vloc_fn, vctx_fn, h, out_ap) = u
        Pexp, Dg = Pexp2[bpar], Dg2[bpar]
        S = psS[0]
        ntot = nloc + 256
        if nloc:
            bias_ap = bias_fn()
            k.mm(S[:, 0, 0:nloc].rearrange("p (a b) -> p a b", b=32), q_ap, kloc_ap, start=True, stop=False)
            k.mm(S[:, 0, 0:nloc], idb[:], maskb_ap, start=False, stop=True)
            k.tt(Ssb[:, 0:nloc], S[:, 0, 0:nloc], bias_ap, ALU.add)
        k.mm(S[:, 1, 0:256], q_ap, kctx_ap)
        k.copy(Ssb[:, nloc:ntot], S[:, 1, 0:256], eng="act")
        k.reduce(sst[:, 0:1], Ssb[:, 0:ntot], ALU.max)
        k.ts(sst[:, 1:2], sst[:, 0:1], -1.0, ALU.mult)
        k.act(Pexp[:, 0:ntot], Ssb[:, 0:ntot], AF.Exp, bias=sst[:, 1:2], accum_out=sst[:, 2:3])
        k.recip(sst[:, 3:4], sst[:, 2:3])
        k.ts(Dg[:], idf[:], sst[:, 3:4], ALU.mult)

    def unit_back(u, bpar):
        (q_ap, kloc_ap, nloc, maskb_ap, bias_fn, kctx_ap, vloc_fn, vctx_fn, h, out_ap) = u
        Pexp, Dg = Pexp2[bpar], Dg2[bpar]
        hp = 64 * (h % 2)
        chunks = []
        off = 0
        while off < nloc:
            kn = min(128, nloc - off)
            chunks.append((off, kn, "l", len(chunks)))
            off += kn
        chunks.append((nloc, 128, "c", 0))
        chunks.append((nloc + 128, 128, "c", 1))
        for ci, (o, kn, kind, idx) in enumerate(chunks):
            k.mm(psPT[:kn, ci, :], Pexp[:, o:o + kn], Dg[:])
        nch = len(chunks)
        if nch > 4:
            k.copy(PT[:, 0:3, :], psPT[:, 0:3, :], eng="dve")
            k.copy(PT[:96, 3, :], psPT[:96, 3, :], eng="dve")
            k.copy(PT[:, 4:nch, :], psPT[:, 4:nch, :], eng="act")
        else:
            k.copy(PT[:, 0:nch, :], psPT[:, 0:nch, :], eng="dve")
        for ci, (o, kn, kind, idx) in enumerate(chunks):
            v = vloc_fn(idx, kn) if kind == "l" else vctx_fn(idx)
            k.mm(psO[hp:hp + 64, :], v, PT[:kn, ci, :], start=(ci == 0), stop=(ci == nch - 1))
        if len(out_ap.shape) == 3:
            k.copy(out_ap, psO[hp:hp + 64, :].rearrange("p (a b) -> p a b", b=16), eng="act")
        else:
            k.copy(out_ap, psO[hp:hp + 64, :], eng="act")

    def capture(fn, *a):
        n0 = len(k.S.ops)
        fn(*a)
        lst = k.S.ops[n0:]
        del k.S.ops[n0:]
        return lst

    def merge(A, B):
        out = []
        ia = ib = 0
        na, nb = len(A), len(B)
        while ia < na or ib < nb:
            if ib >= nb or (ia < na and ia * nb <= ib * na):
                out.append(A[ia]); ia += 1
            else:
                out.append(B[ib]); ib += 1
        return out

    def run_units(units):
        n = len(units)
        unit_front(units[0], 0)
        for i in range(n):
            if i + 1 < n:
                A = capture(unit_front, units[i + 1], (i + 1) % 2)
                B = capture(unit_back, units[i], i % 2)
                k.S.ops.extend(merge(A, B))
            else:
                unit_back(units[i], i % 2)

    cmT = k.sb("cmT", [128, 8, 256], BF)
    for t in range(2):
        norm.run(ctx[t * 128:(t + 1) * 128, :], 128,
                 lambda kk, t=t: cmT[:, kk, t * 128:(t + 1) * 128],
                 lambda kk: AB[:, 0, kk, 1:2], lambda kk: modT[:, kk, 1:2])
    acT = k.sb("acT", [128, 2, 256])
    qcT = k.sb("qcT", [128, 6, 256], BF)
    kcT = k.sb("kcT", [128, 6, 256], BF)
    Vc = k.sb("Vc", [128, 2, 768], BF)
    for oc in range(14):
        p = nextA()
        for kk in range(8):
            k.mm(p[:, 0:256], winb[:, kk, oc * 128:(oc + 1) * 128], cmT[:, kk, :],
                 start=(kk == 0), stop=(kk == 7))
        if oc < 2:
            evac(acT[:, oc, :], p[:, 0:256], oc)
        elif oc < 8:
            evac(qcT[:, oc - 2, :], p[:, 0:256], oc, scale=0.125)
        else:
            evac(kcT[:, oc - 8, :], p[:, 0:256], oc)
    for t in range(2):
        for half in range(2):
            p = nextA()
            for kk in range(8):
                k.mm(p[:, 0:384], cmT[:, kk, t * 128:(t + 1) * 128],
                     winb[:, kk, 1792 + half * 384:1792 + (half + 1) * 384],
                     start=(kk == 0), stop=(kk == 7))
            evac(Vc[:, t, half * 384:(half + 1) * 384], p[:, 0:384], half)
    if phase < 3:
        return k, k.finish()
    cst = k.sb("cst", [128, 2, 128])
    k.dma(cst[:, 0, :], c64b)
    k.dma(cst[:, 1, :], s64b)
    c2s = k.sb("c2s", [128, 2, 2, 256])
    k.dma(c2s[:, 0, :, :], c256.rearrange("(t p) n -> p t n", p=128))
    k.dma(c2s[:, 1, :, :], ns256.rearrange("(t p) n -> p t n", p=128))
    aCS = k.sb("aCS", [128, 2, 2, 256])
    for which in range(2):
        for t in range(2):
            for cch in range(2):
                p = nextA()
                k.mm(p[:, 0:128], acT[:, cch, t * 128:(t + 1) * 128], cst[:, which, :])
                evac(aCS[:, which, t, cch * 128:(cch + 1) * 128], p[:, 0:128], cch)
    mcs = k.sb("mcs", [128, 8, 256])
    for cch in range(2):
        p = nextA()
        n = 0
        for which in range(2):
            for t in range(2):
                k.mm(p[:, 0:256], aCS[:, which, t, cch * 128:(cch + 1) * 128], c2s[:, which, t, :],
                     start=(n == 0), stop=(n == 3))
                n += 1
        evac(mcs[:, cch, :], p[:, 0:256], cch)
    if phase < 4:
        return k, k.finish()
    units = []
    for t in range(2):
        for h in range(12):
            hp, hc = 64 * (h % 2), h // 2
            units.append((qcT[hp:hp + 64, hc, t * 128:(t + 1) * 128], None, 0, None, None,
                          kcT[hp:hp + 64, hc, :], None,
                          (lambda idx, h=h: Vc[:, idx, h * 64:(h + 1) * 64]), h,
                          mcs[hp:hp + 64, 2 + hc, t * 128:(t + 1) * 128]))
    run_units(units)
    k.dma(mcT.rearrange("(k p) n -> p k n", p=128), mcs[:], q="sp")

    if phase < 5:
        return k, k.finish()
    xmT = k.sb("xmT", [128, 8, 15, 64], BF)
    xmK = k.sb("xmK", [128, 8, 512], BF)
    qT = k.sb("qT", [128, 6, 4, 8, 16], BF)
    kT = k.sb("kT", [128, 6, 15, 64], BF)
    Vt = k.sb("Vt", [128, 4, 768], BF)
    aTs = k.sb("aTs", [128, 2, 512])
    oTs = k.sb("oTs", [128, 6, 8, 64], BF)
    maskst = k.sb("maskst", [128, 480])
    maskb = k.sb("maskb", [128, 4, 480], BF)
    rb = [k.sb("rb%d" % i, [128, 480]) for i in range(3)]
    xmTf = xmT[:].rearrange("p k a b -> p k (a b)")
    kTf = kT[:].rearrange("p k a b -> p k (a b)")
    for rg in range(4 if phase > 5 else 1):
        R0 = rg * 8
        for ti in range(8):
            n = 128 if ti < 7 else 64
            norm.run(xh[R0 * 64 + ti * 128: R0 * 64 + ti * 128 + n, :], n,
                     lambda kk, ti=ti, n=n: xmTf[:, kk, ti * 128: ti * 128 + n],
                     lambda kk: AB[:, 0, kk, 0:1], lambda kk: modT[:, kk, 0:1])
        for j in range(4):
            k.dma(maskst[:], mask[rg, j], q="act")
            k.copy(maskb[:, j, :], maskst[:], eng="act")
        for oc in range(8):
            p = nextA()
            for kk in range(8):
                k.mm(p[:, :], winb[:, kk, oc * 128:(oc + 1) * 128], xmTf[:, kk, 256:768],
                     start=(kk == 0), stop=(kk == 7))
            if oc < 2:
                evac(aTs[:, oc, :], p[:, :], oc)
            else:
                pv = p[:, :].rearrange("p (r j c) -> p r j c", r=8, j=4)
                ov = qT[:, oc - 2, :, :, :].rearrange("p j r c -> p r j c")
                evac(ov, pv, oc, scale=0.125)
        k.dma(aT.rearrange("(k p) n -> p k n", p=128)[:, :, rg * 512:(rg + 1) * 512], aTs[:], q="sp")
        for oc in range(6):
            for half in range(2):
                p = nextA()
                for kk in range(8):
                    k.mm(p[:, 0:480], winb[:, kk, 1024 + oc * 128:1024 + (oc + 1) * 128],
                         xmTf[:, kk, half * 480:(half + 1) * 480], start=(kk == 0), stop=(kk == 7))
                evac(kTf[:, oc, half * 480:(half + 1) * 480], p[:, 0:480], half)
        u = 0
        for j in range(4):
            cs_ = KEY_START[j]
            k.copy(xmK[:, :, 0:480].rearrange("p k (a b) -> p k a b", b=32),
                   xmT[:, :, :, cs_:cs_ + 32], eng=("dve" if j % 2 == 0 else "act"))
            for rc in range(4):
                nk = 128 if rc < 3 else 96
                for half in range(2):
                    p = nextA()
                    for kk in range(8):
                        k.mm(p[:nk, 0:384], xmK[:, kk, rc * 128: rc * 128 + nk],
                             winb[:, kk, 1792 + half * 384:1792 + (half + 1) * 384],
                             start=(kk == 0), stop=(kk == 7))
                    evac(Vt[:nk, rc, half * 384:(half + 1) * 384], p[:nk, 0:384], half)
            units = []
            for h in range(12):
                hp, hc = 64 * (h % 2), h // 2
                r = rb[u % 3]
                u += 1

                def bias_fn(r=r, h=h, j=j):
                    k.dma(r[:], rpbg[h, j], q="sp")
                    return r[:]
                units.append((qT[hp:hp + 64, hc, j, :, :].rearrange("p r c -> p (r c)"),
                              kT[hp:hp + 64, hc, :, cs_:cs_ + 32], 480, maskb[:, j, :], bias_fn,
                              kcT[hp:hp + 64, hc, :],
                              (lambda idx, kn, h=h: Vt[:kn, idx, h * 64:(h + 1) * 64]),
                              (lambda idx, h=h: Vc[:, idx, h * 64:(h + 1) * 64]), h,
                              oTs[hp:hp + 64, hc, :, j * 16:(j + 1) * 16]))
            run_units(units)
        k.dma(oT.rearrange("(k p) (g n) -> p k g n", p=128, g=4)[:, :, rg, :],
              oTs[:].rearrange("p k a b -> p k (a b)"), q="sp")
    st = k.finish()
    return k, st


def build_l0b():
    k = KB()
    X = k.din("X", [128, 32, 128])
    cs1 = k.din("cs1", [128, 256])
    tw = k.din("tw", [128, 2, 128])
    cs2 = k.din("cs2", [128, 3, 128])
    U = k.dout("U", [128, 2, 32, 128])
    Xs = k.sb("Xs", [128, 32, 128])
    k.dma(Xs[:, 0:16, :], X[:, 0:16, :], q="sp")
    k.dma(Xs[:, 16:32, :], X[:, 16:32, :], q="act")
    c1 = k.sb("c1", [128, 256]); k.dma(c1[:], cs1)
    tws = k.sb("tws", [128, 2, 128]); k.dma(tws[:], tw)
    c2 = k.sb("c2", [128, 3, 128]); k.dma(c2[:], cs2)
    Bs = k.sb("Bs", [128, 2, 32, 128])
    t = [k.sb("dt%d" % i, [128, 128]) for i in range(4)]
    ps = [k.ps("dps%d" % i, [128, 512]) for i in range(4)]
    for ch in range(32):
        p = ps[ch % 2]
        k.mm(p[:, 0:256], Xs[:, ch, :], c1[:])
        Ar, Ai = p[:, 0:128], p[:, 128:256]
        k.tt(t[0][:], Ar, tws[:, 0, :], ALU.mult)
        k.tt(t[1][:], Ai, tws[:, 1, :], ALU.mult)
        k.tt(Bs[:, 0, ch, :], t[0][:], t[1][:], ALU.add)
        k.tt(t[2][:], Ai, tws[:, 0, :], ALU.mult)
        k.tt(t[3][:], Ar, tws[:, 1, :], ALU.mult)
        k.tt(Bs[:, 1, ch, :], t[2][:], t[3][:], ALU.subtract)
    Us = k.sb("Us", [128, 2, 32, 128])
    for blk in range(8):
        sl = slice(blk * 4, blk * 4 + 4)
        pr = ps[2]; pi = ps[3]
        br = Bs[:, 0, sl, :].rearrange("p a b -> p (a b)")
        bi = Bs[:, 1, sl, :].rearrange("p a b -> p (a b)")
        k.mm(pr[:], c2[:, 0, :], br, start=True, stop=False)
        k.mm(pr[:], c2[:, 1, :], bi, start=False, stop=True)
        k.mm(pi[:], c2[:, 0, :], bi, start=True, stop=False)
        k.mm(pi[:], c2[:, 2, :], br, start=False, stop=True)
        k.copy(Us[:, 0, sl, :].rearrange("p a b -> p (a b)"), pr[:], eng="dve")
        k.copy(Us[:, 1, sl, :].rearrange("p a b -> p (a b)"), pi[:], eng="act")
    k.dma(U[:, 0, :, :], Us[:, 0, :, :], q="sp")
    k.dma(U[:, 1, :, :], Us[:, 1, :, :], q="act")
    return k, k.finish()


def l0b_consts():
    n = np.arange(128)
    ang = 2 * np.pi * np.outer(n, n) / 128
    cs1 = np.concatenate([np.cos(ang), -np.sin(ang)], axis=1).astype(np.float32)
    ang2 = 2 * np.pi * np.outer(n, n) / 16384
    tw = np.stack([np.cos(ang2), np.sin(ang2)], axis=1).astype(np.float32)
    sc = 1.0 / 1024.0
    cs2 = np.stack([np.cos(ang) * sc, np.sin(ang) * sc, -np.sin(ang) * sc], axis=1).astype(np.float32)
    return dict(cs1=cs1, tw=tw, cs2=cs2)


def l0b_inputs(aT_full, core, consts):
    X = aT_full[32 * core:32 * core + 32].reshape(32, 128, 128).transpose(1, 0, 2)
    d = dict(consts)
    d["X"] = np.ascontiguousarray(X)
    return d


class Post:
    def __init__(self, k, NT, ncol_of, idf, modT, AB, T0, pbank, psR, hs, actT, wbuf, stg, Gb,
                 rw, rbb, w1, w3, w2, Gsrc):
        self.k = k
        self.NT = NT
        self.col_of = ncol_of
        self.idf, self.modT, self.AB = idf, modT, AB
        self.T0, self.pbank, self.psR = T0, pbank, psR
        self.hs, self.actT, self.wbuf, self.stg, self.Gb = hs, actT, wbuf, stg, Gb
        self.w1, self.w3, self.w2, self.Gsrc = w1, w3, w2, Gsrc
        self.xs = k.sb("p_xs", [128, 1024])
        self.junk = k.sb("p_junk", [128, 1024], BF)
        self.st = k.sb("p_st", [128, 4])
        self.xm2f = k.sb("p_xm2f", [128, 8, 128])
        self.gate = k.sb("p_gate", [128, NT, 16])
        self.rws = k.sb("p_rws", [128, 8, 16])
        k.dma(self.rws[:], rw.rearrange("(k p) n -> p k n", p=128))
        self.rbs = k.sb("p_rbs", [128, 16])
        k.dma(self.rbs[:], rbb)
        self.r = k.sb("p_r", [128, 8, 16])
        self.pr = k.sb("p_pr", [128, 4, 6])
        self.rs = k.sb("p_rs", [128, 8])
        self.hid = k.sb("p_hid", [128, 4, 512], BF)
        self.s1 = [k.sb("p_s1%d" % i, [128, 512]) for i in range(2)]

    def norm_router(self, ti):
        k = self.k
        col = self.col_of(ti)
        h = self.hs[:, ti, :]
        st = self.st
        k.act(self.junk[:], h, AF.Square, accum_out=st[:, 0:1])
        k.ts(st[:, 1:2], st[:, 0:1], 1.0 / 1024.0, ALU.mult, 1e-6, ALU.add)
        k.act(st[:, 2:3], st[:, 1:2], AF.Sqrt)
        k.recip(st[:, 3:4], st[:, 2:3])
        k.ts(self.xs[:], h, st[:, 3:4], ALU.mult)
        for kk in range(8):
            k.tr(self.T0[:, kk * 128:(kk + 1) * 128], self.xs[:, kk * 128:(kk + 1) * 128], self.idf[:])
        for kk in range(8):
            src = self.T0[:, kk * 128:(kk + 1) * 128]
            A = self.AB[:, 1, kk, col:col + 1]
            B = self.modT[:, 24 + kk, col:col + 1]
            if kk < 4:
                k.ts(self.xm2f[:, kk, :], src, A, ALU.mult, B, ALU.add)
            else:
                k.act(self.xm2f[:, kk, :], src, AF.Identity, bias=B, scale=A)
        k.copy(self.actT[:, :, ti * 128:(ti + 1) * 128], self.xm2f[:], eng=("dve" if ti % 2 == 0 else "act"))
        pR = self.psR[:, 0:16]
        for kk in range(8):
            k.mm(pR, self.xm2f[:, kk, :], self.rws[:, kk, :], start=(kk == 0), stop=(kk == 7))
        r = self.r
        sc, bi, mb, m1, tmp, sel = (r[:, i, :] for i in range(6))
        k.act(sc, pR, AF.Sigmoid)
        k.tt(bi, sc, self.rbs[:], ALU.add)
        b4 = r[:, 1, :].rearrange("p (g i) -> p g i", i=4)
        pr = self.pr
        k.tt(pr[:, :, 0:3], b4[:, :, 0:3], b4[:, :, 1:4], ALU.add)
        k.tt(pr[:, :, 3:5], b4[:, :, 0:2], b4[:, :, 2:4], ALU.add)
        k.tt(pr[:, :, 5:6], b4[:, :, 0:1], b4[:, :, 3:4], ALU.add)
        rs = self.rs
        k.reduce(rs[:, 0:4], pr[:], ALU.max)
        k.reduce(rs[:, 4:5], rs[:, 0:4], ALU.max)
        k.ts(rs[:, 0:4], rs[:, 0:4], rs[:, 4:5], ALU.is_ge)
        mb4 = r[:, 2, :].rearrange("p (g i) -> p g i", i=4)
        for i in range(4):
            k.ts(mb4[:, :, i], rs[:, 0:4], 1.0, ALU.subtract, 1.0e4, ALU.mult)
        k.tt(mb, mb, bi, ALU.add)
        k.reduce(rs[:, 5:6], mb, ALU.max)
        k.ts(m1, mb, rs[:, 5:6], ALU.is_ge)
        k.ts(tmp, m1, -1.0e4, ALU.mult)
        k.tt(tmp, tmp, mb, ALU.add)
        k.reduce(rs[:, 6:7], tmp, ALU.max)
        k.ts(sel, mb, rs[:, 6:7], ALU.is_ge)
        k.tt(sel, sel, sc, ALU.mult)
        k.reduce(rs[:, 7:8], sel, ALU.add)
        k.recip(rs[:, 7:8], rs[:, 7:8])
        k.ts(self.gate[:, ti, :], sel, rs[:, 7:8], ALU.mult)

    def moe(self, blocks):
        k = self.k
        wb = self.wbuf
        w1b = wb[:, 0:4096].rearrange("p (a b) -> p a b", b=512)
        w3b = wb[:, 4096:8192].rearrange("p (a b) -> p a b", b=512)
        w2b = [wb[:, 8192:12288].rearrange("p (a b) -> p a b", b=1024),
               wb[:, 12288:16384].rearrange("p (a b) -> p a b", b=1024)]
        ncols = sorted(set(b[2] for b in blocks))
        for c in ncols:
            k.dma(self.Gb[:, c, :], self.Gsrc[:, c, 1, :], q="act")
        pb = self.pbank
        si = 0
        for e in range(16):
            for wi, (wsrc, wdst) in enumerate(((self.w1, w1b), (self.w3, w3b))):
                for hf in range(2):
                    s_ = self.stg[si % 2]
                    si += 1
                    k.dma(s_[:], wsrc[e].rearrange("(k p) n -> p k n", p=128)[:, hf * 4:(hf + 1) * 4, :],
                          q=("sp" if si % 2 == 0 else "act"))
                    k.copy(wdst[:, hf * 4:(hf + 1) * 4, :], s_[:], eng=("dve" if si % 2 == 0 else "act"))
            for hf in range(2):
                s_ = self.stg[si % 2]
                si += 1
                k.dma(s_[:], self.w2[e].rearrange("(k p) n -> p k n", p=128)[:, :, hf * 512:(hf + 1) * 512],
                      q=("sp" if si % 2 == 0 else "act"))
                for c in ncols:
                    for fc in range(4):
                        k.tt(w2b[c][:, fc, hf * 512:(hf + 1) * 512], s_[:, fc, :],
                             self.Gb[:, c, hf * 512:(hf + 1) * 512], ALU.mult)
            for bi_, (tok0, ntok, col, tiles) in enumerate(blocks):
                for fc in range(4):
                    p1 = pb[(2 * fc) % 4]
                    p3 = pb[(2 * fc + 1) % 4]
                    for kk in range(8):
                        k.mm(p1[:, 0:ntok], w1b[:, kk, fc * 128:(fc + 1) * 128],
                             self.actT[:, kk, tok0:tok0 + ntok], start=(kk == 0), stop=(kk == 7))
                    for kk in range(8):
                        k.mm(p3[:, 0:ntok], w3b[:, kk, fc * 128:(fc + 1) * 128],
                             self.actT[:, kk, tok0:tok0 + ntok], start=(kk == 0), stop=(kk == 7))
                    s1 = self.s1[fc % 2]
                    k.act(s1[:, 0:ntok], p1[:, 0:ntok], AF.Silu)
                    k.tt(self.hid[:, fc, 0:ntok], s1[:, 0:ntok], p3[:, 0:ntok], ALU.mult)
                for tl, ti in enumerate(tiles):
                    for half in range(2):
                        pY = self.T0[:, half * 512:(half + 1) * 512]
                        for fc in range(4):
                            k.mm(pY, self.hid[:, fc, tl * 128:(tl + 1) * 128],
                                 w2b[col][:, fc, half * 512:(half + 1) * 512],
                                 start=(fc == 0), stop=(fc == 3))
                        hsl = self.hs[:, ti, half * 512:(half + 1) * 512]
                        k.stt(hsl, pY, self.gate[:, ti, e:e + 1], hsl, ALU.mult, ALU.add)


def build_l0c():
    k = KB()
    x = k.din("x", [2048, 1024])
    ctx = k.din("ctx", [256, 1024])
    UT = k.din("UT", [2, 256, 2048])
    oT = k.din("oT", [768, 2048], BF)
    mcT = k.din("mcT", [1024, 256])
    Gsrc = k.din("Gsrc", [128, 2, 2, 1024])
    modT_i = k.din("modT", [128, 48, 2])
    ng = k.din("ng", [128, 2, 8])
    wout = k.din("wout", [1024, 1024])
    c64b = k.din("c64b", [128, 128])
    s64b = k.din("s64b", [128, 128])
    identf = k.din("identf", [128, 128])
    rw = k.din("rw", [1024, 16])
    rbb = k.din("rbb", [128, 16])
    w1 = k.din("w1", [16, 1024, 512])
    w3 = k.din("w3", [16, 1024, 512])
    w2 = k.din("w2", [16, 512, 1024])
    h_o = k.dout("h", [2048, 1024])
    hc_o = k.dout("hc", [256, 1024])
    NT = 18

    idf = k.sb("idf", [128, 128]); k.dma(idf[:], identf)
    modT = k.sb("modTs", [128, 48, 2]); k.dma(modT[:], modT_i)
    ngs = k.sb("ngs", [128, 2, 8]); k.dma(ngs[:], ng)
    AB = k.sb("AB", [128, 2, 8, 2])
    tmp8 = k.sb("tmp8", [128, 8])
    for col in range(2):
        k.ts(tmp8[:], modT[:, 32:40, col], 1.0, ALU.add)
        k.tt(AB[:, 1, :, col], tmp8[:], ngs[:, 1, :], ALU.mult)
    T0 = k.ps("T0", [128, 1024])
    pbank = [k.ps("pb%d" % i, [128, 512]) for i in range(4)]
    psR = k.ps("psR", [128, 512])
    hs = k.sb("hs", [128, NT, 1024])
    actT = k.sb("actT", [128, 8, NT * 128], BF)
    wbuf = k.sb("wbuf", [128, 16384], BF)
    stg = [k.sb("stg%d" % i, [128, 4, 512]) for i in range(2)]
    Gb = k.sb("Gb", [128, 2, 1024])
    cst = k.sb("cst", [128, 2, 128])
    k.dma(cst[:, 0, :], c64b); k.dma(cst[:, 1, :], s64b)
    for c in range(2):
        k.dma(Gb[:, c, :], Gsrc[:, c, 0, :], q="act")
    k.dma(actT[:, 2:8, 0:2048], oT.rearrange("(k p) n -> p k n", p=128), q="sp")
    UTv = UT.rearrange("r (c p) n -> p r c n", p=128)
    for blk in range(4):
        s_ = stg[blk % 2]
        sv = s_[:].rearrange("p (r c) n -> p r c n", r=2)
        k.dma(sv, UTv[:, :, :, blk * 512:(blk + 1) * 512], q="sp")
        for cch in range(2):
            p = pbank[cch]
            k.mm(p[:], cst[:, 0, :], sv[:, 0, cch, :], start=True, stop=False)
            k.mm(p[:], cst[:, 1, :], sv[:, 1, cch, :], start=False, stop=True)
            k.copy(actT[:, cch, blk * 512:(blk + 1) * 512], p[:], eng=("dve" if cch == 0 else "act"))
    mcv = mcT.rearrange("(k p) n -> p k n", p=128)
    for hf in range(2):
        s_ = stg[hf % 2]
        k.dma(s_[:, :, 0:256], mcv[:, hf * 4:(hf + 1) * 4, :], q="act")
        k.copy(actT[:, hf * 4:(hf + 1) * 4, 2048:2304], s_[:, :, 0:256], eng="dve")
    woutb = wbuf[:, 0:8192].rearrange("p (a b) -> p a b", b=1024)
    wv = wout.rearrange("(k p) n -> p k n", p=128)
    for q4 in range(4):
        s_ = stg[q4 % 2]
        k.dma(s_[:].rearrange("p (a c) n -> p a (c n)", a=2), wv[:, q4 * 2:(q4 + 1) * 2, :], q="sp")
        k.copy(woutb[:, q4 * 2:(q4 + 1) * 2, :], s_[:].rearrange("p (a c) n -> p a (c n)", a=2),
               eng=("dve" if q4 % 2 == 0 else "act"))
    post = Post(k, NT, lambda ti: 0 if ti < 16 else 1, idf, modT, AB, T0, pbank, psR, hs, actT, wbuf, stg, Gb,
                rw, rbb, w1, w3, w2, Gsrc)
    xt = [k.sb("xt%d" % i, [128, 1024]) for i in range(2)]
    for ti in range(NT):
        col = 0 if ti < 16 else 1
        src = x[ti * 128:(ti + 1) * 128, :] if ti < 16 else ctx[(ti - 16) * 128:(ti - 15) * 128, :]
        t = xt[ti % 2]
        k.dma(t[:], src, q="sp")
        for half in range(2):
            pY = T0[:, half * 512:(half + 1) * 512]
            for kk in range(8):
                k.mm(pY, actT[:, kk, ti * 128:(ti + 1) * 128], woutb[:, kk, half * 512:(half + 1) * 512],
                     start=(kk == 0), stop=(kk == 7))
            hsl = hs[:, ti, half * 512:(half + 1) * 512]
            k.tt(hsl, pY, Gb[:, col, half * 512:(half + 1) * 512], ALU.mult)
            k.tt(hsl, hsl, t[:, half * 512:(half + 1) * 512], ALU.add)
        post.norm_router(ti)
    blocks = [(b * 512, 512, 0, [4 * b + i for i in range(4)]) for b in range(4)]
    blocks.append((2048, 256, 1, [16, 17]))
    post.moe(blocks)
    for ti in range(NT):
        dst = h_o[ti * 128:(ti + 1) * 128, :] if ti < 16 else hc_o[(ti - 16) * 128:(ti - 15) * 128, :]
        k.dma(dst, hs[:, ti, :], q=("sp" if ti % 2 == 0 else "act"))
    return k, k.finish()


def build_l1a(nchunks=130):
    k = KB()
    hseq = k.din("hseq", [16640, 1024])
    cc = k.din("cc", [128, 8, 2])
    adaw = k.din("adaw", [1024, 6144])
    adab = k.din("adab", [128, 48])
    ng = k.din("ng", [128, 2, 8])
    wq = k.din("wq", [1024, 256])
    wk = k.din("wk", [1024, 256])
    wv = k.din("wv", [1024, 512])
    dec = k.din("dec", [128, 1])
    iota1 = k.din("iota1", [128, 128])
    kpos = k.din("kpos", [128, 1])
    diffm = k.din("diffm", [128, 128])
    tri = k.din("tri", [128, 128])
    rowcs = k.din("rowcs", [128, 2, 256])
    colcs = k.din("colcs", [128, 2, 128])
    identf = k.din("identf", [128, 128])
    o = k.dout("o", [16384, 512])
    modT_o = k.dout("modT", [128, 48, 2])

    idf = k.sb("idf", [128, 128]); k.dma(idf[:], identf)
    idb = k.sb("idb", [128, 128], BF); k.copy(idb[:], idf[:])
    T0 = k.ps("T0", [128, 1024])
    pQ = k.ps("pQ", [128, 512])
    pK = k.ps("pK", [128, 512])
    pV = k.ps("pV", [128, 512])
    pO = k.ps("pO", [128, 512])
    pS = k.ps("pS", [128, 128])
    pT = k.ps("pT", [128, 256], BF)
    modT, AB = emit_mod(k, adaw, adab, ng, cc, pQ)
    k.dma(modT_o, modT[:], q="sp")

    stg = k.sb("stg", [128, 8, 512])
    wqb = k.sb("wqb", [128, 8, 256], BF)
    wqr = k.sb("wqr", [128, 8, 256], BF)
    wkb = k.sb("wkb", [128, 8, 256], BF)
    wkr = k.sb("wkr", [128, 8, 256], BF)
    wvb = k.sb("wvb", [128, 8, 512], BF)
    for src, dst, rot, sc in ((wq, wqb, wqr, 1.0), (wk, wkb, wkr, 0.0625)):
        k.dma(stg[:, :, 0:256], src.rearrange("(k p) n -> p k n", p=128), q="sp")
        k.ts(dst[:], stg[:, :, 0:256], sc, ALU.mult)
        for hf in range(2):
            b = hf * 128
            k.ts(rot[:, :, b:b + 64], stg[:, :, b + 64:b + 128], -sc, ALU.mult)
            k.ts(rot[:, :, b + 64:b + 128], stg[:, :, b:b + 64], sc, ALU.mult)
    k.dma(stg[:], wv.rearrange("(k p) n -> p k n", p=128), q="sp")
    k.copy(wvb[:], stg[:], eng="act")

    d = k.sb("dcy", [128, 8])
    k.dma(d[:, 0:1], dec)
    k.act(d[:, 1:2], d[:, 0:1], AF.Exp)
    k.ts(d[:, 2:3], d[:, 1:2], -1.0, ALU.mult, 1.0, ALU.add)
    k.act(d[:, 3:4], d[:, 2:3], AF.Ln)
    lg = d[:, 3:4]
    cst = k.sb("cst", [128, 4, 128])
    k.dma(cst[:, 0, :], iota1); k.dma(cst[:, 1, :], diffm); k.dma(cst[:, 2, :], tri)
    kps = k.sb("kps", [128, 1]); k.dma(kps[:], kpos)
    QD = k.sb("QD", [128, 128])
    k.act(QD[:], cst[:, 0, :], AF.Exp, scale=lg)
    DT = k.sb("DT", [128, 128])
    k.act(DT[:], cst[:, 1, :], AF.Exp, scale=lg)
    k.tt(DT[:], DT[:], cst[:, 2, :], ALU.mult)
    k.act(d[:, 4:5], kps[:], AF.Exp, scale=lg)
    KD = d[:, 4:5]
    k.ts(d[:, 5:6], lg, 128.0, ALU.mult)
    k.act(d[:, 6:7], d[:, 5:6], AF.Exp)
    CD = d[:, 6:7]
    rcs = k.sb("rcs", [128, 2, 256]); k.dma(rcs[:], rowcs)
    ccs = k.sb("ccs", [128, 2, 128]); k.dma(ccs[:], colcs)

    norm = NormT(k, idf, T0)
    xmT = [k.sb("xmT%d" % i, [128, 8, 128], BF) for i in range(2)]
    qTr = [k.sb("qTr%d" % i, [128, 2, 128], BF) for i in range(2)]
    kTr = [k.sb("kTr%d" % i, [128, 2, 128], BF) for i in range(2)]
    qd = [k.sb("qd%d" % i, [128, 2, 128], BF) for i in range(2)]
    tq = [k.sb("tq%d" % i, [128, 128]) for i in range(2)]
    vb = [k.sb("vb%d" % i, [128, 512], BF) for i in range(2)]
    kdec = [k.sb("kdec%d" % i, [128, 256], BF) for i in range(2)]
    sTd = k.sb("sTd", [128, 128], BF)
    ob = [k.sb("ob%d" % i, [128, 512]) for i in range(2)]
    Sf = k.sb("Sf", [128, 2, 512])
    Sb = k.sb("Sb", [128, 2, 512], BF)
    k.memset(Sf[:], 0.0)
    k.memset(Sb[:], 0.0)

    def rope(dst, P, gi0):
        for g in range(2):
            sl = slice(g * 64, (g + 1) * 64)
            gi = gi0 + g
            k.ts(tq[0][:, sl], P[:, 0:128][:, sl], rcs[:, 0, gi:gi + 1], ALU.mult)
            k.stt(dst[:, 0, sl], P[:, 256:384][:, sl], rcs[:, 1, gi:gi + 1], tq[0][:, sl], ALU.mult, ALU.add)
        k.tt(tq[0][:], P[:, 128:256], ccs[:, 0, :], ALU.mult)
        k.tt(tq[1][:], P[:, 384:512], ccs[:, 1, :], ALU.mult)
        k.tt(dst[:, 1, :], tq[0][:], tq[1][:], ALU.add)

    def proj(c):
        b = c % 2
        is_ctx = c < 2
        col = 1 if is_ctx else 0
        xm = xmT[b]
        norm.run(hseq[c * 128:(c + 1) * 128, :], 128, lambda kk, xm=xm: xm[:, kk, :],
                 lambda kk, col=col: AB[:, 0, kk, col:col + 1], lambda kk, col=col: modT[:, kk, col:col + 1])
        for P, wa, wr in ((pQ, wqb, wqr), (pK, wkb, wkr)):
            for part, w in enumerate((wa, wr)):
                if is_ctx and part == 1:
                    continue
                for oc in range(2):
                    out = P[:, part * 256 + oc * 128: part * 256 + (oc + 1) * 128]
                    for kk in range(8):
                        k.mm(out, w[:, kk, oc * 128:(oc + 1) * 128], xm[:, kk, :], start=(kk == 0), stop=(kk == 7))
        for kk in range(8):
            k.mm(pV[:], xm[:, kk, :], wvb[:, kk, :], start=(kk == 0), stop=(kk == 7))
        if is_ctx:
            k.copy(qTr[b][:].rearrange("p a b -> p (a b)"), pQ[:, 0:256], eng="dve")
            k.copy(kTr[b][:].rearrange("p a b -> p (a b)"), pK[:, 0:256], eng="dve")
        else:
            gi0 = 2 * (c - 2)
            rope(qTr[b], pQ, gi0)
            rope(kTr[b], pK, gi0)
        k.copy(vb[b][:], pV[:], eng="act")
        for dc in range(2):
            k.tr(pT[:, dc * 128:(dc + 1) * 128], kTr[b][:, dc, :], idb[:])
        k.ts(kdec[b][:], pT[:], KD, ALU.mult)
        if not is_ctx:
            for dc in range(2):
                k.tt(qd[b][:, dc, :], qTr[b][:, dc, :], QD[:], ALU.mult)

    def scan(c):
        b = c % 2
        is_ctx = c < 2
        if not is_ctx:
            for dc in range(2):
                k.mm(pS[:], kTr[b][:, dc, :], qTr[b][:, dc, :], start=(dc == 0), stop=(dc == 1))
            k.tt(sTd[:], pS[:], DT[:], ALU.mult)
            k.mm(pO[:], sTd[:], vb[b][:], start=True, stop=False)
            for dc in range(2):
                k.mm(pO[:], qd[b][:, dc, :], Sb[:, dc, :], start=False, stop=(dc == 1))
            obt = ob[c % 2]
            k.copy(obt[:], pO[:], eng="act")
            k.dma(o[(c - 2) * 128:(c - 1) * 128, :], obt[:], q="act")
        for dc in range(2):
            k.mm(pO[:], kdec[b][:, dc * 128:(dc + 1) * 128], vb[b][:], start=True, stop=True)
            k.stt(Sf[:, dc, :], Sf[:, dc, :], CD, pO[:], ALU.mult, ALU.add)
            k.copy(Sb[:, dc, :], Sf[:, dc, :], eng="act")

    def capture(fn, *a):
        n0 = len(k.S.ops)
        fn(*a)
        lst = k.S.ops[n0:]
        del k.S.ops[n0:]
        return lst

    def merge(A, B):
        out = []
        ia = ib = 0
        na, nb = len(A), len(B)
        while ia < na or ib < nb:
            if ib >= nb or (ia < na and ia * nb <= ib * na):
                out.append(A[ia]); ia += 1
            else:
                out.append(B[ib]); ib += 1
        return out

    proj(0)
    for c in range(nchunks):
        A = capture(proj, c + 1) if c + 1 < nchunks else []
        B = capture(scan, c)
        k.S.ops.extend(merge(A, B))
    return k, k.finish()


def rope_tables(rows, cols):
    p = np.arange(128)
    inv = 10000.0 ** (-(p % 64).astype(np.float64) / 64.0)
    ar = inv[:, None] * np.asarray(rows, np.float64)[None, :]
    ac = inv[:, None] * np.asarray(cols, np.float64)[None, :]
    rowcs = np.stack([np.cos(ar), np.sin(ar)], axis=1).astype(np.float32)
    colcs = np.stack([np.cos(ac), np.sin(ac)], axis=1).astype(np.float32)
    return rowcs, colcs


def l1a_inputs(inp, core, h_all, hc):
    hd, dr = core % 4, core // 4
    if dr == 0:
        seq = np.concatenate([hc, h_all], axis=0)
        rows = np.arange(256)
        cols = np.concatenate([np.arange(64), np.arange(64)])
    else:
        seq = np.concatenate([hc[::-1], h_all[::-1]], axis=0)
        rows = np.arange(256)[::-1]
        cols = np.concatenate([np.arange(64)[::-1], np.arange(64)[::-1]])
    rowcs, colcs = rope_tables(rows, cols)
    w = inp['ret_w_in'][0]
    n = np.arange(128)
    cc = np.stack([lay_vec(inp['c'][0]), lay_vec(inp['c_ctx'])], axis=-1)
    ng = np.stack([lay_vec(inp['norm_g'][1, 0]), lay_vec(inp['norm_g'][1, 1])], axis=1)
    diff = (n[None, :] - n[:, None]).astype(np.float32)
    return dict(hseq=np.ascontiguousarray(seq), cc=np.ascontiguousarray(cc), adaw=np.ascontiguousarray(inp['ada_w'][1]),
                adab=lay_vec(inp['ada_b'][1]), ng=np.ascontiguousarray(ng),
                wq=np.ascontiguousarray(w[:, hd * 256:(hd + 1) * 256]),
                wk=np.ascontiguousarray(w[:, 1024 + hd * 256:1024 + (hd + 1) * 256]),
                wv=np.ascontiguousarray(w[:, 2048 + hd * 512:2048 + (hd + 1) * 512]),
                dec=np.full((128, 1), inp['ret_decay'][0, dr, hd], np.float32),
                iota1=np.ascontiguousarray(np.broadcast_to((n + 1).astype(np.float32)[None, :], (128, 128))),
                kpos=(127 - n).astype(np.float32).reshape(128, 1),
                diffm=np.maximum(diff, 0.0), tri=(diff >= 0).astype(np.float32),
                rowcs=rowcs, colcs=colcs, identf=np.eye(128, dtype=np.float32))


def build_l1b():
    k = KB()
    h0 = k.din("h0", [2048, 1024])
    of = k.din("of", [2048, 2048])
    obk = k.din("ob", [2048, 2048])
    modT_i = k.din("modT", [128, 48, 2])
    ng = k.din("ng", [128, 2, 8])
    wg = k.din("wg", [1024, 2048])
    wo = k.din("wo", [2048, 1024])
    Gsrc = k.din("Gsrc", [128, 2, 2, 1024])
    identf = k.din("identf", [128, 128])
    hmid = k.dout("hmid", [2048, 1024])
    idf = k.sb("idf", [128, 128]); k.dma(idf[:], identf)
    idb = k.sb("idb", [128, 128], BF); k.copy(idb[:], idf[:])
    modT = k.sb("modTs", [128, 48, 2]); k.dma(modT[:], modT_i)
    ngs = k.sb("ngs", [128, 2, 8]); k.dma(ngs[:], ng)
    AB = k.sb("AB", [128, 2, 8, 2])
    tmp8 = k.sb("tmp8", [128, 8])
    k.ts(tmp8[:], modT[:, 8:16, 0], 1.0, ALU.add)
    k.tt(AB[:, 0, :, 0], tmp8[:], ngs[:, 0, :], ALU.mult)
    Gb = k.sb("Gb", [128, 1024]); k.dma(Gb[:], Gsrc[:, 0, 0, :], q="act")
    T0 = k.ps("T0", [128, 1024])
    pG = [k.ps("pG%d" % i, [128, 512]) for i in range(4)]
    pTr = k.ps("pTr", [128, 2048], BF)
    stg = k.sb("stg", [128, 8, 512])
    wgb = k.sb("wgb", [128, 8, 2048], BF)
    wob = k.sb("wob", [128, 16, 1024], BF)
    wgv = wg.rearrange("(k p) n -> p k n", p=128)
    for b in range(4):
        k.dma(stg[:], wgv[:, :, b * 512:(b + 1) * 512], q="sp")
        k.copy(wgb[:, :, b * 512:(b + 1) * 512], stg[:], eng=("dve" if b % 2 == 0 else "act"))
    wov = wo.rearrange("(c p) n -> p c n", p=128)
    sv = stg[:].rearrange("p (a c) n -> p a (c n)", a=4)
    for b in range(4):
        k.dma(sv, wov[:, b * 4:(b + 1) * 4, :], q="sp")
        k.copy(wob[:, b * 4:(b + 1) * 4, :], sv, eng=("dve" if b % 2 == 0 else "act"))
    norm = NormT(k, idf, T0)
    xmT = k.sb("xmT", [128, 8, 128], BF)
    oft = [k.sb("oft%d" % i, [128, 2048]) for i in range(2)]
    obt = [k.sb("obt%d" % i, [128, 2048]) for i in range(2)]
    sg = k.sb("sg", [128, 2048])
    gated = k.sb("gated", [128, 2048], BF)
    gT = k.sb("gT", [128, 16, 128], BF)
    junk = k.sb("junk2", [128, 512], BF)
    st = k.sb("st2", [128, 16])
    xt = [k.sb("xres%d" % i, [128, 1024]) for i in range(2)]
    hm = [k.sb("hm%d" % i, [128, 1024]) for i in range(2)]
    for ti in range(16):
        rows = slice(ti * 128, (ti + 1) * 128)
        norm.run(h0[rows, :], 128, lambda kk: xmT[:, kk, :],
                 lambda kk: AB[:, 0, kk, 0:1], lambda kk: modT[:, kk, 0:1])
        a, b = oft[ti % 2], obt[ti % 2]
        k.dma(a[:], of[rows, :], q="sp")
        k.dma(b[:], obk[rows, :], q="act")
        x = xt[ti % 2]
        k.dma(x[:], h0[rows, :], q="sp")
        for blk in range(4):
            for kk in range(8):
                k.mm(pG[blk][:], xmT[:, kk, :], wgb[:, kk, blk * 512:(blk + 1) * 512],
                     start=(kk == 0), stop=(kk == 7))
            k.act(sg[:, blk * 512:(blk + 1) * 512], pG[blk][:], AF.Silu)
        k.tt(a[:], a[:], b[:], ALU.add)
        for hd in range(4):
            hs_ = slice(hd * 512, (hd + 1) * 512)
            k.act(junk[:], a[:, hs_], AF.Square, accum_out=st[:, hd:hd + 1])
        k.ts(st[:, 4:8], st[:, 0:4], 1.0 / 512.0, ALU.mult, 1e-6, ALU.add)
        k.act(st[:, 8:12], st[:, 4:8], AF.Sqrt)
        k.recip(st[:, 12:16], st[:, 8:12])
        for hd in range(4):
            hs_ = slice(hd * 512, (hd + 1) * 512)
            k.stt(gated[:, hs_], a[:, hs_], st[:, 12 + hd:13 + hd], sg[:, hs_], ALU.mult, ALU.mult)
        for c in range(16):
            k.tr(pTr[:, c * 128:(c + 1) * 128], gated[:, c * 128:(c + 1) * 128], idb[:])
        gTf = gT[:].rearrange("p a b -> p (a b)")
        k.copy(gTf[:, 0:1024], pTr[:, 0:1024], eng="dve")
        k.copy(gTf[:, 1024:2048], pTr[:, 1024:2048], eng="act")
        h = hm[ti % 2]
        for half in range(2):
            pY = T0[:, half * 512:(half + 1) * 512]
            for c in range(16):
                k.mm(pY, gT[:, c, :], wob[:, c, half * 512:(half + 1) * 512], start=(c == 0), stop=(c == 15))
            hsl = h[:, half * 512:(half + 1) * 512]
            k.tt(hsl, pY, Gb[:, half * 512:(half + 1) * 512], ALU.mult)
            k.tt(hsl, hsl, x[:, half * 512:(half + 1) * 512], ALU.add)
        k.dma(hmid[rows, :], h[:], q="act")
    return k, k.finish()


def build_l1c():
    k = KB()
    hmid = k.din("hmid", [2048, 1024])
    modT_i = k.din("modT", [128, 48, 2])
    ng = k.din("ng", [128, 2, 8])
    Gsrc = k.din("Gsrc", [128, 2, 2, 1024])
    identf = k.din("identf", [128, 128])
    rw = k.din("rw", [1024, 16])
    rbb = k.din("rbb", [128, 16])
    w1 = k.din("w1", [16, 1024, 512])
    w3 = k.din("w3", [16, 1024, 512])
    w2 = k.din("w2", [16, 512, 1024])
    fgb = k.din("fgb", [128, 1024])
    out = k.dout("out", [2048, 1024])
    NT = 16
    idf = k.sb("idf", [128, 128]); k.dma(idf[:], identf)
    modT = k.sb("modTs", [128, 48, 2]); k.dma(modT[:], modT_i)
    ngs = k.sb("ngs", [128, 2, 8]); k.dma(ngs[:], ng)
    AB = k.sb("AB", [128, 2, 8, 2])
    tmp8 = k.sb("tmp8", [128, 8])
    k.ts(tmp8[:], modT[:, 32:40, 0], 1.0, ALU.add)
    k.tt(AB[:, 1, :, 0], tmp8[:], ngs[:, 1, :], ALU.mult)
    T0 = k.ps("T0", [128, 1024])
    pbank = [k.ps("pb%d" % i, [128, 512]) for i in range(4)]
    psR = k.ps("psR", [128, 512])
    hs = k.sb("hs", [128, NT, 1024])
    actT = k.sb("actT", [128, 8, NT * 128], BF)
    wbuf = k.sb("wbuf", [128, 16384], BF)
    stg = [k.sb("stg%d" % i, [128, 4, 512]) for i in range(2)]
    Gb = k.sb("Gb", [128, 2, 1024])
    fgs = k.sb("fgs", [128, 1024]); k.dma(fgs[:], fgb, q="act")
    post = Post(k, NT, lambda ti: 0, idf, modT, AB, T0, pbank, psR, hs, actT, wbuf, stg, Gb,
                rw, rbb, w1, w3, w2, Gsrc)
    for ti in range(NT):
        k.dma(hs[:, ti, :], hmid[ti * 128:(ti + 1) * 128, :], q=("sp" if ti % 2 == 0 else "act"))
        post.norm_router(ti)
    post.moe([(b * 512, 512, 0, [4 * b + i for i in range(4)]) for b in range(4)])
    st = k.sb("fst", [128, 4])
    ot = [k.sb("fot%d" % i, [128, 1024]) for i in range(2)]
    for ti in range(NT):
        h = hs[:, ti, :]
        k.act(post.junk[:], h, AF.Square, accum_out=st[:, 0:1])
        k.ts(st[:, 1:2], st[:, 0:1], 1.0 / 1024.0, ALU.mult, 1e-6, ALU.add)
        k.act(st[:, 2:3], st[:, 1:2], AF.Sqrt)
        k.recip(st[:, 3:4], st[:, 2:3])
        o_ = ot[ti % 2]
        k.stt(o_[:], h, st[:, 3:4], fgs[:], ALU.mult, ALU.mult)
        k.dma(out[ti * 128:(ti + 1) * 128, :], o_[:], q=("sp" if ti % 2 == 0 else "act"))
    return k, k.finish()


GRID_W = 64; NA_KW = 16; NA_KEYW = 32; NA_NCB = 4; NA_KH = 8

def na_static():
    j = np.arange(NA_NCB)
    key_start = np.clip(j * NA_KW - NA_KW // 2, 0, GRID_W - NA_KEYW)
    key_cols = key_start[:, None] + np.arange(NA_KEYW)[None, :]
    q_cols = j[:, None] * NA_KW + np.arange(NA_KW)[None, :]
    win_start = np.clip(q_cols - NA_KW // 2, 0, GRID_W - NA_KW)[:, :, None]
    kc = key_cols[:, None, :]
    col_mask = (kc >= win_start) & (kc < win_start + NA_KW)
    dc_idx = np.clip(kc - q_cols[:, :, None] + NA_KW - 1, 0, 2 * NA_KW - 2)
    return key_cols, col_mask, dc_idx

def lay_vec(v):
    v = np.asarray(v, np.float32).reshape(-1, 128)
    return np.ascontiguousarray(v.T)

def l0a_inputs(inp, core):
    x = inp['x'][0]
    rows = x.reshape(256, 64, 1024)
    xh = np.zeros((39, 64, 1024), np.float32)
    r0 = core * 32 - 4
    lo, hi = max(r0, 0), min(r0 + 39, 256)
    xh[lo - r0: hi - r0] = rows[lo:hi]
    cc = np.stack([lay_vec(inp['c'][0]), lay_vec(inp['c_ctx'])], axis=-1)
    ng = np.stack([lay_vec(inp['norm_g'][0, 0]), lay_vec(inp['norm_g'][0, 1])], axis=1)
    _, col_mask, dc_idx = na_static()
    rpb = inp['na_rpb'][0]
    qr = np.arange(8)[:, None, None, None]; kr = np.arange(15)[None, None, :, None]
    dr = np.clip(kr - qr + 3, 0, 14)
    rpbg = np.empty((12, 4, 128, 480), np.float32)
    mask = np.empty((4, 4, 128, 480), np.float32)
    for j in range(4):
        dc = dc_idx[j][None, :, None, :]
        drb = np.broadcast_to(dr, (8, 16, 15, 32)); dcb = np.broadcast_to(dc, (8, 16, 15, 32))
        rpbg[:, j] = rpb[:, drb, dcb].reshape(12, 128, 480)
        cm = np.broadcast_to(col_mask[j][None, :, None, :], (8, 16, 15, 32))
        for rg in range(4):
            r = core * 32 + rg * 8 + np.arange(8)
            rs = np.clip(r - 4, 0, 248)
            keyrow = core * 32 + rg * 8 - 4 + np.arange(15)
            rm = (keyrow[None, :] >= rs[:, None]) & (keyrow[None, :] < rs[:, None] + 8)
            valid = cm & rm[:, None, :, None]
            mask[rg, j] = np.where(valid, 0.0, -30000.0).reshape(128, 480).astype(np.float32)
    a = np.arange(64)
    C64 = np.cos(2 * np.pi * np.outer(a, a) / 64); S64 = np.sin(2 * np.pi * np.outer(a, a) / 64)
    c64b = np.kron(np.eye(2), C64).astype(np.float32); s64b = np.kron(np.eye(2), S64).astype(np.float32)
    n = np.arange(256)
    c256 = (np.cos(2 * np.pi * np.outer(n, n) / 256) / 128).astype(np.float32)
    ns256 = (-np.sin(2 * np.pi * np.outer(n, n) / 256) / 128).astype(np.float32)
    return dict(xh=xh.reshape(39 * 64, 1024), ctx=np.ascontiguousarray(inp['ctx'][0]), cc=np.ascontiguousarray(cc),
                adaw=np.ascontiguousarray(inp['ada_w'][0]), adab=lay_vec(inp['ada_b'][0]), ng=np.ascontiguousarray(ng),
                win=np.ascontiguousarray(inp['mixab_w_in'][0]), rpbg=rpbg, mask=mask,
                identf=np.eye(128, dtype=np.float32), c64b=c64b, s64b=s64b, c256=c256, ns256=ns256)

def gsrc_from_modT(modT):
    rows = np.empty((2, 2, 1024), np.float32)
    for col in range(2):
        for gi, which in enumerate((2, 5)):
            rows[col, gi] = modT[:, which * 8:(which + 1) * 8, col].T.reshape(-1)
    return np.ascontiguousarray(np.broadcast_to(rows[None], (128, 2, 2, 1024)))

def l0c_inputs(inp, core, UT, oT, mcT, modT, layer=0):
    a = np.arange(64)
    C64 = np.cos(2 * np.pi * np.outer(a, a) / 64); S64 = np.sin(2 * np.pi * np.outer(a, a) / 64)
    ng = np.stack([lay_vec(inp['norm_g'][layer, 0]), lay_vec(inp['norm_g'][layer, 1])], axis=1)
    return dict(x=np.ascontiguousarray(inp['x'][0, core * 2048:(core + 1) * 2048]), ctx=np.ascontiguousarray(inp['ctx'][0]),
                UT=np.ascontiguousarray(UT), oT=oT, mcT=mcT, Gsrc=gsrc_from_modT(modT), modT=modT, ng=np.ascontiguousarray(ng),
                wout=np.ascontiguousarray(inp['mixab_w_out'][0]),
                c64b=np.kron(np.eye(2), C64).astype(np.float32), s64b=np.kron(np.eye(2), S64).astype(np.float32),
                identf=np.eye(128, dtype=np.float32), rw=np.ascontiguousarray(inp['router_w']),
                rbb=np.ascontiguousarray(np.broadcast_to(inp['router_b'][None, :], (128, 16))),
                w1=np.ascontiguousarray(inp['moe_w1'][layer]), w3=np.ascontiguousarray(inp['moe_w3'][layer]),
                w2=np.ascontiguousarray(inp['moe_w2'][layer]))


def _run(kb, ins):
    return run_bass_kernel_spmd(kb.nc, ins, core_ids=list(range(8))).results


def kernel(**inputs):
    inp = {k_: np.asarray(v) for k_, v in inputs.items()}
    NC = 8
    kb, _ = build_l0a()
    r = _run(kb, [l0a_inputs(inp, c) for c in range(NC)])
    aT = np.concatenate([np.asarray(r[c]["aT"]) for c in range(NC)], axis=1)
    oT = [np.asarray(r[c]["oT"]) for c in range(NC)]
    mcT = np.asarray(r[0]["mcT"])
    modT0 = np.asarray(r[0]["modT"])
    del r
    kb, _ = build_l0b()
    cst = l0b_consts()
    r = _run(kb, [l0b_inputs(aT, c, cst) for c in range(NC)])
    U = np.concatenate([np.asarray(r[c]["U"]).transpose(1, 2, 0, 3).reshape(2, 32, 16384) for c in range(NC)], axis=1)
    del r
    kb, _ = build_l0c()
    r = _run(kb, [l0c_inputs(inp, c, U[:, :, c * 2048:(c + 1) * 2048], oT[c], mcT, modT0) for c in range(NC)])
    h_all = np.concatenate([np.asarray(r[c]["h"]) for c in range(NC)], axis=0)
    hc = np.asarray(r[0]["hc"])
    del r, U, oT
    kb, _ = build_l1a()
    r = _run(kb, [l1a_inputs(inp, c, h_all, hc) for c in range(NC)])
    modT1 = np.asarray(r[0]["modT"])
    of = np.concatenate([np.asarray(r[c]["o"]) for c in range(4)], axis=1)
    ob = np.concatenate([np.asarray(r[c]["o"])[::-1] for c in range(4, 8)], axis=1)
    del r
    ng1 = np.ascontiguousarray(np.stack([lay_vec(inp['norm_g'][1, 0]), lay_vec(inp['norm_g'][1, 1])], axis=1))
    G1 = gsrc_from_modT(modT1)
    idn = np.eye(128, dtype=np.float32)
    kb, _ = build_l1b()
    wg = np.ascontiguousarray(inp['ret_w_in'][0][:, 4096:6144])
    wo = np.ascontiguousarray(inp['ret_w_out'][0])
    r = _run(kb, [dict(h0=np.ascontiguousarray(h_all[c * 2048:(c + 1) * 2048]),
                       of=np.ascontiguousarray(of[c * 2048:(c + 1) * 2048]),
                       ob=np.ascontiguousarray(ob[c * 2048:(c + 1) * 2048]),
                       modT=modT1, ng=ng1, wg=wg, wo=wo, Gsrc=G1, identf=idn) for c in range(NC)])
    hmid = [np.asarray(r[c]["hmid"]) for c in range(NC)]
    del r, of, ob
    kb, _ = build_l1c()
    common = dict(modT=modT1, ng=ng1, Gsrc=G1, identf=idn, rw=np.ascontiguousarray(inp['router_w']),
                  rbb=np.ascontiguousarray(np.broadcast_to(inp['router_b'][None, :], (128, 16))),
                  w1=np.ascontiguousarray(inp['moe_w1'][1]), w3=np.ascontiguousarray(inp['moe_w3'][1]),
                  w2=np.ascontiguousarray(inp['moe_w2'][1]),
                  fgb=np.ascontiguousarray(np.broadcast_to(inp['final_norm_g'][None, :], (128, 1024))))
    r = _run(kb, [dict(common, hmid=hmid[c]) for c in range(NC)])
    out = np.concatenate([np.asarray(r[c]["out"]) for c in range(NC)], axis=0)
    return out.reshape(1, 16384, 1024).astype(np.float32, copy=False)
```
